# Optimizing a Trainium2 kernel written in Bass

```python
import jax, jax.numpy as jnp
from jax import lax
import numpy as np

D_MODEL = 4096
BATCH = 1
SEQ = 16384
DEPTH = 1

CHUNK = 64
Q_BLOCK = 128
HEAD_DIM = 128
FOX_HEADS = 16
HGRN_HEADS = 16
FOX_WIDTH = FOX_HEADS * HEAD_DIM
HGRN_WIDTH = HGRN_HEADS * HEAD_DIM
N_GROUPS = 8
EXPERTS_PER_GROUP = 8
N_EXPERTS = N_GROUPS * EXPERTS_PER_GROUP
TOP_K_IN_GROUP = 2
D_EXPERT = 512
EXPERT_BLOCK = 128
PLE_DIM = 256
LN_EPS = 1e-5
RMS_EPS = 1e-6
DEEPNORM_ALPHA = (2 * DEPTH) ** 0.25
DEEPNORM_BETA = (8 * DEPTH) ** -0.25
IN_COLS = 3 * FOX_WIDTH + FOX_HEADS + 4 * HGRN_WIDTH + 2 * D_MODEL

kernel_name = 'fox_hgrn2_hier_moe_deepnorm_block'


def _layer_norm(x, g, b):
    xf = x.astype(jnp.float32)
    mu = jnp.mean(xf, axis=-1, keepdims=True)
    xc = xf - mu
    var = jnp.mean(xc * xc, axis=-1, keepdims=True)
    return (xc * lax.rsqrt(var + LN_EPS) * g.astype(jnp.float32) + b.astype(jnp.float32)).astype(x.dtype)


def _split_columns(a, sizes):
    out, start = [], 0
    for s in sizes:
        out.append(a[..., start:start + s])
        start += s
    return out


def _fox_attention(q, k, v, log_f):
    B, T, H, dh = q.shape
    nb = T // Q_BLOCK
    q = q.transpose(0, 2, 1, 3)
    k = k.transpose(0, 2, 1, 3)
    v = v.transpose(0, 2, 1, 3)
    c = jnp.cumsum(log_f, axis=1).transpose(0, 2, 1)
    scale = dh ** -0.5
    q_blocks = q.reshape(B, H, nb, Q_BLOCK, dh).transpose(2, 0, 1, 3, 4)
    c_blocks = c.reshape(B, H, nb, Q_BLOCK).transpose(2, 0, 1, 3)
    starts = jnp.arange(nb, dtype=jnp.int32) * Q_BLOCK
    k_pos = jnp.arange(T, dtype=jnp.int32)

    def block(args):
        q_blk, c_blk, start = args
        s = jnp.einsum('bhqd,bhkd->bhqk', q_blk, k).astype(jnp.float32) * scale
        s = s + c_blk[..., :, None] - c[:, :, None, :]
        q_pos = start + jnp.arange(Q_BLOCK, dtype=jnp.int32)
        mask = k_pos[None, :] <= q_pos[:, None]
        s = jnp.where(mask, s, -jnp.inf)
        prob = jax.nn.softmax(s, axis=-1)
        return jnp.einsum('bhqk,bhkd->bhqd', prob.astype(v.dtype), v)

    o = lax.map(block, (q_blocks, c_blocks, starts))
    return o.transpose(1, 0, 3, 2, 4).reshape(B, T, H * dh)


def _hgrn2_recurrence(q, k, v, log_f):
    B, T, H, K = q.shape
    V = v.shape[-1]
    nc = T // CHUNK

    def to_chunks(a):
        return a.reshape(B, nc, CHUNK, H, a.shape[-1]).transpose(1, 0, 3, 2, 4)

    causal = jnp.tril(jnp.ones((CHUNK, CHUNK), dtype=bool))

    def step(S, xs):
        qc, kc, vc, lc = xs
        b = jnp.cumsum(lc, axis=2)
        o_inter = jnp.einsum('bhlk,bhkv->bhlv', qc * jnp.exp(b), S)
        diff = b[:, :, :, None, :] - b[:, :, None, :, :]
        decay = jnp.exp(jnp.where(causal[:, :, None], diff, -jnp.inf))
        a = jnp.einsum('bhtk,bhsk,bhtsk->bhts', qc, kc, decay)
        o_intra = jnp.einsum('bhts,bhsv->bhtv', a, vc)
        b_last = b[:, :, -1:, :]
        S = jnp.exp(b_last[:, :, 0, :])[..., None] * S + jnp.einsum('bhsk,bhsv->bhkv', kc * jnp.exp(b_last - b), vc)
        return S, o_inter + o_intra

    S0 = jnp.zeros((B, H, K, V), jnp.float32)
    _, o = lax.scan(step, S0, (to_chunks(q), to_chunks(k), to_chunks(v), to_chunks(log_f)))
    return o.transpose(1, 0, 3, 2, 4).reshape(B, T, H, V)


def _hier_moe(h, w_rg, b_rg, w_re, b_re, w_gate, w_up, w_down):
    B, T, D = h.shape
    N = B * T
    xf = h.reshape(N, D)
    g_logits = (xf @ w_rg).astype(jnp.float32) + b_rg.astype(jnp.float32)
    g_prob = jax.nn.softmax(g_logits, axis=-1)
    g_sel = jnp.argmax(g_logits, axis=-1).astype(jnp.int32)
    g_w = jnp.take_along_axis(g_prob, g_sel[:, None], axis=1)[:, 0]
    e_logits = ((xf @ w_re).astype(jnp.float32) + b_re.astype(jnp.float32)).reshape(N, N_GROUPS, EXPERTS_PER_GROUP)
    e_logits = jnp.take_along_axis(e_logits, g_sel[:, None, None], axis=1)[:, 0]
    e_prob = jax.nn.softmax(e_logits, axis=-1)
    top_p, top_i = lax.top_k(e_prob, TOP_K_IN_GROUP)
    gate = top_p / jnp.sum(top_p, axis=-1, keepdims=True) * g_w[:, None]
    expert_id = g_sel[:, None] * EXPERTS_PER_GROUP + top_i.astype(jnp.int32)

    M = N * TOP_K_IN_GROUP
    n_blocks = -(-(M + N_EXPERTS * (EXPERT_BLOCK - 1)) // EXPERT_BLOCK)
    P = n_blocks * EXPERT_BLOCK
    flat_e = expert_id.reshape(M)
    flat_w = gate.reshape(M)
    flat_tok = jnp.repeat(jnp.arange(N, dtype=jnp.int32), TOP_K_IN_GROUP)
    order = jnp.argsort(flat_e)
    sorted_e = flat_e[order]
    counts = jnp.bincount(flat_e, length=N_EXPERTS).astype(jnp.int32)
    starts = jnp.cumsum(counts) - counts
    pcounts = (counts + EXPERT_BLOCK - 1) // EXPERT_BLOCK * EXPERT_BLOCK
    pends = jnp.cumsum(pcounts)
    pstarts = pends - pcounts
    rank = jnp.arange(M, dtype=jnp.int32) - starts[sorted_e]
    dest = pstarts[sorted_e] + rank
    slot_tok = jnp.full((P,), N, jnp.int32).at[dest].set(flat_tok[order])
    slot_w = jnp.zeros((P,), jnp.float32).at[dest].set(flat_w[order])
    block_start = jnp.arange(n_blocks, dtype=jnp.int32) * EXPERT_BLOCK
    block_exp = jnp.clip(jnp.searchsorted(pends, block_start, side='right'), 0, N_EXPERTS - 1).astype(jnp.int32)
    x_pad = jnp.concatenate([xf, jnp.zeros((1, D), xf.dtype)], axis=0)

    def body(y, blk):
        tok, e, wt = blk
        xb = x_pad[tok]
        hid = jax.nn.silu(xb @ w_gate[e]) * (xb @ w_up[e])
        out = (hid @ w_down[e]) * wt[:, None].astype(xb.dtype)
        return y.at[tok].add(out.astype(y.dtype)), None

    y0 = jnp.zeros((N + 1, D), xf.dtype)
    y, _ = lax.scan(body, y0, (slot_tok.reshape(n_blocks, EXPERT_BLOCK), block_exp, slot_w.reshape(n_blocks, EXPERT_BLOCK)))
    return y[:N].reshape(B, T, D)


def setup_inputs(seed: int = 0) -> dict:
    key = jax.random.key(seed)
    ks = jax.random.split(key, 23)
    f32 = jnp.float32
    L = DEPTH

    def nrm(k, shape, scale):
        return jax.random.normal(k, shape, f32) * scale

    return {
        'x': nrm(ks[0], (BATCH, SEQ, D_MODEL), 1.0),
        'p': nrm(ks[1], (L, BATCH, SEQ, PLE_DIM), 1.0),
        'w_in': nrm(ks[2], (L, D_MODEL, IN_COLS), D_MODEL ** -0.5),
        'b_fox_f': 3.0 + nrm(ks[3], (L, FOX_HEADS), 0.5),
        'hgrn_lb': nrm(ks[4], (L + 1, HGRN_WIDTH), 0.1),
        'hgrn_norm_g': 1.0 + nrm(ks[5], (L, HGRN_WIDTH), 0.01),
        'w_branch_a': nrm(ks[6], (L, FOX_WIDTH, D_MODEL), FOX_WIDTH ** -0.5),
        'w_branch_b': nrm(ks[7], (L, HGRN_WIDTH, D_MODEL), HGRN_WIDTH ** -0.5),
        'w_out': nrm(ks[8], (L, D_MODEL, D_MODEL), DEEPNORM_BETA * D_MODEL ** -0.5),
        'ln1_g': 1.0 + nrm(ks[9], (L, D_MODEL), 0.01),
        'ln1_b': nrm(ks[10], (L, D_MODEL), 0.01),
        'w_group_router': nrm(ks[11], (L, D_MODEL, N_GROUPS), D_MODEL ** -0.5),
        'b_group_router': nrm(ks[12], (L, N_GROUPS), 0.01),
        'w_expert_router': nrm(ks[13], (L, D_MODEL, N_EXPERTS), D_MODEL ** -0.5),
        'b_expert_router': nrm(ks[14], (L, N_EXPERTS), 0.01),
        'w_exp_gate': nrm(ks[15], (L, N_EXPERTS, D_MODEL, D_EXPERT), D_MODEL ** -0.5),
        'w_exp_up': nrm(ks[16], (L, N_EXPERTS, D_MODEL, D_EXPERT), D_MODEL ** -0.5),
        'w_exp_down': nrm(ks[17], (L, N_EXPERTS, D_EXPERT, D_MODEL), DEEPNORM_BETA * D_EXPERT ** -0.5),
        'ln2_g': 1.0 + nrm(ks[18], (L, D_MODEL), 0.01),
        'ln2_b': nrm(ks[19], (L, D_MODEL), 0.01),
        'w_ple_gate': nrm(ks[20], (L, D_MODEL, D_MODEL), D_MODEL ** -0.5),
        'b_ple_gate': nrm(ks[21], (L, D_MODEL), 0.01),
        'w_ple_proj': nrm(ks[22], (L, PLE_DIM, D_MODEL), PLE_DIM ** -0.5),
    }


def reference(x, p, w_in, b_fox_f, hgrn_lb, hgrn_norm_g, w_branch_a, w_branch_b, w_out, ln1_g, ln1_b,
              w_group_router, b_group_router, w_expert_router, b_expert_router, w_exp_gate, w_exp_up,
              w_exp_down, ln2_g, ln2_b, w_ple_gate, b_ple_gate, w_ple_proj):
    B, T, D = x.shape
    f32 = jnp.float32
    sizes = (FOX_WIDTH, FOX_WIDTH, FOX_WIDTH, FOX_HEADS,
             HGRN_WIDTH, HGRN_WIDTH, HGRN_WIDTH, HGRN_WIDTH, D_MODEL, D_MODEL)
    lower_bounds = jnp.cumsum(jax.nn.softmax(hgrn_lb.astype(f32), axis=0), axis=0)
    for layer in range(DEPTH):
        u = x @ w_in[layer]
        qa, ka, va, fa, qb, fb, ib, gb, gate_a, gate_b = _split_columns(u, sizes)

        log_fa = jax.nn.log_sigmoid(fa.astype(f32) + b_fox_f[layer].astype(f32))
        oa = _fox_attention(qa.reshape(B, T, FOX_HEADS, HEAD_DIM), ka.reshape(B, T, FOX_HEADS, HEAD_DIM),
                            va.reshape(B, T, FOX_HEADS, HEAD_DIM), log_fa)

        lb = lower_bounds[layer]
        f_b = lb + (1.0 - lb) * jax.nn.sigmoid(fb.astype(f32))
        shp = (B, T, HGRN_HEADS, HEAD_DIM)
        ob = _hgrn2_recurrence((qb.astype(f32) * HEAD_DIM ** -0.5).reshape(shp), (1.0 - f_b).reshape(shp),
                               ib.astype(f32).reshape(shp), jnp.log(f_b).reshape(shp))
        ob = ob * lax.rsqrt(jnp.mean(ob * ob, axis=-1, keepdims=True) + RMS_EPS)
        ob = ob * hgrn_norm_g[layer].astype(f32).reshape(HGRN_HEADS, HEAD_DIM)
        ob = (ob.reshape(B, T, HGRN_WIDTH) * jax.nn.silu(gb.astype(f32))).astype(x.dtype)

        y = jax.nn.sigmoid(gate_a) * (oa @ w_branch_a[layer]) + jax.nn.sigmoid(gate_b) * (ob @ w_branch_b[layer])
        x = _layer_norm(DEEPNORM_ALPHA * x + y @ w_out[layer], ln1_g[layer], ln1_b[layer])

        moe = _hier_moe(x, w_group_router[layer], b_group_router[layer], w_expert_router[layer],
                        b_expert_router[layer], w_exp_gate[layer], w_exp_up[layer], w_exp_down[layer])
        x = _layer_norm(DEEPNORM_ALPHA * x + moe, ln2_g[layer], ln2_b[layer])

        x = x + jax.nn.sigmoid(x @ w_ple_gate[layer] + b_ple_gate[layer]) * (p[layer] @ w_ple_proj[layer])
    return x
```

```python
import numpy as np
from contextlib import ExitStack
import concourse.bass as bass
import concourse.mybir as mybir
from concourse.bass_utils import run_bass_kernel_spmd

F32 = mybir.dt.float32
BF16 = mybir.dt.bfloat16
U32 = mybir.dt.uint32
AF = mybir.ActivationFunctionType
ALU = mybir.AluOpType
AX = mybir.AxisListType

NCORES = 8
D = 4096
FOXH = 16
HD = 128
NG = 8
EPG = 8
NE = 64
DE = 512
PLE = 256
ALPHA = 2.0 ** 0.25
LN_EPS = 1e-5
RMS_EPS = 1e-6


class Sched:
    COMPUTE = ("tensor", "vector", "scalar", "gpsimd")
    LIMIT = 30000

    def __init__(self, nc, stack, ndma=8, nrot=3):
        self.nc = nc
        self.eng = {"tensor": nc.tensor, "vector": nc.vector, "scalar": nc.scalar,
                    "gpsimd": nc.gpsimd, "sync": nc.sync}
        self.csem = {e: [stack.enter_context(nc.semaphore(f"c_{e}_{k}")) for k in range(nrot)]
                     for e in self.COMPUTE}
        self.ccount = {e: 0 for e in self.COMPUTE}
        self.dsem = {q: [stack.enter_context(nc.semaphore(f"d_{q}_{k}")) for k in range(ndma)]
                     for q in ("sync", "gpsimd")}
        self.dcount = {q: [0] * ndma for q in ("sync", "gpsimd")}
        self.dnext = {q: 0 for q in ("sync", "gpsimd")}
        self.known = {e: {} for e in self.eng}
        self.kord = {e: {c: -1 for c in self.COMPUTE} for e in self.eng}
        self.last_w = {}
        self.readers = {}
        self.out_events = []

    def _wait(self, e, ev):
        if ev[0] == "c":
            _, src, n = ev
            if src == e and e == "tensor":
                return
            if self.kord[e][src] >= n:
                return
            k, v = divmod(n, self.LIMIT)
            self.eng[e].wait_ge(self.csem[src][k], v + 1)
            self.kord[e][src] = n
        else:
            _, sem, val, key = ev
            if self.known[e].get(key, 0) >= val:
                return
            self.eng[e].wait_ge(sem, val)
            self.known[e][key] = val

    def _deps(self, reads, writes):
        evs = []
        for b in reads:
            if b in self.last_w:
                evs.append(self.last_w[b])
        for b in writes:
            if b in self.last_w:
                evs.append(self.last_w[b])
            r = self.readers.get(b)
            if r:
                evs.extend(r["c"].values())
                evs.extend(r["d"])
        return evs

    def _record(self, ev, reads, writes):
        for b in writes:
            self.last_w[b] = ev
            self.readers[b] = {"c": {}, "d": []}
        for b in reads:
            r = self.readers.setdefault(b, {"c": {}, "d": []})
            if ev[0] == "c":
                r["c"][ev[1]] = ev
            else:
                r["d"].append(ev)

    def op(self, e, fn, reads=(), writes=(), signal=True):
        self.nops = getattr(self, "nops", 0) + 1
        import os
        if self.nops > int(os.environ.get("DBG_LIMIT", "100000000")):
            return None
        for ev in self._deps(reads, writes):
            self._wait(e, ev)
        ins = fn(self.eng[e])
        if not signal:
            return None
        n = self.ccount[e]
        self.ccount[e] += 1
        k, _ = divmod(n, self.LIMIT)
        ins.then_inc(self.csem[e][k], 1)
        ev = ("c", e, n)
        self._record(ev, reads, writes)
        return ev

    def dma(self, q, out, in_, reads=(), writes=(), is_output=False):
        slot = self.dnext[q]
        self.dnext[q] = (slot + 1) % len(self.dsem[q])
        sem = self.dsem[q][slot]
        key = (q, slot)
        if self.dcount[q][slot] > 0:
            self._wait(q, ("d", sem, self.dcount[q][slot], key))
        for ev in self._deps(reads, writes):
            self._wait(q, ev)
        self.eng[q].dma_start(out=out, in_=in_).then_inc(sem, 16)
        self.dcount[q][slot] += 16
        ev = ("d", sem, self.dcount[q][slot], key)
        self._record(ev, reads, writes)
        if is_output:
            self.out_events.append(ev)
        return ev

    def finish(self):
        for c in self.COMPUTE:
            if self.ccount[c] > 0:
                self._wait("sync", ("c", c, self.ccount[c] - 1))
        for ev in self.out_events:
            self._wait("sync", ev)
        for q in self.dsem:
            for slot, sem in enumerate(self.dsem[q]):
                if self.dcount[q][slot] > 0:
                    self._wait("sync", ("d", sem, self.dcount[q][slot], (q, slot)))


def _run(nc, in_maps):
    res = run_bass_kernel_spmd(nc, in_maps, core_ids=list(range(NCORES)))
    return res.results


def _c(a):
    return np.ascontiguousarray(a, dtype=np.float32)


def build_gemm(M, N, groups, epilogue, extra=(), out_cols=None, B=1, NB=512, row_final=None,
               consts=None, outs=None, SBo=None):
    nc = bass.Bass("TRN2", target_bir_lowering=False)
    out_cols = N if out_cols is None else out_cols
    dr = {}
    for g in groups:
        if g["xt"] not in dr:
            dr[g["xt"]] = nc.dram_tensor(g["xt"], [B, g["K"], M], F32, kind="ExternalInput").ap()
        dr[g["w"]] = nc.dram_tensor(g["w"], [B, g["K"], N], F32, kind="ExternalInput").ap()
    for name, shape in extra:
        dr[name] = nc.dram_tensor(name, list(shape), F32, kind="ExternalInput").ap()
    outs = outs or [("y", [B, M, out_cols])]
    for name, shape in outs:
        dr[name] = nc.dram_tensor(name, list(shape), F32, kind="ExternalOutput").ap()
    SB = 512 if M % 512 == 0 else 128
    if SBo is not None:
        SB = SBo
    nsub = SB // 128
    with ExitStack() as st:
        s = Sched(nc, st)
        xt_t = {}
        for g in groups:
            if g["xt"] not in xt_t:
                KC = g["K"] // 128
                xt_t[g["xt"]] = st.enter_context(nc.sbuf_tensor("xt_" + g["xt"], [128, KC, SB], BF16))
        w_t = {}
        for g in groups:
            KC = g["K"] // 128
            w_t[g["w"]] = [st.enter_context(nc.sbuf_tensor(f"w_{g['w']}_{i}", [128, KC, NB], BF16))
                           for i in range(2)]
        ps = [[st.enter_context(nc.psum_tensor(f"ps_{gi}_{i}", [128, 512], F32)) for i in range(2)]
              for gi in range(len(groups))]
        ctx = dict(nc=nc, s=s, st=st, dr=dr, SB=SB, nsub=nsub, NB=NB, M=M, N=N)
        if consts is not None:
            consts(ctx)
        it = 0
        for b in range(B):
            for sb in range(M // SB):
                r0 = sb * SB
                for name, t in xt_t.items():
                    K = dr[name].shape[1]
                    s.dma("gpsimd", t[:, :, :],
                          dr[name][b, :, r0:r0 + SB].rearrange("(kc p) m -> p kc m", p=128),
                          writes=[("xt", name)])
                for nb in range((N + NB - 1) // NB):
                    n0 = nb * NB
                    nbw = min(NB, N - n0)
                    par = it % 2
                    it += 1
                    for g in groups:
                        s.dma("gpsimd", w_t[g["w"]][par][:, :, 0:nbw],
                              dr[g["w"]][b, :, n0:n0 + nbw].rearrange("(kc p) n -> p kc n", p=128),
                              writes=[("w", g["w"], par)])
                    for sub in range(nsub):
                        pp = (it * nsub + sub) % 2
                        for gi, g in enumerate(groups):
                            KC = g["K"] // 128
                            for kc in range(KC):
                                s.op("tensor",
                                     lambda e, gi=gi, g=g, kc=kc, pp=pp, sub=sub, par=par, KC=KC, nbw=nbw:
                                     e.matmul(ps[gi][pp][:, 0:nbw],
                                              xt_t[g["xt"]][:, kc, sub * 128:(sub + 1) * 128],
                                              w_t[g["w"]][par][:, kc, 0:nbw],
                                              start=(kc == 0), stop=(kc == KC - 1)),
                                     reads=[("xt", g["xt"]), ("w", g["w"], par)],
                                     writes=[("ps", gi, pp)], signal=(kc == KC - 1))
                        epilogue(ctx, [ps[gi][pp] for gi in range(len(groups))],
                                 [("ps", gi, pp) for gi in range(len(groups))],
                                 b, r0 + sub * 128, n0, nbw)
                if row_final is not None:
                    row_final(ctx, b, r0)
        s.finish()
    return nc


class Rot:
    def __init__(self, ctx, name, shape, dtype, n=2):
        self.t = [ctx["st"].enter_context(ctx["nc"].sbuf_tensor(f"{name}_{i}", list(shape), dtype))
                  for i in range(n)]
        self.name = name
        self.i = 0

    def next(self):
        k = self.i % len(self.t)
        self.i += 1
        return self.t[k], (self.name, k)


def epi_simple(func=None, bias=None, rowscale=None, out="y"):
    S = {}

    def consts(ctx):
        nc, s, dr = ctx["nc"], ctx["s"], ctx["dr"]
        S["o"] = Rot(ctx, "ost", [128, 512], F32, 3)
        if bias:
            S["b"] = ctx["st"].enter_context(nc.sbuf_tensor("biasb", [128, ctx["N"]], F32))
            s.dma("sync", S["b"][:, :], dr[bias][:, :], writes=["biasb"])
        if rowscale:
            S["rs"] = Rot(ctx, "rs", [128, 1], F32, 2)

    def epi(ctx, ps, keys, b, row0, n0, nbw):
        s, dr = ctx["s"], ctx["dr"]
        o, ok = S["o"].next()
        if bias:
            s.op("vector", lambda e: e.tensor_tensor(o[:, 0:nbw], ps[0][:, 0:nbw], S["b"][:, n0:n0 + nbw], ALU.add),
                 reads=[keys[0], "biasb"], writes=[ok])
            if func is not None:
                s.op("scalar", lambda e: e.activation(out=o[:, 0:nbw], in_=o[:, 0:nbw], func=func),
                     reads=[ok], writes=[ok])
        else:
            s.op("scalar", lambda e: e.activation(out=o[:, 0:nbw], in_=ps[0][:, 0:nbw],
                                                  func=(func if func is not None else AF.Copy)),
                 reads=[keys[0]], writes=[ok])
        if rowscale:
            rs, rk = S["rs"].next()
            s.dma("sync", rs[:, :], dr[rowscale][b, row0:row0 + 128, :], writes=[rk])
            s.op("vector", lambda e: e.tensor_scalar(o[:, 0:nbw], o[:, 0:nbw], rs[:, 0:1], None, ALU.mult),
                 reads=[ok, rk], writes=[ok])
        s.dma("sync", dr[out][b, row0:row0 + 128, n0:n0 + nbw], o[:, 0:nbw], reads=[ok], is_output=True)

    return consts, epi


def epi_glu():
    S = {}

    def consts(ctx):
        S["o"] = Rot(ctx, "ost", [128, 512], F32, 3)

    def epi(ctx, ps, keys, b, row0, n0, nbw):
        s, dr = ctx["s"], ctx["dr"]
        o, ok = S["o"].next()
        s.op("scalar", lambda e: e.activation(out=o[:, 0:nbw], in_=ps[0][:, 0:nbw], func=AF.Silu),
             reads=[keys[0]], writes=[ok])
        s.op("vector", lambda e: e.tensor_tensor(o[:, 0:nbw], o[:, 0:nbw], ps[1][:, 0:nbw], ALU.mult),
             reads=[ok, keys[1]], writes=[ok])
        s.dma("sync", dr["y"][b, row0:row0 + 128, n0:n0 + nbw], o[:, 0:nbw], reads=[ok], is_output=True)

    return consts, epi


def epi_merge():
    S = {}

    def consts(ctx):
        S["o"] = Rot(ctx, "ost", [128, 512], F32, 3)
        S["t"] = Rot(ctx, "tst", [128, 512], F32, 2)

    def epi(ctx, ps, keys, b, row0, n0, nbw):
        s, dr = ctx["s"], ctx["dr"]
        o, ok = S["o"].next()
        t, tk = S["t"].next()
        s.op("scalar", lambda e: e.activation(out=o[:, 0:nbw], in_=ps[0][:, 0:nbw], func=AF.Sigmoid),
             reads=[keys[0]], writes=[ok])
        s.op("scalar", lambda e: e.activation(out=t[:, 0:nbw], in_=ps[1][:, 0:nbw], func=AF.Sigmoid),
             reads=[keys[1]], writes=[tk])
        s.op("vector", lambda e: e.tensor_tensor(o[:, 0:nbw], o[:, 0:nbw], ps[2][:, 0:nbw], ALU.mult),
             reads=[ok, keys[2]], writes=[ok])
        s.op("vector", lambda e: e.tensor_tensor(t[:, 0:nbw], t[:, 0:nbw], ps[3][:, 0:nbw], ALU.mult),
             reads=[tk, keys[3]], writes=[tk])
        s.op("vector", lambda e: e.tensor_tensor(o[:, 0:nbw], o[:, 0:nbw], t[:, 0:nbw], ALU.add),
             reads=[ok, tk], writes=[ok])
        s.dma("sync", dr["y"][b, row0:row0 + 128, n0:n0 + nbw], o[:, 0:nbw], reads=[ok], is_output=True)

    return consts, epi


def ln_rows(ctx, S, r, rk, gk="lng", bk="lnb"):
    s = ctx["s"]
    st1, k1 = S["st"].next()
    s.op("vector", lambda e: e.reduce_sum(st1[:, 0:1], r[:, :], AX.X), reads=[rk], writes=[k1])
    s.op("vector", lambda e: e.tensor_scalar(st1[:, 0:1], st1[:, 0:1], -1.0 / D, None, ALU.mult),
         reads=[k1], writes=[k1])
    s.op("vector", lambda e: e.tensor_scalar(r[:, :], r[:, :], st1[:, 0:1], None, ALU.add),
         reads=[rk, k1], writes=[rk])
    sq, sk = S["sq"].next()
    s.op("scalar", lambda e: e.activation(out=sq[:, :], in_=r[:, :], func=AF.Square, accum_out=st1[:, 1:2]),
         reads=[rk], writes=[sk, k1])
    s.op("vector", lambda e: e.tensor_scalar(st1[:, 1:2], st1[:, 1:2], 1.0 / D, LN_EPS, ALU.mult, ALU.add),
         reads=[k1], writes=[k1])
    s.op("scalar", lambda e: e.activation(out=st1[:, 1:2], in_=st1[:, 1:2], func=AF.Sqrt),
         reads=[k1], writes=[k1])
    s.op("vector", lambda e: e.reciprocal(st1[:, 1:2], st1[:, 1:2]), reads=[k1], writes=[k1])
    s.op("vector", lambda e: e.scalar_tensor_tensor(r[:, :], r[:, :], st1[:, 1:2], S["g"][:, :], ALU.mult, ALU.mult),
         reads=[rk, k1, gk], writes=[rk])
    s.op("vector", lambda e: e.tensor_tensor(r[:, :], r[:, :], S["b"][:, :], ALU.add),
         reads=[rk, bk], writes=[rk])


def ln_consts(ctx, S, gname, bname):
    nc, s, dr, st = ctx["nc"], ctx["s"], ctx["dr"], ctx["st"]
    S["g"] = st.enter_context(nc.sbuf_tensor("lng", [128, D], F32))
    S["b"] = st.enter_context(nc.sbuf_tensor("lnb", [128, D], F32))
    s.dma("sync", S["g"][:, :], dr[gname][:, :], writes=["lng"])
    s.dma("sync", S["b"][:, :], dr[bname][:, :], writes=["lnb"])
    S["st"] = Rot(ctx, "lnst", [128, 2], F32, 2)
    S["sq"] = Rot(ctx, "lnsq", [128, D], F32, 1)


def epi_res_ln():
    S = {}

    def consts(ctx):
        ln_consts(ctx, S, "ln_g", "ln_b")
        S["r"] = Rot(ctx, "rrow", [128, D], F32, 2)
        S["cur"] = {}

    def epi(ctx, ps, keys, b, row0, n0, nbw):
        s, dr = ctx["s"], ctx["dr"]
        if n0 == 0 and row0 not in S["cur"]:
            pass
        key = row0
        if key not in S["cur"]:
            r, rk = S["r"].next()
            S["cur"][key] = (r, rk)
            s.dma("sync", r[:, :], dr["xres"][row0:row0 + 128, :], writes=[rk])
        r, rk = S["cur"][key]
        s.op("vector", lambda e: e.scalar_tensor_tensor(r[:, n0:n0 + nbw], r[:, n0:n0 + nbw], ALPHA,
                                                        ps[0][:, 0:nbw], ALU.mult, ALU.add),
             reads=[rk, keys[0]], writes=[rk])

    def row_final(ctx, b, r0):
        s, dr = ctx["s"], ctx["dr"]
        for sub in range(ctx["nsub"]):
            row0 = r0 + sub * 128
            r, rk = S["cur"].pop(row0)
            ln_rows(ctx, S, r, rk)
            s.dma("sync", dr["y"][b, row0:row0 + 128, :], r[:, :], reads=[rk], is_output=True)

    return consts, epi, row_final


def _mk_consts(nc, s, st):
    C = {}
    C["J"] = st.enter_context(nc.sbuf_tensor("cJ", [128, 512], F32))
    C["U"] = st.enter_context(nc.sbuf_tensor("cU", [128, 128], F32))
    C["onesf"] = st.enter_context(nc.sbuf_tensor("cOf", [128, 128], F32))
    C["onesb"] = st.enter_context(nc.sbuf_tensor("cOb", [128, 128], BF16))
    C["sel"] = st.enter_context(nc.sbuf_tensor("cSel", [128, 128], F32))
    s.op("gpsimd", lambda e: e.iota(C["J"][:, :], [[1, 512]], base=0, channel_multiplier=-1,
                                    allow_small_or_imprecise_dtypes=True), writes=["cJ"])
    s.op("vector", lambda e: e.tensor_single_scalar(C["U"][:, :], C["J"][:, 0:128], 0.0, ALU.is_ge),
         reads=["cJ"], writes=["cU"])
    s.op("vector", lambda e: e.memset(C["onesf"][:, :], 1.0), writes=["cOf"])
    s.op("vector", lambda e: e.memset(C["onesb"][:, :], 1.0), writes=["cOb"])
    s.op("gpsimd", lambda e: e.iota(C["sel"][:, :], [[0, 128]], base=0, channel_multiplier=1,
                                    allow_small_or_imprecise_dtypes=True), writes=["cSel"])
    s.op("vector", lambda e: e.tensor_single_scalar(C["sel"][:, :], C["sel"][:, :], 127.0, ALU.is_equal),
         reads=["cSel"], writes=["cSel"])
    C["Ub"] = st.enter_context(nc.sbuf_tensor("cUb", [128, 128], BF16))
    C["selb"] = st.enter_context(nc.sbuf_tensor("cSelb", [128, 128], BF16))
    s.op("vector", lambda e: e.tensor_copy(C["Ub"][:, :], C["U"][:, :]), reads=["cU"], writes=["cUb"])
    s.op("vector", lambda e: e.tensor_copy(C["selb"][:, :], C["sel"][:, :]), reads=["cSel"], writes=["cSelb"])
    return C


def split_bf16(s, src, skey, parts, tmp, name, n):
    keys = []
    cur, ck = src, skey
    for i in range(n):
        k = (name, i)
        s.op("vector", lambda e, i=i, cur=cur: e.tensor_copy(parts[i], cur), reads=[ck], writes=[k])
        keys.append(k)
        if i + 1 < n:
            tk = (name, "r")
            s.op("vector", lambda e, i=i, cur=cur: e.tensor_tensor(tmp, cur, parts[i], ALU.subtract),
                 reads=[ck, k], writes=[tk])
            cur, ck = tmp, tk
    return keys


def mm_split(s, out, okey, lhs_parts, lkeys, rhs_parts, rkeys):
    pairs = [(a, b, ka, kb) for a, ka in zip(lhs_parts, lkeys) for b, kb in zip(rhs_parts, rkeys)]
    for idx, (a, b, ka, kb) in enumerate(pairs):
        s.op("tensor", lambda e, a=a, b=b, idx=idx: e.matmul(out, a, b, start=(idx == 0), stop=(idx == len(pairs) - 1)),
             reads=[ka, kb], writes=[okey], signal=(idx == len(pairs) - 1))


def build_attn(T, NH=2):
    nc = bass.Bass("TRN2", target_bir_lowering=False)
    NBk = T // 128
    NQ = T // 512
    qT = nc.dram_tensor("qT", [NH, 128, T], F32, kind="ExternalInput").ap()
    kT = nc.dram_tensor("kT", [NH, 128, T], F32, kind="ExternalInput").ap()
    v = nc.dram_tensor("v", [NH, T, 128], F32, kind="ExternalInput").ap()
    fac = nc.dram_tensor("fac", [NH, 128, NBk], F32, kind="ExternalInput").ap()
    bfb = nc.dram_tensor("bfb", [NH, 128, 1], F32, kind="ExternalInput").ap()
    oT = nc.dram_tensor("oT", [NH, 128, T], F32, kind="ExternalOutput").ap()
    scale = float(HD) ** -0.5
    with ExitStack() as st:
        s = Sched(nc, st)
        C = _mk_consts(nc, s, st)
        sb = lambda n, shp, dt=F32: st.enter_context(nc.sbuf_tensor(n, shp, dt))
        masks = [sb(f"mask{i}", [128, 512], BF16) for i in range(4)]
        for i in range(4):
            s.op("vector", lambda e, i=i: e.tensor_single_scalar(masks[i][:, :], C["J"][:, :], 128.0 * i, ALU.is_ge),
                 reads=["cJ"], writes=[("mask", i)])
        q_sb = sb("q_sb", [128, T], BF16)
        k_sb = sb("k_sb", [128, T], BF16)
        v_sb = sb("v_sb", [128, NBk, 128], BF16)
        z = sb("z", [128, NBk]); a = sb("a", [128, NBk]); ls = sb("ls", [128, NBk])
        tot = sb("tot", [128, NBk]); incl = sb("incl", [128, NBk]); ccol = sb("ccol", [128, NBk])
        lsp = [sb(f"lsp{i}", [128, NBk], BF16) for i in range(3)]; lst = sb("lst", [128, NBk])
        cm = sb("cm", [128, NBk]); bfs = sb("bfs", [128, 1]); biast = [sb(f"biast{i}", [128, NBk]) for i in range(2)]
        Pb = [sb(f"P{i}", [128, 512], BF16) for i in range(3)]
        rec = [sb(f"rec{i}", [128, 512]) for i in range(2)]
        osb = [sb(f"osb{i}", [128, 512]) for i in range(2)]
        psS = [st.enter_context(nc.psum_tensor(f"psS{i}", [128, 512], F32)) for i in range(2)]
        psO = [st.enter_context(nc.psum_tensor(f"psO{i}", [128, 512], F32)) for i in range(2)]
        psD = [st.enter_context(nc.psum_tensor(f"psD{i}", [128, 512], F32)) for i in range(2)]
        psM = st.enter_context(nc.psum_tensor("psM", [128, 512], F32))
        gq = 0
        for h in range(NH):
            s.dma("sync", z[:, :], fac[h], writes=["z"])
            s.dma("sync", bfs[:, :], bfb[h], writes=["bfs"])
            s.op("vector", lambda e: e.tensor_scalar(z[:, :], z[:, :], bfs[:, 0:1], None, ALU.add),
                 reads=["z", "bfs"], writes=["z"])
            s.op("vector", lambda e: e.tensor_scalar(a[:, :], z[:, :], -1.0, None, ALU.mult), reads=["z"], writes=["a"])
            s.op("vector", lambda e: e.tensor_tensor(a[:, :], a[:, :], z[:, :], ALU.max), reads=["z", "a"], writes=["a"])
            s.op("scalar", lambda e: e.activation(out=a[:, :], in_=a[:, :], func=AF.Exp, scale=-1.0), reads=["a"], writes=["a"])
            s.op("scalar", lambda e: e.activation(out=a[:, :], in_=a[:, :], func=AF.Ln, bias=1.0), reads=["a"], writes=["a"])
            s.op("vector", lambda e: e.tensor_scalar_min(ls[:, :], z[:, :], 0.0), reads=["z"], writes=["ls"])
            s.op("vector", lambda e: e.tensor_tensor(ls[:, :], ls[:, :], a[:, :], ALU.subtract), reads=["ls", "a"], writes=["ls"])
            lk = split_bf16(s, ls[:, :], "ls", [x[:, :] for x in lsp], lst[:, :], "lsp", 3)
            mm_split(s, psM[:, 0:NBk], "psM0", [C["Ub"][:, :]], ["cUb"], [x[:, :] for x in lsp], lk)
            mm_split(s, psM[:, NBk:2 * NBk], "psM1", [C["onesb"][:, :]], ["cOb"], [x[:, :] for x in lsp], lk)
            s.op("vector", lambda e: e.tensor_copy(tot[:, :], psM[:, NBk:2 * NBk]), reads=["psM1"], writes=["tot"])
            s.op("vector", lambda e: e.tensor_tensor_scan(incl[:, :], C["onesf"][:, 0:NBk], tot[:, :], 0.0, ALU.mult, ALU.add),
                 reads=["tot", "cOf"], writes=["incl"])
            s.op("vector", lambda e: e.tensor_tensor(incl[:, :], incl[:, :], tot[:, :], ALU.subtract), reads=["incl", "tot"], writes=["incl"])
            s.op("vector", lambda e: e.tensor_tensor(ccol[:, :], incl[:, :], psM[:, 0:NBk], ALU.add), reads=["incl", "psM0"], writes=["ccol"])
            ck_ = split_bf16(s, ccol[:, :], "ccol", [x[:, :] for x in lsp], lst[:, :], "lsp", 3)
            mm_split(s, psM[:, 2 * NBk:3 * NBk], "psM2", [C["selb"][:, :]], ["cSelb"], [x[:, :] for x in lsp], ck_)
            s.op("vector", lambda e: e.tensor_copy(cm[:, :], psM[:, 2 * NBk:3 * NBk]), reads=["psM2"], writes=["cm"])
            for j in range(4):
                sl = slice(j * T // 4, (j + 1) * T // 4)
                s.dma("gpsimd", q_sb[:, sl], qT[h, :, sl], writes=[("q", j)])
                s.dma("gpsimd", k_sb[:, sl], kT[h, :, sl], writes=[("k", j)])
                bs = slice(j * NBk // 4, (j + 1) * NBk // 4)
                s.dma("gpsimd", v_sb[:, bs, :], v[h].rearrange("(b p) d -> p b d", p=128)[:, bs, :], writes=[("v", j)])
            allq = [("q", j) for j in range(4)]; allk = [("k", j) for j in range(4)]; allv = [("v", j) for j in range(4)]
            for qb in range(NQ):
                nkb = 4 * (qb + 1)
                par = gq % 2
                gq += 1
                bt = biast[par]
                s.op("vector", lambda e, bt=bt, qb=qb, nkb=nkb: e.tensor_scalar(
                    bt[:, 0:nkb], ccol[:, 0:nkb], -1.0, cm[:, 4 * qb + 1:4 * qb + 2], ALU.mult, ALU.add),
                    reads=["ccol", "cm"], writes=[("bt", par)])
                qs = slice(qb * 512, (qb + 1) * 512)

                def mmS(kb):
                    s.op("tensor", lambda e: e.matmul(psS[kb % 2][:, :], k_sb[:, kb * 128:(kb + 1) * 128], q_sb[:, qs],
                                                      start=True, stop=True),
                         reads=allq + allk, writes=[("S", kb % 2)])
                mmS(0)
                for kb in range(nkb):
                    if kb + 1 < nkb:
                        mmS(kb + 1)
                    P = Pb[kb % 3]
                    pk = ("P", kb % 3)
                    s.op("scalar", lambda e, P=P, kb=kb, bt=bt: e.activation(
                        out=P[:, :], in_=psS[kb % 2][:, :], func=AF.Exp, bias=bt[:, kb:kb + 1], scale=scale),
                        reads=[("S", kb % 2), ("bt", par)], writes=[pk])
                    di = kb - 4 * qb
                    if di >= 0:
                        s.op("vector", lambda e, P=P, di=di: e.tensor_tensor(P[:, :], P[:, :], masks[di][:, :], ALU.mult),
                             reads=[pk, ("mask", di)], writes=[pk])
                    s.op("tensor", lambda e, P=P, kb=kb: e.matmul(psO[par][:, :], v_sb[:, kb, :], P[:, :],
                                                                  start=(kb == 0), stop=(kb == nkb - 1)),
                         reads=allv + [pk], writes=[("O", par)], signal=False)
                    s.op("tensor", lambda e, P=P, kb=kb: e.matmul(psD[par][:, :], C["onesb"][:, :], P[:, :],
                                                                  start=(kb == 0), stop=(kb == nkb - 1)),
                         reads=["cOb", pk], writes=[("O", par), ("Dn", par)])
                s.op("vector", lambda e: e.reciprocal(rec[par][:, :], psD[par][:, :]), reads=[("Dn", par)], writes=[("rec", par)])
                s.op("vector", lambda e: e.tensor_tensor(osb[par][:, :], psO[par][:, :], rec[par][:, :], ALU.mult),
                     reads=[("O", par), ("rec", par)], writes=[("osb", par)])
                s.dma("sync", oT[h, :, qs], osb[par][:, :], reads=[("osb", par)], is_output=True)
        s.finish()
    return nc


def build_hgrn(T, NH=2):
    nc = bass.Bass("TRN2", target_bir_lowering=False)
    NCH = T // 128
    din = lambda n, shp: nc.dram_tensor(n, shp, F32, kind="ExternalInput").ap()
    fb_tm = din("fb_tm", [NH, T, 128]); ib_tm = din("ib_tm", [NH, T, 128])
    fbT = din("fbT", [NH, 128, T]); qT = din("qT", [NH, 128, T]); gbT = din("gbT", [NH, 128, T])
    a0r = din("a0r", [NH, 128, 128]); a1r = din("a1r", [NH, 128, 128])
    a0c = din("a0c", [NH, 128, 1]); a1c = din("a1c", [NH, 128, 1]); gnc = din("gnc", [NH, 128, 1])
    obT = nc.dram_tensor("obT", [NH, 128, T], F32, kind="ExternalOutput").ap()
    scale = float(HD) ** -0.5
    with ExitStack() as st:
        s = Sched(nc, st)
        C = _mk_consts(nc, s, st)
        U, onesf = C["U"], C["onesf"]
        cnt = [0]

        def sb(shp, dt=F32):
            cnt[0] += 1
            return st.enter_context(nc.sbuf_tensor(f"h{cnt[0]}", shp, dt))
        lbr = sb([128, 128]); omr = sb([128, 128]); lbc = sb([128, 1]); omc = sb([128, 1]); gn = sb([128, 1])
        t1 = sb([128, 128]); t1c = sb([128, 1])
        in_fb = [sb([128, 128]) for _ in range(2)]; in_fbT = [sb([128, 128]) for _ in range(2)]
        in_q = [sb([128, 128]) for _ in range(2)]; in_g = [sb([128, 128]) for _ in range(2)]
        in_v = [sb([128, 128], BF16) for _ in range(2)]
        LFp = [sb([128, 128], BF16) for _ in range(3)]; LFt = sb([128, 128])
        Ftm = sb([128, 128]); LF = sb([128, 128]); KKtm = sb([128, 128]); Ffm = sb([128, 128]); KKfm = sb([128, 128])
        bfm = sb([128, 128]); btm = sb([128, 128]); nbm = sb([128, 1]); eQ = sb([128, 128]); eK = sb([128, 128]); eB = sb([128, 128])
        Qt = sb([128, 128], BF16); Kt = sb([128, 128], BF16); Qb = sb([128, 128], BF16)
        ATm = sb([128, 128], BF16); dif = sb([128, 128]); Kh = sb([128, 128], BF16)
        S = sb([128, 128]); Sbf = sb([128, 128], BF16); ebl = sb([128, 1])
        osb = sb([128, 128]); sq = sb([128, 128]); rstd = sb([128, 128]); sg = sb([128, 128])
        res = [sb([128, 128]) for _ in range(2)]
        pA = st.enter_context(nc.psum_tensor("pA", [128, 512], F32))
        pB = st.enter_context(nc.psum_tensor("pB", [128, 512], F32))
        pC = st.enter_context(nc.psum_tensor("pC", [128, 512], F32))
        pD = st.enter_context(nc.psum_tensor("pD", [128, 512], F32))
        pE = st.enter_context(nc.psum_tensor("pE", [128, 512], F32))
        V = lambda f, r, w: s.op("vector", f, reads=r, writes=w)
        A = lambda f, r, w: s.op("scalar", f, reads=r, writes=w)
        PE = lambda f, r, w: s.op("tensor", f, reads=r, writes=w)
        g = 0
        for h in range(NH):
            s.dma("sync", lbr[:, :], a0r[h], writes=["lbr"]); s.dma("sync", t1[:, :], a1r[h], writes=["t1"])
            s.dma("sync", lbc[:, :], a0c[h], writes=["lbc"]); s.dma("sync", t1c[:, :], a1c[h], writes=["t1c"])
            s.dma("sync", gn[:, :], gnc[h], writes=["gn"])
            V(lambda e: e.tensor_tensor(lbr[:, :], lbr[:, :], t1[:, :], ALU.subtract), ["lbr", "t1"], ["lbr"])
            A(lambda e: e.activation(out=lbr[:, :], in_=lbr[:, :], func=AF.Sigmoid), ["lbr"], ["lbr"])
            V(lambda e: e.tensor_scalar(omr[:, :], lbr[:, :], -1.0, 1.0, ALU.mult, ALU.add), ["lbr"], ["omr"])
            V(lambda e: e.tensor_tensor(lbc[:, :], lbc[:, :], t1c[:, :], ALU.subtract), ["lbc", "t1c"], ["lbc"])
            A(lambda e: e.activation(out=lbc[:, :], in_=lbc[:, :], func=AF.Sigmoid), ["lbc"], ["lbc"])
            V(lambda e: e.tensor_scalar(omc[:, :], lbc[:, :], -1.0, 1.0, ALU.mult, ALU.add), ["lbc"], ["omc"])
            V(lambda e: e.memset(S[:, :], 0.0), [], ["S"])
            V(lambda e: e.memset(Sbf[:, :], 0.0), [], ["Sbf"])
            for c in range(NCH):
                p = g % 2
                g += 1
                ts = slice(c * 128, (c + 1) * 128)
                s.dma("sync", in_fb[p][:, :], fb_tm[h, ts, :], writes=[("ifb", p)])
                s.dma("sync", in_fbT[p][:, :], fbT[h, :, ts], writes=[("ifbT", p)])
                s.dma("sync", in_q[p][:, :], qT[h, :, ts], writes=[("iq", p)])
                s.dma("sync", in_g[p][:, :], gbT[h, :, ts], writes=[("ig", p)])
                s.dma("gpsimd", in_v[p][:, :], ib_tm[h, ts, :], writes=[("iv", p)])
                fbt, fbTt, qt, gt, vt = in_fb[p], in_fbT[p], in_q[p], in_g[p], in_v[p]
                A(lambda e: e.activation(out=Ftm[:, :], in_=fbt[:, :], func=AF.Sigmoid), [("ifb", p)], ["Ftm"])
                V(lambda e: e.tensor_tensor(Ftm[:, :], Ftm[:, :], omr[:, :], ALU.mult), ["Ftm", "omr"], ["Ftm"])
                V(lambda e: e.tensor_tensor(Ftm[:, :], Ftm[:, :], lbr[:, :], ALU.add), ["Ftm", "lbr"], ["Ftm"])
                A(lambda e: e.activation(out=LF[:, :], in_=Ftm[:, :], func=AF.Ln), ["Ftm"], ["LF"])
                V(lambda e: e.tensor_scalar(KKtm[:, :], Ftm[:, :], -1.0, 1.0, ALU.mult, ALU.add), ["Ftm"], ["KKtm"])
                A(lambda e: e.activation(out=Ffm[:, :], in_=fbTt[:, :], func=AF.Sigmoid), [("ifbT", p)], ["Ffm"])
                V(lambda e: e.tensor_scalar(Ffm[:, :], Ffm[:, :], omc[:, 0:1], lbc[:, 0:1], ALU.mult, ALU.add),
                  ["Ffm", "omc", "lbc"], ["Ffm"])
                V(lambda e: e.tensor_scalar(KKfm[:, :], Ffm[:, :], -1.0, 1.0, ALU.mult, ALU.add), ["Ffm"], ["KKfm"])
                lk = split_bf16(s, LF[:, :], "LF", [x[:, :] for x in LFp], LFt[:, :], "LFp", 3)
                lp = [x[:, :] for x in LFp]
                mm_split(s, pA[:, 0:128], "pA0", [C["Ub"][:, :]], ["cUb"], lp, lk)
                mm_split(s, pA[:, 128:256], "pA1", lp, lk, [C["Ub"][:, :]], ["cUb"])
                mm_split(s, pD[:, 256:384], "pA2", [C["onesb"][:, :]], ["cOb"], lp, lk)
                A(lambda e: e.activation(out=bfm[:, :], in_=pA[:, 128:256], func=AF.Copy), ["pA1"], ["bfm"])
                A(lambda e: e.activation(out=btm[:, :], in_=pA[:, 0:128], func=AF.Copy), ["pA0"], ["btm"])
                V(lambda e: e.tensor_scalar(nbm[:, :], bfm[:, 63:64], -1.0, None, ALU.mult), ["bfm"], ["nbm"])
                A(lambda e: e.activation(out=eQ[:, :], in_=bfm[:, :], func=AF.Exp, bias=nbm[:, 0:1], scale=1.0), ["bfm", "nbm"], ["eQ"])
                A(lambda e: e.activation(out=eK[:, :], in_=bfm[:, :], func=AF.Exp, bias=bfm[:, 63:64], scale=-1.0), ["bfm"], ["eK"])
                A(lambda e: e.activation(out=eB[:, :], in_=bfm[:, :], func=AF.Exp), ["bfm"], ["eB"])
                A(lambda e: e.activation(out=ebl[:, :], in_=bfm[:, 127:128], func=AF.Exp), ["bfm"], ["ebl"])
                V(lambda e: e.scalar_tensor_tensor(Qt[:, :], qt[:, :], scale, eQ[:, :], ALU.mult, ALU.mult), [("iq", p), "eQ"], ["Qt"])
                V(lambda e: e.tensor_tensor(Kt[:, :], KKfm[:, :], eK[:, :], ALU.mult), ["KKfm", "eK"], ["Kt"])
                V(lambda e: e.scalar_tensor_tensor(Qb[:, :], qt[:, :], scale, eB[:, :], ALU.mult, ALU.mult), [("iq", p), "eB"], ["Qb"])
                PE(lambda e: e.matmul(pB[:, 0:128], Kt[:, :], Qt[:, :], start=True, stop=True), ["Kt", "Qt"], ["pB"])
                V(lambda e: e.tensor_tensor(ATm[:, :], pB[:, 0:128], U[:, :], ALU.mult), ["pB", "cU"], ["ATm"])
                V(lambda e: e.tensor_tensor(dif[:, :], pD[:, 256:384], btm[:, :], ALU.subtract), ["pA2", "btm"], ["dif"])
                A(lambda e: e.activation(out=dif[:, :], in_=dif[:, :], func=AF.Exp), ["dif"], ["dif"])
                V(lambda e: e.tensor_tensor(Kh[:, :], KKtm[:, :], dif[:, :], ALU.mult), ["KKtm", "dif"], ["Kh"])
                s.op("tensor", lambda e: e.matmul(pC[:, 0:128], vt[:, :], ATm[:, :], start=True, stop=False),
                     reads=[("iv", p), "ATm"], writes=["pC"], signal=False)
                PE(lambda e: e.matmul(pC[:, 0:128], Sbf[:, :], Qb[:, :], start=False, stop=True), ["Sbf", "Qb", ("iv", p), "ATm"], ["pC"])
                PE(lambda e: e.matmul(pD[:, 0:128], Kh[:, :], vt[:, :], start=True, stop=True), ["Kh", ("iv", p)], ["pD"])
                V(lambda e: e.scalar_tensor_tensor(S[:, :], S[:, :], ebl[:, 0:1], pD[:, 0:128], ALU.mult, ALU.add), ["S", "ebl", "pD"], ["S"])
                A(lambda e: e.activation(out=Sbf[:, :], in_=S[:, :], func=AF.Copy), ["S"], ["Sbf"])
                A(lambda e: e.activation(out=osb[:, :], in_=pC[:, 0:128], func=AF.Copy), ["pC"], ["osb"])
                A(lambda e: e.activation(out=sq[:, :], in_=osb[:, :], func=AF.Square), ["osb"], ["sq"])
                qk_ = split_bf16(s, sq[:, :], "sq", [x[:, :] for x in LFp[0:2]], LFt[:, :], "LFp", 2)
                mm_split(s, pE[:, 0:128], "pE", [C["onesb"][:, :]], ["cOb"], [x[:, :] for x in LFp[0:2]], qk_)
                V(lambda e: e.tensor_scalar(rstd[:, :], pE[:, 0:128], 1.0 / HD, RMS_EPS, ALU.mult, ALU.add), ["pE"], ["rstd"])
                A(lambda e: e.activation(out=rstd[:, :], in_=rstd[:, :], func=AF.Sqrt), ["rstd"], ["rstd"])
                V(lambda e: e.reciprocal(rstd[:, :], rstd[:, :]), ["rstd"], ["rstd"])
                A(lambda e: e.activation(out=sg[:, :], in_=gt[:, :], func=AF.Silu), [("ig", p)], ["sg"])
                r = res[p]
                V(lambda e: e.tensor_tensor(r[:, :], osb[:, :], rstd[:, :], ALU.mult), ["osb", "rstd"], [("res", p)])
                V(lambda e: e.scalar_tensor_tensor(r[:, :], r[:, :], gn[:, 0:1], sg[:, :], ALU.mult, ALU.mult), [("res", p), "gn", "sg"], [("res", p)])
                s.dma("sync", obT[h, :, ts], r[:, :], reads=[("res", p)], is_output=True)
        s.finish()
    return nc


def build_route(M):
    nc = bass.Bass("TRN2", target_bir_lowering=False)
    lg = nc.dram_tensor("lg", [M, 72], F32, kind="ExternalInput").ap()
    R = nc.dram_tensor("R", [M, 8], F32, kind="ExternalOutput").ap()
    with ExitStack() as st:
        s = Sched(nc, st)
        cnt = [0]

        def sb(shp, dt=F32):
            cnt[0] += 1
            return st.enter_context(nc.sbuf_tensor(f"r{cnt[0]}", shp, dt))
        io8 = sb([128, 8])
        s.op("gpsimd", lambda e: e.iota(io8[:, :], [[1, 8]], base=0, channel_multiplier=0,
                                        allow_small_or_imprecise_dtypes=True), writes=["io8"])
        V = lambda f, r, w: s.op("vector", f, reads=r, writes=w)
        A = lambda f, r, w: s.op("scalar", f, reads=r, writes=w)
        Ls = [sb([128, 72]) for _ in range(2)]
        Ro = [sb([128, 8]) for _ in range(2)]
        G8 = sb([128, 8]); GI = sb([128, 8], U32); gs = sb([128, 1]); nb = sb([128, 1]); ex = sb([128, 8]); sm = sb([128, 1])
        oh = sb([128, 8]); EL = sb([128, 8]); E8 = sb([128, 8]); EI = sb([128, 8], U32); d = sb([128, 1]); r1 = sb([128, 1])
        for blk in range(M // 128):
            p = blk % 2
            L, Rt = Ls[p], Ro[p]
            rs = slice(blk * 128, (blk + 1) * 128)
            s.dma("sync", L[:, :], lg[rs, :], writes=[("L", p)])
            V(lambda e: e.memset(Rt[:, :], 0.0), [], [("R", p)])
            V(lambda e: e.max(G8[:, :], L[:, 0:8]), [("L", p)], ["G8"])
            V(lambda e: e.max_index(GI[:, :], G8[:, :], L[:, 0:8]), [("L", p), "G8"], ["GI"])
            V(lambda e: e.tensor_copy(gs[:, :], GI[:, 0:1]), ["GI"], ["gs"])
            V(lambda e: e.tensor_scalar(nb[:, :], G8[:, 0:1], -1.0, None, ALU.mult), ["G8"], ["nb"])
            A(lambda e: e.activation(out=ex[:, :], in_=L[:, 0:8], func=AF.Exp, bias=nb[:, 0:1], scale=1.0, accum_out=sm[:, 0:1]),
              [("L", p), "nb"], ["ex", "sm"])
            V(lambda e: e.reciprocal(sm[:, :], sm[:, :]), ["sm"], ["sm"])
            V(lambda e: e.tensor_scalar(oh[:, :], io8[:, :], gs[:, 0:1], None, ALU.is_equal), ["io8", "gs"], ["oh"])
            V(lambda e: e.tensor_scalar(EL[:, :], L[:, 8:16], oh[:, 0:1], None, ALU.mult), [("L", p), "oh"], ["EL"])
            for g in range(1, 8):
                V(lambda e, g=g: e.scalar_tensor_tensor(EL[:, :], L[:, 8 + 8 * g:16 + 8 * g], oh[:, g:g + 1], EL[:, :], ALU.mult, ALU.add),
                  [("L", p), "oh", "EL"], ["EL"])
            V(lambda e: e.max(E8[:, :], EL[:, :]), ["EL"], ["E8"])
            V(lambda e: e.max_index(EI[:, :], E8[:, :], EL[:, :]), ["EL", "E8"], ["EI"])
            V(lambda e: e.tensor_tensor(d[:, :], E8[:, 1:2], E8[:, 0:1], ALU.subtract), ["E8"], ["d"])
            A(lambda e: e.activation(out=d[:, :], in_=d[:, :], func=AF.Exp), ["d"], ["d"])
            V(lambda e: e.tensor_scalar(r1[:, :], d[:, :], 1.0, None, ALU.add), ["d"], ["r1"])
            V(lambda e: e.reciprocal(r1[:, :], r1[:, :]), ["r1"], ["r1"])
            V(lambda e: e.tensor_tensor(r1[:, :], r1[:, :], sm[:, :], ALU.mult), ["r1", "sm"], ["r1"])
            V(lambda e: e.tensor_copy(Rt[:, 0:1], gs[:, :]), ["gs", ("R", p)], [("R", p)])
            V(lambda e: e.tensor_copy(Rt[:, 1:3], EI[:, 0:2]), ["EI", ("R", p)], [("R", p)])
            V(lambda e: e.tensor_copy(Rt[:, 3:4], r1[:, :]), ["r1", ("R", p)], [("R", p)])
            V(lambda e: e.tensor_tensor(Rt[:, 4:5], r1[:, :], d[:, :], ALU.mult), ["r1", "d", ("R", p)], [("R", p)])
            s.dma("sync", R[rs, :], Rt[:, :], reads=[("R", p)], is_output=True)
        s.finish()
    return nc


def build_ln2(M):
    nc = bass.Bass("TRN2", target_bir_lowering=False)
    din = lambda n, shp: nc.dram_tensor(n, shp, F32, kind="ExternalInput").ap()
    dr = dict(x1=din("x1", [M, D]), o1=din("o1", [M, D]), o2=din("o2", [M, D]),
              ln_g=din("ln_g", [128, D]), ln_b=din("ln_b", [128, D]))
    y = nc.dram_tensor("y", [M, D], F32, kind="ExternalOutput").ap()
    with ExitStack() as st:
        s = Sched(nc, st)
        ctx = dict(nc=nc, s=s, st=st, dr=dr)
        S = {}
        ln_consts(ctx, S, "ln_g", "ln_b")
        rr = Rot(ctx, "r", [128, D], F32, 2)
        aa = Rot(ctx, "a", [128, D], F32, 2)
        for blk in range(M // 128):
            rs = slice(blk * 128, (blk + 1) * 128)
            r, rk = rr.next()
            s.dma("sync", r[:, :], dr["x1"][rs, :], writes=[rk])
            a, ak = aa.next()
            s.dma("sync", a[:, :], dr["o1"][rs, :], writes=[ak])
            s.op("vector", lambda e, r=r, a=a: e.scalar_tensor_tensor(r[:, :], r[:, :], ALPHA, a[:, :], ALU.mult, ALU.add),
                 reads=[rk, ak], writes=[rk])
            a, ak = aa.next()
            s.dma("sync", a[:, :], dr["o2"][rs, :], writes=[ak])
            s.op("vector", lambda e, r=r, a=a: e.tensor_tensor(r[:, :], r[:, :], a[:, :], ALU.add), reads=[rk, ak], writes=[rk])
            ln_rows(ctx, S, r, rk)
            s.dma("sync", y[rs, :], r[:, :], reads=[rk], is_output=True)
        s.finish()
    return nc


def epi_ple():
    S = {}

    def consts(ctx):
        nc, s, dr = ctx["nc"], ctx["s"], ctx["dr"]
        S["o"] = Rot(ctx, "ost", [128, 512], F32, 3)
        S["x"] = Rot(ctx, "xst", [128, 512], F32, 3)
        S["b"] = ctx["st"].enter_context(nc.sbuf_tensor("biasb", [128, ctx["N"]], F32))
        s.dma("sync", S["b"][:, :], dr["bias"][:, :], writes=["biasb"])

    def epi(ctx, ps, keys, b, row0, n0, nbw):
        s, dr = ctx["s"], ctx["dr"]
        o, ok = S["o"].next()
        x, xk = S["x"].next()
        s.dma("sync", x[:, 0:nbw], dr["xres"][row0:row0 + 128, n0:n0 + nbw], writes=[xk])
        s.op("vector", lambda e: e.tensor_tensor(o[:, 0:nbw], ps[0][:, 0:nbw], S["b"][:, n0:n0 + nbw], ALU.add),
             reads=[keys[0], "biasb"], writes=[ok])
        s.op("scalar", lambda e: e.activation(out=o[:, 0:nbw], in_=o[:, 0:nbw], func=AF.Sigmoid), reads=[ok], writes=[ok])
        s.op("vector", lambda e: e.tensor_tensor(o[:, 0:nbw], o[:, 0:nbw], ps[1][:, 0:nbw], ALU.mult),
             reads=[ok, keys[1]], writes=[ok])
        s.op("vector", lambda e: e.tensor_tensor(o[:, 0:nbw], o[:, 0:nbw], x[:, 0:nbw], ALU.add), reads=[ok, xk], writes=[ok])
        s.dma("sync", dr["y"][b, row0:row0 + 128, n0:n0 + nbw], o[:, 0:nbw], reads=[ok], is_output=True)

    return consts, epi


def _bc(v, n=128):
    v = np.asarray(v, np.float32).reshape(1, -1)
    return np.ascontiguousarray(np.broadcast_to(v, (n, v.shape[1])))


def kernel(x, p, w_in, b_fox_f, hgrn_lb, hgrn_norm_g, w_branch_a, w_branch_b, w_out, ln1_g, ln1_b,
           w_group_router, b_group_router, w_expert_router, b_expert_router, w_exp_gate, w_exp_up,
           w_exp_down, ln2_g, ln2_b, w_ple_gate, b_ple_gate, w_ple_proj):
    x = np.asarray(x, np.float32)
    T = x.shape[1]
    TC = T // NCORES
    X = x[0]
    XT = np.ascontiguousarray(X.T)
    W = np.asarray(w_in[0], np.float32)
    FW = FOXH * HD
    o_q, o_k, o_v, o_f = 0, FW, 2 * FW, 3 * FW
    o_hq = 3 * FW + FOXH
    o_hf, o_hi, o_hg = o_hq + FW, o_hq + 2 * FW, o_hq + 3 * FW
    o_ga = o_hq + 4 * FW
    o_gb = o_ga + D
    cols = []
    for c in range(NCORES):
        hs = [2 * c, 2 * c + 1]
        cc = []
        for base in (o_q, o_k, o_v):
            for h in hs:
                cc.append(np.arange(base + h * HD, base + (h + 1) * HD))
        for base in (o_hq, o_hf, o_hi, o_hg):
            for h in hs:
                cc.append(np.arange(base + h * HD, base + (h + 1) * HD))
        cc.append(np.array([o_f + hs[0], o_f + hs[1]]))
        cols.append(np.concatenate(cc))
    NCOL = len(cols[0])
    c_, e_ = epi_simple()
    nc = build_gemm(T, NCOL, [dict(xt="xt", w="w", K=D)], e_, consts=c_)
    res = _run(nc, [{"xt": XT[None], "w": _c(W[:, cols[c]])[None]} for c in range(NCORES)])
    U_ = [r["y"][0] for r in res]
    del res
    tr = lambda a: np.ascontiguousarray(a.transpose(0, 2, 1))
    nc = build_attn(T, 2)
    ims = []
    for c in range(NCORES):
        u = U_[c]
        q = np.stack([u[:, 0:128], u[:, 128:256]]); k = np.stack([u[:, 256:384], u[:, 384:512]])
        v = np.stack([u[:, 512:640], u[:, 640:768]])
        fa = np.stack([u[:, 1792], u[:, 1793]])
        ims.append({"qT": tr(q), "kT": tr(k), "v": _c(v),
                    "fac": np.ascontiguousarray(fa.reshape(2, T // 128, 128).transpose(0, 2, 1)),
                    "bfb": _c(np.broadcast_to(np.asarray(b_fox_f[0], np.float32)[2 * c:2 * c + 2, None, None], (2, 128, 1)))})
    res = _run(nc, ims)
    oaT = np.concatenate([r["oT"].reshape(256, T) for r in res], 0)
    nc = build_hgrn(T, 2)
    ims = []
    lbv = np.asarray(hgrn_lb, np.float32)
    gnv = np.asarray(hgrn_norm_g[0], np.float32)
    for c in range(NCORES):
        u = U_[c]
        hq = np.stack([u[:, 768:896], u[:, 896:1024]]); hf = np.stack([u[:, 1024:1152], u[:, 1152:1280]])
        hi = np.stack([u[:, 1280:1408], u[:, 1408:1536]]); hg = np.stack([u[:, 1536:1664], u[:, 1664:1792]])
        a0 = lbv[0].reshape(16, 128)[2 * c:2 * c + 2]; a1 = lbv[1].reshape(16, 128)[2 * c:2 * c + 2]
        gn = gnv.reshape(16, 128)[2 * c:2 * c + 2]
        ims.append({"fb_tm": _c(hf), "ib_tm": _c(hi), "fbT": tr(hf), "qT": tr(hq), "gbT": tr(hg),
                    "a0r": _c(np.broadcast_to(a0[:, None, :], (2, 128, 128))), "a1r": _c(np.broadcast_to(a1[:, None, :], (2, 128, 128))),
                    "a0c": _c(a0[:, :, None]), "a1c": _c(a1[:, :, None]), "gnc": _c(gn[:, :, None])})
    res = _run(nc, ims)
    obT = np.concatenate([r["obT"].reshape(256, T) for r in res], 0)
    del U_, ims
    c_, e_ = epi_merge()
    nc = build_gemm(TC, D, [dict(xt="xt", w="wga", K=D), dict(xt="xt", w="wgb", K=D),
                            dict(xt="at", w="wa", K=FW), dict(xt="bt", w="wb", K=FW)], e_, consts=c_, NB=256)
    wga = _c(W[:, o_ga:o_ga + D])[None]; wgb = _c(W[:, o_gb:o_gb + D])[None]
    wa = _c(w_branch_a[0])[None]; wb = _c(w_branch_b[0])[None]
    sl = lambda c: slice(c * TC, (c + 1) * TC)
    res = _run(nc, [{"xt": _c(XT[:, sl(c)])[None], "at": _c(oaT[:, sl(c)])[None], "bt": _c(obT[:, sl(c)])[None],
                     "wga": wga, "wgb": wgb, "wa": wa, "wb": wb} for c in range(NCORES)])
    Y = np.concatenate([r["y"][0] for r in res], 0)
    del wga, wgb, W
    c_, e_, rf_ = epi_res_ln()
    nc = build_gemm(TC, D, [dict(xt="xt", w="w", K=D)], e_, extra=[("xres", [TC, D]), ("ln_g", [128, D]), ("ln_b", [128, D])],
                    consts=c_, row_final=rf_, SBo=128)
    YT = np.ascontiguousarray(Y.T)
    res = _run(nc, [{"xt": _c(YT[:, sl(c)])[None], "w": _c(w_out[0])[None], "xres": _c(X[sl(c)]),
                     "ln_g": _bc(ln1_g[0]), "ln_b": _bc(ln1_b[0])} for c in range(NCORES)])
    X1 = np.concatenate([r["y"][0] for r in res], 0)
    X1T = np.ascontiguousarray(X1.T)
    wr = _c(np.concatenate([w_group_router[0], w_expert_router[0]], 1))[None]
    br = _bc(np.concatenate([b_group_router[0], b_expert_router[0]]))
    c_, e_ = epi_simple(bias="bias")
    nc = build_gemm(TC, 72, [dict(xt="xt", w="w", K=D)], e_, extra=[("bias", [128, 72])], consts=c_)
    res = _run(nc, [{"xt": _c(X1T[:, sl(c)])[None], "w": wr, "bias": br} for c in range(NCORES)])
    nc = build_route(TC)
    res = _run(nc, [{"lg": _c(res[c]["y"][0])} for c in range(NCORES)])
    R = np.concatenate([r["R"] for r in res], 0)
    gsel = np.rint(R[:, 0]).astype(np.int64)
    eid = np.stack([gsel * EPG + np.rint(R[:, 1]).astype(np.int64), gsel * EPG + np.rint(R[:, 2]).astype(np.int64)], 1)
    gate = R[:, 3:5]
    flat_e = eid.reshape(-1)
    order = np.argsort(flat_e, kind="stable")
    counts = np.bincount(flat_e, minlength=NE)
    starts = np.cumsum(counts) - counts
    pos = np.empty(2 * T, np.int64)
    pos[order] = np.arange(2 * T) - starts[flat_e[order]]
    ME = int(max(512, -(-counts.max() // 512) * 512))
    tok_tab = np.full((NE, ME), -1, np.int64)
    tok_tab[flat_e, pos] = np.repeat(np.arange(T), 2)
    gw_tab = np.zeros((NE, ME, 1), np.float32)
    gw_tab[flat_e, pos, 0] = gate.reshape(-1)
    X1p = np.concatenate([X1, np.zeros((1, D), np.float32)], 0)
    c_, e_ = epi_glu()
    nc = build_gemm(ME, DE, [dict(xt="xt", w="wg", K=D), dict(xt="xt", w="wu", K=D)], e_, consts=c_, B=EPG)
    ims = []
    for c in range(NCORES):
        es = slice(c * EPG, (c + 1) * EPG)
        xg = X1p[tok_tab[es]]
        ims.append({"xt": np.ascontiguousarray(xg.transpose(0, 2, 1)), "wg": _c(w_exp_gate[0, es]), "wu": _c(w_exp_up[0, es])})
    res = _run(nc, ims)
    H = [r["y"] for r in res]
    del ims
    c_, e_ = epi_simple(rowscale="rs")
    nc = build_gemm(ME, D, [dict(xt="xt", w="w", K=DE)], e_, extra=[("rs", [EPG, ME, 1])], consts=c_, B=EPG)
    res = _run(nc, [{"xt": np.ascontiguousarray(H[c].transpose(0, 2, 1)), "w": _c(w_exp_down[0, c * EPG:(c + 1) * EPG]),
                     "rs": gw_tab[c * EPG:(c + 1) * EPG]} for c in range(NCORES)])
    O = np.concatenate([r["y"] for r in res], 0)
    pos2 = pos.reshape(T, 2)
    O1 = O[eid[:, 0], pos2[:, 0]]
    O2 = O[eid[:, 1], pos2[:, 1]]
    del O, H
    nc = build_ln2(TC)
    res = _run(nc, [{"x1": _c(X1[sl(c)]), "o1": _c(O1[sl(c)]), "o2": _c(O2[sl(c)]),
                     "ln_g": _bc(ln2_g[0]), "ln_b": _bc(ln2_b[0])} for c in range(NCORES)])
    X2 = np.concatenate([r["y"] for r in res], 0)
    X2T = np.ascontiguousarray(X2.T)
    PT = np.ascontiguousarray(np.asarray(p[0, 0], np.float32).T)
    c_, e_ = epi_ple()
    nc = build_gemm(TC, D, [dict(xt="xt", w="wpg", K=D), dict(xt="pt", w="wpe", K=PLE)], e_,
                    extra=[("xres", [TC, D]), ("bias", [128, D])], consts=c_)
    res = _run(nc, [{"xt": _c(X2T[:, sl(c)])[None], "pt": _c(PT[:, sl(c)])[None], "wpg": _c(w_ple_gate[0])[None],
                     "wpe": _c(w_ple_proj[0])[None], "xres": _c(X2[sl(c)]), "bias": _bc(b_ple_gate[0])} for c in range(NCORES)])
    out = np.concatenate([r["y"][0] for r in res], 0)
    return out[None].astype(np.float32)
```

```python
import numpy as np
from contextlib import ExitStack
import concourse.bass as bass
import concourse.mybir as mybir
from concourse.bass_utils import run_bass_kernel_spmd

F32 = mybir.dt.float32
BF16 = mybir.dt.bfloat16
U32 = mybir.dt.uint32
AF = mybir.ActivationFunctionType
ALU = mybir.AluOpType
AX = mybir.AxisListType

NCORES = 8
D = 4096
FOXH = 16
HD = 128
NG = 8
EPG = 8
NE = 64
DE = 512
PLE = 256
ALPHA = 2.0 ** 0.25
LN_EPS = 1e-5
RMS_EPS = 1e-6
import os
SELF_WAIT = os.environ.get('SELF_WAIT', '1') == '1'


class Sched:
    COMPUTE = ("tensor", "vector", "scalar", "gpsimd")
    LIMIT = 30000

    def __init__(self, nc, stack, ndma=8, nrot=3):
        self.nc = nc
        self.eng = {"tensor": nc.tensor, "vector": nc.vector, "scalar": nc.scalar,
                    "gpsimd": nc.gpsimd, "sync": nc.sync}
        self.csem = {e: [stack.enter_context(nc.semaphore(f"c_{e}_{k}")) for k in range(nrot)]
                     for e in self.COMPUTE}
        self.ccount = {e: 0 for e in self.COMPUTE}
        self.dsem = {q: [stack.enter_context(nc.semaphore(f"d_{q}_{k}")) for k in range(ndma)]
                     for q in ("sync", "gpsimd")}
        self.dcount = {q: [0] * ndma for q in ("sync", "gpsimd")}
        self.dnext = {q: 0 for q in ("sync", "gpsimd")}
        self.known = {e: {} for e in self.eng}
        self.kord = {e: {c: -1 for c in self.COMPUTE} for e in self.eng}
        self.last_w = {}
        self.readers = {}
        self.out_events = []

    def _wait(self, e, ev):
        if ev[0] == "c":
            _, src, n = ev
            if src == e and (e == "tensor" or not SELF_WAIT):
                return
            if self.kord[e][src] >= n:
                return
            k, v = divmod(n, self.LIMIT)
            self.eng[e].wait_ge(self.csem[src][k], v + 1)
            self.kord[e][src] = n
        else:
            _, sem, val, key = ev
            if self.known[e].get(key, 0) >= val:
                return
            self.eng[e].wait_ge(sem, val)
            self.known[e][key] = val

    def _deps(self, reads, writes):
        evs = []
        for b in reads:
            if b in self.last_w:
                evs.append(self.last_w[b])
        for b in writes:
            if b in self.last_w:
                evs.append(self.last_w[b])
            r = self.readers.get(b)
            if r:
                evs.extend(r["c"].values())
                evs.extend(r["d"])
        return evs

    def _record(self, ev, reads, writes):
        for b in writes:
            self.last_w[b] = ev
            self.readers[b] = {"c": {}, "d": []}
        for b in reads:
            r = self.readers.setdefault(b, {"c": {}, "d": []})
            if ev[0] == "c":
                r["c"][ev[1]] = ev
            else:
                r["d"].append(ev)

    def op(self, e, fn, reads=(), writes=(), signal=True):
        self.nops = getattr(self, "nops", 0) + 1
        import os
        if self.nops > int(os.environ.get("DBG_LIMIT", "100000000")):
            return None
        for ev in self._deps(reads, writes):
            self._wait(e, ev)
        ins = fn(self.eng[e])
        if not signal:
            return None
        n = self.ccount[e]
        self.ccount[e] += 1
        k, _ = divmod(n, self.LIMIT)
        ins.then_inc(self.csem[e][k], 1)
        ev = ("c", e, n)
        self._record(ev, reads, writes)
        return ev

    def dma(self, q, out, in_, reads=(), writes=(), is_output=False):
        slot = self.dnext[q]
        self.dnext[q] = (slot + 1) % len(self.dsem[q])
        sem = self.dsem[q][slot]
        key = (q, slot)
        if self.dcount[q][slot] > 0:
            self._wait(q, ("d", sem, self.dcount[q][slot], key))
        for ev in self._deps(reads, writes):
            self._wait(q, ev)
        self.eng[q].dma_start(out=out, in_=in_).then_inc(sem, 16)
        self.dcount[q][slot] += 16
        ev = ("d", sem, self.dcount[q][slot], key)
        self._record(ev, reads, writes)
        if is_output:
            self.out_events.append(ev)
        return ev

    def finish(self):
        for c in self.COMPUTE:
            if self.ccount[c] > 0:
                self._wait("sync", ("c", c, self.ccount[c] - 1))
        for ev in self.out_events:
            self._wait("sync", ev)
        for q in self.dsem:
            for slot, sem in enumerate(self.dsem[q]):
                if self.dcount[q][slot] > 0:
                    self._wait("sync", ("d", sem, self.dcount[q][slot], (q, slot)))


def _run(nc, in_maps):
    res = run_bass_kernel_spmd(nc, in_maps, core_ids=list(range(NCORES)))
    return res.results


def _c(a):
    return np.ascontiguousarray(a, dtype=np.float32)


def build_gemm(M, N, groups, epilogue, extra=(), out_cols=None, B=1, NB=512, row_final=None,
               consts=None, outs=None, SBo=None, w_resident=False, xt_bufs=1):
    nc = bass.Bass("TRN2", target_bir_lowering=False)
    out_cols = N if out_cols is None else out_cols
    dr = {}
    for g in groups:
        if g["xt"] not in dr:
            dr[g["xt"]] = nc.dram_tensor(g["xt"], [B, g["K"], M], F32, kind="ExternalInput").ap()
        dr[g["w"]] = nc.dram_tensor(g["w"], [B, g["K"], N], F32, kind="ExternalInput").ap()
    for name, shape in extra:
        dr[name] = nc.dram_tensor(name, list(shape), F32, kind="ExternalInput").ap()
    outs = outs or [("y", [B, M, out_cols])]
    for name, shape in outs:
        dr[name] = nc.dram_tensor(name, list(shape), F32, kind="ExternalOutput").ap()
    if SBo is not None:
        SB = SBo
    elif M <= 1024 and M % 128 == 0:
        SB = M
    else:
        SB = 512 if M % 512 == 0 else 128
    nsub = SB // 128
    with ExitStack() as st:
        s = Sched(nc, st)
        xt_t = {}
        for g in groups:
            if g["xt"] not in xt_t:
                KC = g["K"] // 128
                xt_t[g["xt"]] = [st.enter_context(nc.sbuf_tensor(f"xt_{g['xt']}_{i}", [128, KC, SB], BF16))
                                 for i in range(xt_bufs)]
        w_t = {}
        for g in groups:
            KC = g["K"] // 128
            if w_resident:
                assert B == 1
                w_t[g["w"]] = st.enter_context(nc.sbuf_tensor(f"w_{g['w']}", [128, KC, N], BF16))
            else:
                w_t[g["w"]] = [st.enter_context(nc.sbuf_tensor(f"w_{g['w']}_{i}", [128, KC, NB], BF16))
                               for i in range(2)]
        ps = [[st.enter_context(nc.psum_tensor(f"ps_{gi}_{i}", [128, 512], F32)) for i in range(2)]
              for gi in range(len(groups))]
        ctx = dict(nc=nc, s=s, st=st, dr=dr, SB=SB, nsub=nsub, NB=NB, M=M, N=N)
        if consts is not None:
            consts(ctx)
        it = 0
        nblk = (N + NB - 1) // NB
        if w_resident:
            for g in groups:
                for nb in range(nblk):
                    n0 = nb * NB
                    nbw = min(NB, N - n0)
                    s.dma("gpsimd", w_t[g["w"]][:, :, n0:n0 + nbw],
                          dr[g["w"]][0, :, n0:n0 + nbw].rearrange("(kc p) n -> p kc n", p=128),
                          writes=[("w", g["w"], nb)])
        sbi = 0
        for b in range(B):
            for sb in range(M // SB):
                r0 = sb * SB
                xp = sbi % xt_bufs
                sbi += 1
                for name, t in xt_t.items():
                    s.dma("gpsimd", t[xp][:, :, :],
                          dr[name][b, :, r0:r0 + SB].rearrange("(kc p) m -> p kc m", p=128),
                          writes=[("xt", name, xp)])
                for nb in range(nblk):
                    n0 = nb * NB
                    nbw = min(NB, N - n0)
                    par = it % 2
                    it += 1
                    if not w_resident:
                        for g in groups:
                            s.dma("gpsimd", w_t[g["w"]][par][:, :, 0:nbw],
                                  dr[g["w"]][b, :, n0:n0 + nbw].rearrange("(kc p) n -> p kc n", p=128),
                                  writes=[("w", g["w"], par)])
                    for sub in range(nsub):
                        pp = (it * nsub + sub) % 2
                        for gi, g in enumerate(groups):
                            KC = g["K"] // 128
                            if w_resident:
                                wt, wk, wo = w_t[g["w"]], ("w", g["w"], nb), n0
                            else:
                                wt, wk, wo = w_t[g["w"]][par], ("w", g["w"], par), 0
                            for kc in range(KC):
                                s.op("tensor",
                                     lambda e, gi=gi, g=g, kc=kc, pp=pp, sub=sub, KC=KC, nbw=nbw, wt=wt, wo=wo, xp=xp:
                                     e.matmul(ps[gi][pp][:, 0:nbw],
                                              xt_t[g["xt"]][xp][:, kc, sub * 128:(sub + 1) * 128],
                                              wt[:, kc, wo:wo + nbw],
                                              start=(kc == 0), stop=(kc == KC - 1)),
                                     reads=[("xt", g["xt"], xp), wk],
                                     writes=[("ps", gi, pp)], signal=(kc == KC - 1))
                        epilogue(ctx, [ps[gi][pp] for gi in range(len(groups))],
                                 [("ps", gi, pp) for gi in range(len(groups))],
                                 b, r0 + sub * 128, n0, nbw)
                if row_final is not None:
                    row_final(ctx, b, r0)
        s.finish()
    return nc


class Rot:
    def __init__(self, ctx, name, shape, dtype, n=2):
        self.t = [ctx["st"].enter_context(ctx["nc"].sbuf_tensor(f"{name}_{i}", list(shape), dtype))
                  for i in range(n)]
        self.name = name
        self.i = 0

    def next(self):
        k = self.i % len(self.t)
        self.i += 1
        return self.t[k], (self.name, k)


def epi_simple(func=None, bias=None, rowscale=None, out="y"):
    S = {}

    def consts(ctx):
        nc, s, dr = ctx["nc"], ctx["s"], ctx["dr"]
        S["o"] = Rot(ctx, "ost", [128, 512], F32, 3)
        if bias:
            S["b"] = ctx["st"].enter_context(nc.sbuf_tensor("biasb", [128, ctx["N"]], F32))
            s.dma("sync", S["b"][:, :], dr[bias][:, :], writes=["biasb"])
        if rowscale:
            S["rs"] = Rot(ctx, "rs", [128, 1], F32, 2)

    def epi(ctx, ps, keys, b, row0, n0, nbw):
        s, dr = ctx["s"], ctx["dr"]
        o, ok = S["o"].next()
        if bias:
            s.op("vector", lambda e: e.tensor_tensor(o[:, 0:nbw], ps[0][:, 0:nbw], S["b"][:, n0:n0 + nbw], ALU.add),
                 reads=[keys[0], "biasb"], writes=[ok])
            if func is not None:
                s.op("scalar", lambda e: e.activation(out=o[:, 0:nbw], in_=o[:, 0:nbw], func=func),
                     reads=[ok], writes=[ok])
        else:
            s.op("scalar", lambda e: e.activation(out=o[:, 0:nbw], in_=ps[0][:, 0:nbw],
                                                  func=(func if func is not None else AF.Copy)),
                 reads=[keys[0]], writes=[ok])
        if rowscale:
            rs, rk = S["rs"].next()
            s.dma("sync", rs[:, :], dr[rowscale][b, row0:row0 + 128, :], writes=[rk])
            s.op("vector", lambda e: e.tensor_scalar(o[:, 0:nbw], o[:, 0:nbw], rs[:, 0:1], None, ALU.mult),
                 reads=[ok, rk], writes=[ok])
        s.dma("sync", dr[out][b, row0:row0 + 128, n0:n0 + nbw], o[:, 0:nbw], reads=[ok], is_output=True)

    return consts, epi


def epi_glu():
    S = {}

    def consts(ctx):
        S["o"] = Rot(ctx, "ost", [128, 512], F32, 3)

    def epi(ctx, ps, keys, b, row0, n0, nbw):
        s, dr = ctx["s"], ctx["dr"]
        o, ok = S["o"].next()
        s.op("scalar", lambda e: e.activation(out=o[:, 0:nbw], in_=ps[0][:, 0:nbw], func=AF.Silu),
             reads=[keys[0]], writes=[ok])
        s.op("vector", lambda e: e.tensor_tensor(o[:, 0:nbw], o[:, 0:nbw], ps[1][:, 0:nbw], ALU.mult),
             reads=[ok, keys[1]], writes=[ok])
        s.dma("sync", dr["y"][b, row0:row0 + 128, n0:n0 + nbw], o[:, 0:nbw], reads=[ok], is_output=True)

    return consts, epi


def epi_merge():
    S = {}

    def consts(ctx):
        S["o"] = Rot(ctx, "ost", [128, 512], F32, 3)
        S["t"] = Rot(ctx, "tst", [128, 512], F32, 2)

    def epi(ctx, ps, keys, b, row0, n0, nbw):
        s, dr = ctx["s"], ctx["dr"]
        o, ok = S["o"].next()
        t, tk = S["t"].next()
        s.op("scalar", lambda e: e.activation(out=o[:, 0:nbw], in_=ps[0][:, 0:nbw], func=AF.Sigmoid),
             reads=[keys[0]], writes=[ok])
        s.op("scalar", lambda e: e.activation(out=t[:, 0:nbw], in_=ps[1][:, 0:nbw], func=AF.Sigmoid),
             reads=[keys[1]], writes=[tk])
        s.op("vector", lambda e: e.tensor_tensor(o[:, 0:nbw], o[:, 0:nbw], ps[2][:, 0:nbw], ALU.mult),
             reads=[ok, keys[2]], writes=[ok])
        s.op("vector", lambda e: e.tensor_tensor(t[:, 0:nbw], t[:, 0:nbw], ps[3][:, 0:nbw], ALU.mult),
             reads=[tk, keys[3]], writes=[tk])
        s.op("vector", lambda e: e.tensor_tensor(o[:, 0:nbw], o[:, 0:nbw], t[:, 0:nbw], ALU.add),
             reads=[ok, tk], writes=[ok])
        s.dma("sync", dr["y"][b, row0:row0 + 128, n0:n0 + nbw], o[:, 0:nbw], reads=[ok], is_output=True)

    return consts, epi


def ln_rows(ctx, S, r, rk, gk="lng", bk="lnb"):
    s = ctx["s"]
    st1, k1 = S["st"].next()
    s.op("vector", lambda e: e.reduce_sum(st1[:, 0:1], r[:, :], AX.X), reads=[rk], writes=[k1])
    s.op("vector", lambda e: e.tensor_scalar(st1[:, 0:1], st1[:, 0:1], -1.0 / D, None, ALU.mult),
         reads=[k1], writes=[k1])
    s.op("vector", lambda e: e.tensor_scalar(r[:, :], r[:, :], st1[:, 0:1], None, ALU.add),
         reads=[rk, k1], writes=[rk])
    sq, sk = S["sq"].next()
    s.op("scalar", lambda e: e.activation(out=sq[:, :], in_=r[:, :], func=AF.Square, accum_out=st1[:, 1:2]),
         reads=[rk], writes=[sk, k1])
    s.op("vector", lambda e: e.tensor_scalar(st1[:, 1:2], st1[:, 1:2], 1.0 / D, LN_EPS, ALU.mult, ALU.add),
         reads=[k1], writes=[k1])
    s.op("scalar", lambda e: e.activation(out=st1[:, 1:2], in_=st1[:, 1:2], func=AF.Sqrt),
         reads=[k1], writes=[k1])
    s.op("vector", lambda e: e.reciprocal(st1[:, 1:2], st1[:, 1:2]), reads=[k1], writes=[k1])
    s.op("vector", lambda e: e.scalar_tensor_tensor(r[:, :], r[:, :], st1[:, 1:2], S["g"][:, :], ALU.mult, ALU.mult),
         reads=[rk, k1, gk], writes=[rk])
    s.op("vector", lambda e: e.tensor_tensor(r[:, :], r[:, :], S["b"][:, :], ALU.add),
         reads=[rk, bk], writes=[rk])


def ln_consts(ctx, S, gname, bname):
    nc, s, dr, st = ctx["nc"], ctx["s"], ctx["dr"], ctx["st"]
    S["g"] = st.enter_context(nc.sbuf_tensor("lng", [128, D], F32))
    S["b"] = st.enter_context(nc.sbuf_tensor("lnb", [128, D], F32))
    s.dma("sync", S["g"][:, :], dr[gname][:, :], writes=["lng"])
    s.dma("sync", S["b"][:, :], dr[bname][:, :], writes=["lnb"])
    S["st"] = Rot(ctx, "lnst", [128, 2], F32, 2)
    S["sq"] = Rot(ctx, "lnsq", [128, D], BF16, 1)


def epi_res_ln():
    S = {}

    def consts(ctx):
        ln_consts(ctx, S, "ln_g", "ln_b")
        S["r"] = Rot(ctx, "rrow", [128, D], F32, max(2, ctx["nsub"]))
        S["cur"] = {}

    def epi(ctx, ps, keys, b, row0, n0, nbw):
        s, dr = ctx["s"], ctx["dr"]
        if n0 == 0 and row0 not in S["cur"]:
            pass
        key = row0
        if key not in S["cur"]:
            r, rk = S["r"].next()
            S["cur"][key] = (r, rk)
            s.dma("sync", r[:, :], dr["xres"][row0:row0 + 128, :], writes=[rk])
        r, rk = S["cur"][key]
        s.op("vector", lambda e: e.scalar_tensor_tensor(r[:, n0:n0 + nbw], r[:, n0:n0 + nbw], ALPHA,
                                                        ps[0][:, 0:nbw], ALU.mult, ALU.add),
             reads=[rk, keys[0]], writes=[rk])

    def row_final(ctx, b, r0):
        s, dr = ctx["s"], ctx["dr"]
        for sub in range(ctx["nsub"]):
            row0 = r0 + sub * 128
            r, rk = S["cur"].pop(row0)
            ln_rows(ctx, S, r, rk)
            s.dma("sync", dr["y"][b, row0:row0 + 128, :], r[:, :], reads=[rk], is_output=True)

    return consts, epi, row_final


def _mk_consts(nc, s, st):
    C = {}
    C["J"] = st.enter_context(nc.sbuf_tensor("cJ", [128, 512], F32))
    C["U"] = st.enter_context(nc.sbuf_tensor("cU", [128, 128], F32))
    C["onesf"] = st.enter_context(nc.sbuf_tensor("cOf", [128, 128], F32))
    C["onesb"] = st.enter_context(nc.sbuf_tensor("cOb", [128, 128], BF16))
    C["sel"] = st.enter_context(nc.sbuf_tensor("cSel", [128, 128], F32))
    s.op("gpsimd", lambda e: e.iota(C["J"][:, :], [[1, 512]], base=0, channel_multiplier=-1,
                                    allow_small_or_imprecise_dtypes=True), writes=["cJ"])
    s.op("vector", lambda e: e.tensor_single_scalar(C["U"][:, :], C["J"][:, 0:128], 0.0, ALU.is_ge),
         reads=["cJ"], writes=["cU"])
    s.op("vector", lambda e: e.memset(C["onesf"][:, :], 1.0), writes=["cOf"])
    s.op("vector", lambda e: e.memset(C["onesb"][:, :], 1.0), writes=["cOb"])
    s.op("gpsimd", lambda e: e.iota(C["sel"][:, :], [[0, 128]], base=0, channel_multiplier=1,
                                    allow_small_or_imprecise_dtypes=True), writes=["cSel"])
    s.op("vector", lambda e: e.tensor_single_scalar(C["sel"][:, :], C["sel"][:, :], 127.0, ALU.is_equal),
         reads=["cSel"], writes=["cSel"])
    C["Ub"] = st.enter_context(nc.sbuf_tensor("cUb", [128, 128], BF16))
    C["selb"] = st.enter_context(nc.sbuf_tensor("cSelb", [128, 128], BF16))
    s.op("vector", lambda e: e.tensor_copy(C["Ub"][:, :], C["U"][:, :]), reads=["cU"], writes=["cUb"])
    s.op("vector", lambda e: e.tensor_copy(C["selb"][:, :], C["sel"][:, :]), reads=["cSel"], writes=["cSelb"])
    return C


def split_bf16(s, src, skey, parts, tmp, name, n):
    keys = []
    cur, ck = src, skey
    for i in range(n):
        k = (name, i)
        s.op("vector", lambda e, i=i, cur=cur: e.tensor_copy(parts[i], cur), reads=[ck], writes=[k])
        keys.append(k)
        if i + 1 < n:
            tk = (name, "r")
            s.op("vector", lambda e, i=i, cur=cur: e.tensor_tensor(tmp, cur, parts[i], ALU.subtract),
                 reads=[ck, k], writes=[tk])
            cur, ck = tmp, tk
    return keys


def mm_split(s, out, okey, lhs_parts, lkeys, rhs_parts, rkeys):
    pairs = [(a, b, ka, kb) for a, ka in zip(lhs_parts, lkeys) for b, kb in zip(rhs_parts, rkeys)]
    for idx, (a, b, ka, kb) in enumerate(pairs):
        s.op("tensor", lambda e, a=a, b=b, idx=idx: e.matmul(out, a, b, start=(idx == 0), stop=(idx == len(pairs) - 1)),
             reads=[ka, kb], writes=[okey], signal=(idx == len(pairs) - 1))


def build_attn(T, NH=2):
    nc = bass.Bass("TRN2", target_bir_lowering=False)
    NBk = T // 128
    NQ = T // 512
    qT = nc.dram_tensor("qT", [NH, 128, T], F32, kind="ExternalInput").ap()
    kT = nc.dram_tensor("kT", [NH, 128, T], F32, kind="ExternalInput").ap()
    v = nc.dram_tensor("v", [NH, T, 128], F32, kind="ExternalInput").ap()
    fac = nc.dram_tensor("fac", [NH, 128, NBk], F32, kind="ExternalInput").ap()
    bfb = nc.dram_tensor("bfb", [NH, 128, 1], F32, kind="ExternalInput").ap()
    oT = nc.dram_tensor("oT", [NH, 128, T], F32, kind="ExternalOutput").ap()
    scale = float(HD) ** -0.5
    with ExitStack() as st:
        s = Sched(nc, st)
        C = _mk_consts(nc, s, st)
        sb = lambda n, shp, dt=F32: st.enter_context(nc.sbuf_tensor(n, shp, dt))
        masks = [sb(f"mask{i}", [128, 512], BF16) for i in range(4)]
        for i in range(4):
            s.op("vector", lambda e, i=i: e.tensor_single_scalar(masks[i][:, :], C["J"][:, :], 128.0 * i, ALU.is_ge),
                 reads=["cJ"], writes=[("mask", i)])
        q_sb = sb("q_sb", [128, T], BF16)
        k_sb = sb("k_sb", [128, T], BF16)
        v_sb = sb("v_sb", [128, NBk, 128], BF16)
        z = sb("z", [128, NBk]); a = sb("a", [128, NBk]); ls = sb("ls", [128, NBk])
        tot = sb("tot", [128, NBk]); incl = sb("incl", [128, NBk]); ccol = sb("ccol", [128, NBk])
        lsp = [sb(f"lsp{i}", [128, NBk], BF16) for i in range(3)]; lst = sb("lst", [128, NBk])
        cm = sb("cm", [128, NBk]); bfs = sb("bfs", [128, 1]); biast = [sb(f"biast{i}", [128, NBk]) for i in range(2)]
        Pb = [sb(f"P{i}", [128, 512], BF16) for i in range(3)]
        rec = [sb(f"rec{i}", [128, 512]) for i in range(2)]
        osb = [sb(f"osb{i}", [128, 512]) for i in range(2)]
        psS = [st.enter_context(nc.psum_tensor(f"psS{i}", [128, 512], F32)) for i in range(2)]
        psO = [st.enter_context(nc.psum_tensor(f"psO{i}", [128, 512], F32)) for i in range(2)]
        psD = [st.enter_context(nc.psum_tensor(f"psD{i}", [128, 512], F32)) for i in range(2)]
        psM = st.enter_context(nc.psum_tensor("psM", [128, 512], F32))
        gq = 0
        for h in range(NH):
            s.dma("sync", z[:, :], fac[h], writes=["z"])
            s.dma("sync", bfs[:, :], bfb[h], writes=["bfs"])
            s.op("vector", lambda e: e.tensor_scalar(z[:, :], z[:, :], bfs[:, 0:1], None, ALU.add),
                 reads=["z", "bfs"], writes=["z"])
            s.op("vector", lambda e: e.tensor_scalar(a[:, :], z[:, :], -1.0, None, ALU.mult), reads=["z"], writes=["a"])
            s.op("vector", lambda e: e.tensor_tensor(a[:, :], a[:, :], z[:, :], ALU.max), reads=["z", "a"], writes=["a"])
            s.op("scalar", lambda e: e.activation(out=a[:, :], in_=a[:, :], func=AF.Exp, scale=-1.0), reads=["a"], writes=["a"])
            s.op("scalar", lambda e: e.activation(out=a[:, :], in_=a[:, :], func=AF.Ln, bias=1.0), reads=["a"], writes=["a"])
            s.op("vector", lambda e: e.tensor_scalar_min(ls[:, :], z[:, :], 0.0), reads=["z"], writes=["ls"])
            s.op("vector", lambda e: e.tensor_tensor(ls[:, :], ls[:, :], a[:, :], ALU.subtract), reads=["ls", "a"], writes=["ls"])
            lk = split_bf16(s, ls[:, :], "ls", [x[:, :] for x in lsp], lst[:, :], "lsp", 3)
            mm_split(s, psM[:, 0:NBk], "psM0", [C["Ub"][:, :]], ["cUb"], [x[:, :] for x in lsp], lk)
            mm_split(s, psM[:, NBk:2 * NBk], "psM1", [C["onesb"][:, :]], ["cOb"], [x[:, :] for x in lsp], lk)
            s.op("vector", lambda e: e.tensor_copy(tot[:, :], psM[:, NBk:2 * NBk]), reads=["psM1"], writes=["tot"])
            s.op("vector", lambda e: e.tensor_tensor_scan(incl[:, :], C["onesf"][:, 0:NBk], tot[:, :], 0.0, ALU.mult, ALU.add),
                 reads=["tot", "cOf"], writes=["incl"])
            s.op("vector", lambda e: e.tensor_tensor(incl[:, :], incl[:, :], tot[:, :], ALU.subtract), reads=["incl", "tot"], writes=["incl"])
            s.op("vector", lambda e: e.tensor_tensor(ccol[:, :], incl[:, :], psM[:, 0:NBk], ALU.add), reads=["incl", "psM0"], writes=["ccol"])
            ck_ = split_bf16(s, ccol[:, :], "ccol", [x[:, :] for x in lsp], lst[:, :], "lsp", 3)
            mm_split(s, psM[:, 2 * NBk:3 * NBk], "psM2", [C["selb"][:, :]], ["cSelb"], [x[:, :] for x in lsp], ck_)
            s.op("vector", lambda e: e.tensor_copy(cm[:, :], psM[:, 2 * NBk:3 * NBk]), reads=["psM2"], writes=["cm"])
            for j in range(4):
                sl = slice(j * T // 4, (j + 1) * T // 4)
                s.dma("gpsimd", q_sb[:, sl], qT[h, :, sl], writes=[("q", j)])
                s.dma("gpsimd", k_sb[:, sl], kT[h, :, sl], writes=[("k", j)])
                bs = slice(j * NBk // 4, (j + 1) * NBk // 4)
                s.dma("gpsimd", v_sb[:, bs, :], v[h].rearrange("(b p) d -> p b d", p=128)[:, bs, :], writes=[("v", j)])
            allq = [("q", j) for j in range(4)]; allk = [("k", j) for j in range(4)]; allv = [("v", j) for j in range(4)]
            for qb in range(NQ):
                nkb = 4 * (qb + 1)
                par = gq % 2
                gq += 1
                bt = biast[par]
                s.op("vector", lambda e, bt=bt, qb=qb, nkb=nkb: e.tensor_scalar(
                    bt[:, 0:nkb], ccol[:, 0:nkb], -1.0, cm[:, 4 * qb + 1:4 * qb + 2], ALU.mult, ALU.add),
                    reads=["ccol", "cm"], writes=[("bt", par)])
                qs = slice(qb * 512, (qb + 1) * 512)

                def mmS(kb):
                    s.op("tensor", lambda e: e.matmul(psS[kb % 2][:, :], k_sb[:, kb * 128:(kb + 1) * 128], q_sb[:, qs],
                                                      start=True, stop=True),
                         reads=allq + allk, writes=[("S", kb % 2)])
                mmS(0)
                for kb in range(nkb):
                    if kb + 1 < nkb:
                        mmS(kb + 1)
                    P = Pb[kb % 3]
                    pk = ("P", kb % 3)
                    s.op("scalar", lambda e, P=P, kb=kb, bt=bt: e.activation(
                        out=P[:, :], in_=psS[kb % 2][:, :], func=AF.Exp, bias=bt[:, kb:kb + 1], scale=scale),
                        reads=[("S", kb % 2), ("bt", par)], writes=[pk])
                    di = kb - 4 * qb
                    if di >= 0:
                        s.op("vector", lambda e, P=P, di=di: e.tensor_tensor(P[:, :], P[:, :], masks[di][:, :], ALU.mult),
                             reads=[pk, ("mask", di)], writes=[pk])
                    s.op("tensor", lambda e, P=P, kb=kb: e.matmul(psO[par][:, :], v_sb[:, kb, :], P[:, :],
                                                                  start=(kb == 0), stop=(kb == nkb - 1)),
                         reads=allv + [pk], writes=[("O", par)], signal=False)
                    s.op("tensor", lambda e, P=P, kb=kb: e.matmul(psD[par][:, :], C["onesb"][:, :], P[:, :],
                                                                  start=(kb == 0), stop=(kb == nkb - 1)),
                         reads=["cOb", pk], writes=[("O", par), ("Dn", par)])
                s.op("vector", lambda e: e.reciprocal(rec[par][:, :], psD[par][:, :]), reads=[("Dn", par)], writes=[("rec", par)])
                s.op("vector", lambda e: e.tensor_tensor(osb[par][:, :], psO[par][:, :], rec[par][:, :], ALU.mult),
                     reads=[("O", par), ("rec", par)], writes=[("osb", par)])
                s.dma("sync", oT[h, :, qs], osb[par][:, :], reads=[("osb", par)], is_output=True)
        s.finish()
    return nc


def build_hgrn(T, NH=2):
    nc = bass.Bass("TRN2", target_bir_lowering=False)
    NCH = T // 128
    SC = 4 if NCH % 4 == 0 else 1
    din = lambda n, shp: nc.dram_tensor(n, shp, F32, kind="ExternalInput").ap()
    fb_tm = din("fb_tm", [NH, T, 128]); ib_tm = din("ib_tm", [NH, T, 128])
    fbT = din("fbT", [NH, 128, T]); qT = din("qT", [NH, 128, T]); gbT = din("gbT", [NH, 128, T])
    a0r = din("a0r", [NH, 128, 128]); a1r = din("a1r", [NH, 128, 128])
    a0c = din("a0c", [NH, 128, 1]); a1c = din("a1c", [NH, 128, 1]); gnc = din("gnc", [NH, 128, 1])
    obT = nc.dram_tensor("obT", [NH, 128, T], F32, kind="ExternalOutput").ap()
    scale = float(HD) ** -0.5
    assert NH * 4 <= 8
    with ExitStack() as st:
        s = Sched(nc, st)
        C = _mk_consts(nc, s, st)
        U = C["U"]
        cnt = [0]

        def sb(shp, dt=F32):
            cnt[0] += 1
            return st.enter_context(nc.sbuf_tensor(f"h{cnt[0]}", shp, dt))

        class NS:
            pass
        V = lambda f, r, w: s.op("vector", f, reads=r, writes=w)
        A = lambda f, r, w: s.op("scalar", f, reads=r, writes=w)
        PE = lambda f, r, w: s.op("tensor", f, reads=r, writes=w)
        H = []
        for h in range(NH):
            n = NS()
            n.h = h
            n.lbr = sb([128, 128]); n.omr = sb([128, 128]); n.lbc = sb([128, 1]); n.omc = sb([128, 1]); n.gn = sb([128, 1])
            n.t1 = sb([128, 128]); n.t1c = sb([128, 1])
            n.in_fb = [sb([128, SC, 128]) for _ in range(2)]; n.in_fbT = [sb([128, SC * 128]) for _ in range(2)]
            n.in_q = [sb([128, SC * 128]) for _ in range(2)]; n.in_g = [sb([128, SC * 128]) for _ in range(2)]
            n.in_v = [sb([128, SC, 128], BF16) for _ in range(2)]
            n.LFp = [sb([128, 128], BF16) for _ in range(3)]; n.LFt = sb([128, 128])
            n.SQp = [sb([128, 128], BF16) for _ in range(2)]; n.SQt = sb([128, 128])
            n.Ftm = sb([128, 128]); n.LF = sb([128, 128]); n.KKtm = sb([128, 128]); n.Ffm = sb([128, 128]); n.KKfm = sb([128, 128])
            n.bfm = sb([128, 128]); n.btm = sb([128, 128]); n.nbm = sb([128, 1])
            n.eQ = sb([128, 128]); n.eK = sb([128, 128]); n.eB = sb([128, 128])
            n.Qt = sb([128, 128], BF16); n.Kt = sb([128, 128], BF16); n.Qb = sb([128, 128], BF16)
            n.ATm = sb([128, 128], BF16); n.dif = sb([128, 128]); n.Kh = sb([128, 128], BF16)
            n.S = sb([128, 128]); n.Sbf = sb([128, 128], BF16); n.ebl = sb([128, 1])
            n.osb = sb([128, 128]); n.sq = sb([128, 128]); n.rstd = sb([128, 128]); n.sg = sb([128, 128])
            n.res = [sb([128, SC * 128]) for _ in range(2)]
            n.pA = st.enter_context(nc.psum_tensor(f"pA{h}", [128, 512], F32))
            n.pD = st.enter_context(nc.psum_tensor(f"pD{h}", [128, 512], F32))
            n.pB = st.enter_context(nc.psum_tensor(f"pB{h}", [128, 512], F32))
            n.pC = st.enter_context(nc.psum_tensor(f"pC{h}", [128, 512], F32))
            H.append(n)

        def head_setup(n):
            h = n.h
            k = lambda name: (name, h)
            s.dma("sync", n.lbr[:, :], a0r[h], writes=[k("lbr")]); s.dma("sync", n.t1[:, :], a1r[h], writes=[k("t1")])
            s.dma("sync", n.lbc[:, :], a0c[h], writes=[k("lbc")]); s.dma("sync", n.t1c[:, :], a1c[h], writes=[k("t1c")])
            s.dma("sync", n.gn[:, :], gnc[h], writes=[k("gn")])
            V(lambda e: e.tensor_tensor(n.lbr[:, :], n.lbr[:, :], n.t1[:, :], ALU.subtract), [k("lbr"), k("t1")], [k("lbr")])
            A(lambda e: e.activation(out=n.lbr[:, :], in_=n.lbr[:, :], func=AF.Sigmoid), [k("lbr")], [k("lbr")])
            V(lambda e: e.tensor_scalar(n.omr[:, :], n.lbr[:, :], -1.0, 1.0, ALU.mult, ALU.add), [k("lbr")], [k("omr")])
            V(lambda e: e.tensor_tensor(n.lbc[:, :], n.lbc[:, :], n.t1c[:, :], ALU.subtract), [k("lbc"), k("t1c")], [k("lbc")])
            A(lambda e: e.activation(out=n.lbc[:, :], in_=n.lbc[:, :], func=AF.Sigmoid), [k("lbc")], [k("lbc")])
            V(lambda e: e.tensor_scalar(n.omc[:, :], n.lbc[:, :], -1.0, 1.0, ALU.mult, ALU.add), [k("lbc")], [k("omc")])
            V(lambda e: e.memset(n.S[:, :], 0.0), [], [k("S")])
            V(lambda e: e.memset(n.Sbf[:, :], 0.0), [], [k("Sbf")])

        def loads(n, sc):
            h = n.h
            k = lambda name: (name, h)
            p = sc % 2
            ts = slice(sc * SC * 128, (sc + 1) * SC * 128)
            s.dma("sync", n.in_fb[p][:, :, :], fb_tm[h, ts, :].rearrange("(c p) d -> p c d", p=128), writes=[k(("ifb", p))])
            s.dma("sync", n.in_fbT[p][:, :], fbT[h, :, ts], writes=[k(("ifbT", p))])
            s.dma("sync", n.in_q[p][:, :], qT[h, :, ts], writes=[k(("iq", p))])
            s.dma("sync", n.in_g[p][:, :], gbT[h, :, ts], writes=[k(("ig", p))])
            s.dma("gpsimd", n.in_v[p][:, :, :], ib_tm[h, ts, :].rearrange("(c p) d -> p c d", p=128), writes=[k(("iv", p))])

        def chunk(n, c):
            ops = []
            h = n.h
            k = lambda name: (name, h)
            sc, ci = divmod(c, SC)
            p = sc % 2
            cs = slice(ci * 128, (ci + 1) * 128)
            V = lambda f, r, w: ops.append(lambda: s.op("vector", f, reads=r, writes=w))
            A = lambda f, r, w: ops.append(lambda: s.op("scalar", f, reads=r, writes=w))
            PE = lambda f, r, w: ops.append(lambda: s.op("tensor", f, reads=r, writes=w))
            fbt, fbTt, qt, gt, vt = n.in_fb[p][:, ci, :], n.in_fbT[p][:, cs], n.in_q[p][:, cs], n.in_g[p][:, cs], n.in_v[p][:, ci, :]
            kfb, kfbT, kq, kg, kv = k(("ifb", p)), k(("ifbT", p)), k(("iq", p)), k(("ig", p)), k(("iv", p))
            A(lambda e: e.activation(out=n.Ftm[:, :], in_=fbt, func=AF.Sigmoid), [kfb], [k("Ftm")])
            V(lambda e: e.tensor_tensor(n.Ftm[:, :], n.Ftm[:, :], n.omr[:, :], ALU.mult), [k("Ftm"), k("omr")], [k("Ftm")])
            V(lambda e: e.tensor_tensor(n.Ftm[:, :], n.Ftm[:, :], n.lbr[:, :], ALU.add), [k("Ftm"), k("lbr")], [k("Ftm")])
            A(lambda e: e.activation(out=n.LF[:, :], in_=n.Ftm[:, :], func=AF.Ln), [k("Ftm")], [k("LF")])
            V(lambda e: e.tensor_scalar(n.KKtm[:, :], n.Ftm[:, :], -1.0, 1.0, ALU.mult, ALU.add), [k("Ftm")], [k("KKtm")])
            A(lambda e: e.activation(out=n.Ffm[:, :], in_=fbTt, func=AF.Sigmoid), [kfbT], [k("Ffm")])
            V(lambda e: e.tensor_scalar(n.Ffm[:, :], n.Ffm[:, :], n.omc[:, 0:1], n.lbc[:, 0:1], ALU.mult, ALU.add),
              [k("Ffm"), k("omc"), k("lbc")], [k("Ffm")])
            V(lambda e: e.tensor_scalar(n.KKfm[:, :], n.Ffm[:, :], -1.0, 1.0, ALU.mult, ALU.add), [k("Ffm")], [k("KKfm")])
            lp = [x[:, :] for x in n.LFp]
            lk = [(k("LFp"), i) for i in range(3)]
            ops.append(lambda: split_bf16(s, n.LF[:, :], k("LF"), lp, n.LFt[:, :], k("LFp"), 3))
            ops.append(lambda: mm_split(s, n.pA[:, 0:128], k("pA0"), [C["Ub"][:, :]], ["cUb"], lp, lk))
            ops.append(lambda: mm_split(s, n.pA[:, 128:256], k("pA1"), lp, lk, [C["Ub"][:, :]], ["cUb"]))
            ops.append(lambda: mm_split(s, n.pD[:, 256:384], k("pA2"), [C["onesb"][:, :]], ["cOb"], lp, lk))
            A(lambda e: e.activation(out=n.bfm[:, :], in_=n.pA[:, 128:256], func=AF.Copy), [k("pA1")], [k("bfm")])
            A(lambda e: e.activation(out=n.btm[:, :], in_=n.pA[:, 0:128], func=AF.Copy), [k("pA0")], [k("btm")])
            V(lambda e: e.tensor_scalar(n.nbm[:, :], n.bfm[:, 63:64], -1.0, None, ALU.mult), [k("bfm")], [k("nbm")])
            A(lambda e: e.activation(out=n.eQ[:, :], in_=n.bfm[:, :], func=AF.Exp, bias=n.nbm[:, 0:1], scale=1.0), [k("bfm"), k("nbm")], [k("eQ")])
            A(lambda e: e.activation(out=n.eK[:, :], in_=n.bfm[:, :], func=AF.Exp, bias=n.bfm[:, 63:64], scale=-1.0), [k("bfm")], [k("eK")])
            A(lambda e: e.activation(out=n.eB[:, :], in_=n.bfm[:, :], func=AF.Exp), [k("bfm")], [k("eB")])
            A(lambda e: e.activation(out=n.ebl[:, :], in_=n.bfm[:, 127:128], func=AF.Exp), [k("bfm")], [k("ebl")])
            V(lambda e: e.scalar_tensor_tensor(n.Qt[:, :], qt, scale, n.eQ[:, :], ALU.mult, ALU.mult), [kq, k("eQ")], [k("Qt")])
            V(lambda e: e.tensor_tensor(n.Kt[:, :], n.KKfm[:, :], n.eK[:, :], ALU.mult), [k("KKfm"), k("eK")], [k("Kt")])
            V(lambda e: e.scalar_tensor_tensor(n.Qb[:, :], qt, scale, n.eB[:, :], ALU.mult, ALU.mult), [kq, k("eB")], [k("Qb")])
            PE(lambda e: e.matmul(n.pB[:, 0:128], n.Kt[:, :], n.Qt[:, :], start=True, stop=True), [k("Kt"), k("Qt")], [k("pB")])
            V(lambda e: e.tensor_tensor(n.ATm[:, :], n.pB[:, 0:128], U[:, :], ALU.mult), [k("pB"), "cU"], [k("ATm")])
            V(lambda e: e.tensor_tensor(n.dif[:, :], n.pD[:, 256:384], n.btm[:, :], ALU.subtract), [k("pA2"), k("btm")], [k("dif")])
            A(lambda e: e.activation(out=n.dif[:, :], in_=n.dif[:, :], func=AF.Exp), [k("dif")], [k("dif")])
            V(lambda e: e.tensor_tensor(n.Kh[:, :], n.KKtm[:, :], n.dif[:, :], ALU.mult), [k("KKtm"), k("dif")], [k("Kh")])
            ops.append(lambda: s.op("tensor", lambda e: e.matmul(n.pC[:, 0:128], vt, n.ATm[:, :], start=True, stop=False),
                                    reads=[kv, k("ATm")], writes=[k("pC")], signal=False))
            PE(lambda e: e.matmul(n.pC[:, 0:128], n.Sbf[:, :], n.Qb[:, :], start=False, stop=True),
               [k("Sbf"), k("Qb"), kv, k("ATm")], [k("pC")])
            PE(lambda e: e.matmul(n.pD[:, 0:128], n.Kh[:, :], vt, start=True, stop=True), [k("Kh"), kv], [k("pD")])
            V(lambda e: e.scalar_tensor_tensor(n.S[:, :], n.S[:, :], n.ebl[:, 0:1], n.pD[:, 0:128], ALU.mult, ALU.add),
              [k("S"), k("ebl"), k("pD")], [k("S")])
            A(lambda e: e.activation(out=n.Sbf[:, :], in_=n.S[:, :], func=AF.Copy), [k("S")], [k("Sbf")])
            A(lambda e: e.activation(out=n.osb[:, :], in_=n.pC[:, 0:128], func=AF.Copy), [k("pC")], [k("osb")])
            A(lambda e: e.activation(out=n.sq[:, :], in_=n.osb[:, :], func=AF.Square), [k("osb")], [k("sq")])
            qp = [x[:, :] for x in n.SQp]
            qk_ = [(k("SQp"), i) for i in range(2)]
            ops.append(lambda: split_bf16(s, n.sq[:, :], k("sq"), qp, n.SQt[:, :], k("SQp"), 2))
            ops.append(lambda: mm_split(s, n.pB[:, 256:384], k("pE"), [C["onesb"][:, :]], ["cOb"], qp, qk_))
            V(lambda e: e.tensor_scalar(n.rstd[:, :], n.pB[:, 256:384], 1.0 / HD, RMS_EPS, ALU.mult, ALU.add), [k("pE")], [k("rstd")])
            A(lambda e: e.activation(out=n.rstd[:, :], in_=n.rstd[:, :], func=AF.Sqrt), [k("rstd")], [k("rstd")])
            V(lambda e: e.reciprocal(n.rstd[:, :], n.rstd[:, :]), [k("rstd")], [k("rstd")])
            A(lambda e: e.activation(out=n.sg[:, :], in_=gt, func=AF.Silu), [kg], [k("sg")])
            r = n.res[p][:, cs]
            kr = k(("res", p, ci))
            V(lambda e: e.tensor_tensor(r, n.osb[:, :], n.rstd[:, :], ALU.mult), [k("osb"), k("rstd")], [kr])
            V(lambda e: e.scalar_tensor_tensor(r, r, n.gn[:, 0:1], n.sg[:, :], ALU.mult, ALU.mult), [kr, k("gn"), k("sg")], [kr])
            if ci == SC - 1:
                ts = slice(sc * SC * 128, (sc + 1) * SC * 128)
                ops.append(lambda: s.dma("sync", obT[h, :, ts], n.res[p][:, :], reads=[k(("res", p, j)) for j in range(SC)],
                                         is_output=True))
            return ops

        from itertools import zip_longest
        for n in H:
            head_setup(n)
            loads(n, 0)
        for c in range(NCH):
            sc, ci = divmod(c, SC)
            if ci == 0 and sc + 1 < NCH // SC:
                for n in H:
                    loads(n, sc + 1)
            for group in zip_longest(*[chunk(n, c) for n in H]):
                for th in group:
                    if th is not None:
                        th()
        s.finish()
    return nc


def build_route(M):
    nc = bass.Bass("TRN2", target_bir_lowering=False)
    lg = nc.dram_tensor("lg", [M, 72], F32, kind="ExternalInput").ap()
    R = nc.dram_tensor("R", [M, 8], F32, kind="ExternalOutput").ap()
    with ExitStack() as st:
        s = Sched(nc, st)
        cnt = [0]

        def sb(shp, dt=F32):
            cnt[0] += 1
            return st.enter_context(nc.sbuf_tensor(f"r{cnt[0]}", shp, dt))
        io8 = sb([128, 8])
        s.op("gpsimd", lambda e: e.iota(io8[:, :], [[1, 8]], base=0, channel_multiplier=0,
                                        allow_small_or_imprecise_dtypes=True), writes=["io8"])
        V = lambda f, r, w: s.op("vector", f, reads=r, writes=w)
        A = lambda f, r, w: s.op("scalar", f, reads=r, writes=w)
        Ls = [sb([128, 72]) for _ in range(2)]
        Ro = [sb([128, 8]) for _ in range(2)]
        G8 = sb([128, 8]); GI = sb([128, 8], U32); gs = sb([128, 1]); nb = sb([128, 1]); ex = sb([128, 8]); sm = sb([128, 1])
        oh = sb([128, 8]); EL = sb([128, 8]); E8 = sb([128, 8]); EI = sb([128, 8], U32); d = sb([128, 1]); r1 = sb([128, 1])
        for blk in range(M // 128):
            p = blk % 2
            L, Rt = Ls[p], Ro[p]
            rs = slice(blk * 128, (blk + 1) * 128)
            s.dma("sync", L[:, :], lg[rs, :], writes=[("L", p)])
            V(lambda e: e.memset(Rt[:, :], 0.0), [], [("R", p)])
            V(lambda e: e.max(G8[:, :], L[:, 0:8]), [("L", p)], ["G8"])
            V(lambda e: e.max_index(GI[:, :], G8[:, :], L[:, 0:8]), [("L", p), "G8"], ["GI"])
            V(lambda e: e.tensor_copy(gs[:, :], GI[:, 0:1]), ["GI"], ["gs"])
            V(lambda e: e.tensor_scalar(nb[:, :], G8[:, 0:1], -1.0, None, ALU.mult), ["G8"], ["nb"])
            A(lambda e: e.activation(out=ex[:, :], in_=L[:, 0:8], func=AF.Exp, bias=nb[:, 0:1], scale=1.0, accum_out=sm[:, 0:1]),
              [("L", p), "nb"], ["ex", "sm"])
            V(lambda e: e.reciprocal(sm[:, :], sm[:, :]), ["sm"], ["sm"])
            V(lambda e: e.tensor_scalar(oh[:, :], io8[:, :], gs[:, 0:1], None, ALU.is_equal), ["io8", "gs"], ["oh"])
            V(lambda e: e.tensor_scalar(EL[:, :], L[:, 8:16], oh[:, 0:1], None, ALU.mult), [("L", p), "oh"], ["EL"])
            for g in range(1, 8):
                V(lambda e, g=g: e.scalar_tensor_tensor(EL[:, :], L[:, 8 + 8 * g:16 + 8 * g], oh[:, g:g + 1], EL[:, :], ALU.mult, ALU.add),
                  [("L", p), "oh", "EL"], ["EL"])
            V(lambda e: e.max(E8[:, :], EL[:, :]), ["EL"], ["E8"])
            V(lambda e: e.max_index(EI[:, :], E8[:, :], EL[:, :]), ["EL", "E8"], ["EI"])
            V(lambda e: e.tensor_tensor(d[:, :], E8[:, 1:2], E8[:, 0:1], ALU.subtract), ["E8"], ["d"])
            A(lambda e: e.activation(out=d[:, :], in_=d[:, :], func=AF.Exp), ["d"], ["d"])
            V(lambda e: e.tensor_scalar(r1[:, :], d[:, :], 1.0, None, ALU.add), ["d"], ["r1"])
            V(lambda e: e.reciprocal(r1[:, :], r1[:, :]), ["r1"], ["r1"])
            V(lambda e: e.tensor_tensor(r1[:, :], r1[:, :], sm[:, :], ALU.mult), ["r1", "sm"], ["r1"])
            V(lambda e: e.tensor_copy(Rt[:, 0:1], gs[:, :]), ["gs", ("R", p)], [("R", p)])
            V(lambda e: e.tensor_copy(Rt[:, 1:3], EI[:, 0:2]), ["EI", ("R", p)], [("R", p)])
            V(lambda e: e.tensor_copy(Rt[:, 3:4], r1[:, :]), ["r1", ("R", p)], [("R", p)])
            V(lambda e: e.tensor_tensor(Rt[:, 4:5], r1[:, :], d[:, :], ALU.mult), ["r1", "d", ("R", p)], [("R", p)])
            s.dma("sync", R[rs, :], Rt[:, :], reads=[("R", p)], is_output=True)
        s.finish()
    return nc


def build_ln2(M):
    nc = bass.Bass("TRN2", target_bir_lowering=False)
    din = lambda n, shp: nc.dram_tensor(n, shp, F32, kind="ExternalInput").ap()
    dr = dict(x1=din("x1", [M, D]), o1=din("o1", [M, D]), o2=din("o2", [M, D]),
              ln_g=din("ln_g", [128, D]), ln_b=din("ln_b", [128, D]))
    y = nc.dram_tensor("y", [M, D], F32, kind="ExternalOutput").ap()
    with ExitStack() as st:
        s = Sched(nc, st)
        ctx = dict(nc=nc, s=s, st=st, dr=dr)
        S = {}
        ln_consts(ctx, S, "ln_g", "ln_b")
        rr = Rot(ctx, "r", [128, D], F32, 2)
        aa = Rot(ctx, "a", [128, D], F32, 2)
        for blk in range(M // 128):
            rs = slice(blk * 128, (blk + 1) * 128)
            r, rk = rr.next()
            s.dma("sync", r[:, :], dr["x1"][rs, :], writes=[rk])
            a, ak = aa.next()
            s.dma("sync", a[:, :], dr["o1"][rs, :], writes=[ak])
            s.op("vector", lambda e, r=r, a=a: e.scalar_tensor_tensor(r[:, :], r[:, :], ALPHA, a[:, :], ALU.mult, ALU.add),
                 reads=[rk, ak], writes=[rk])
            a, ak = aa.next()
            s.dma("sync", a[:, :], dr["o2"][rs, :], writes=[ak])
            s.op("vector", lambda e, r=r, a=a: e.tensor_tensor(r[:, :], r[:, :], a[:, :], ALU.add), reads=[rk, ak], writes=[rk])
            ln_rows(ctx, S, r, rk)
            s.dma("sync", y[rs, :], r[:, :], reads=[rk], is_output=True)
        s.finish()
    return nc


def epi_ple():
    S = {}

    def consts(ctx):
        nc, s, dr = ctx["nc"], ctx["s"], ctx["dr"]
        S["o"] = Rot(ctx, "ost", [128, 512], F32, 3)
        S["x"] = Rot(ctx, "xst", [128, 512], F32, 3)
        S["b"] = ctx["st"].enter_context(nc.sbuf_tensor("biasb", [128, ctx["N"]], F32))
        s.dma("sync", S["b"][:, :], dr["bias"][:, :], writes=["biasb"])

    def epi(ctx, ps, keys, b, row0, n0, nbw):
        s, dr = ctx["s"], ctx["dr"]
        o, ok = S["o"].next()
        x, xk = S["x"].next()
        s.dma("sync", x[:, 0:nbw], dr["xres"][row0:row0 + 128, n0:n0 + nbw], writes=[xk])
        s.op("vector", lambda e: e.tensor_tensor(o[:, 0:nbw], ps[0][:, 0:nbw], S["b"][:, n0:n0 + nbw], ALU.add),
             reads=[keys[0], "biasb"], writes=[ok])
        s.op("scalar", lambda e: e.activation(out=o[:, 0:nbw], in_=o[:, 0:nbw], func=AF.Sigmoid), reads=[ok], writes=[ok])
        s.op("vector", lambda e: e.tensor_tensor(o[:, 0:nbw], o[:, 0:nbw], ps[1][:, 0:nbw], ALU.mult),
             reads=[ok, keys[1]], writes=[ok])
        s.op("vector", lambda e: e.tensor_tensor(o[:, 0:nbw], o[:, 0:nbw], x[:, 0:nbw], ALU.add), reads=[ok, xk], writes=[ok])
        s.dma("sync", dr["y"][b, row0:row0 + 128, n0:n0 + nbw], o[:, 0:nbw], reads=[ok], is_output=True)

    return consts, epi


def _bc(v, n=128):
    v = np.asarray(v, np.float32).reshape(1, -1)
    return np.ascontiguousarray(np.broadcast_to(v, (n, v.shape[1])))


def kernel(x, p, w_in, b_fox_f, hgrn_lb, hgrn_norm_g, w_branch_a, w_branch_b, w_out, ln1_g, ln1_b,
           w_group_router, b_group_router, w_expert_router, b_expert_router, w_exp_gate, w_exp_up,
           w_exp_down, ln2_g, ln2_b, w_ple_gate, b_ple_gate, w_ple_proj):
    x = np.asarray(x, np.float32)
    T = x.shape[1]
    TC = T // NCORES
    X = x[0]
    XT = np.ascontiguousarray(X.T)
    W = np.asarray(w_in[0], np.float32)
    FW = FOXH * HD
    o_q, o_k, o_v, o_f = 0, FW, 2 * FW, 3 * FW
    o_hq = 3 * FW + FOXH
    o_hf, o_hi, o_hg = o_hq + FW, o_hq + 2 * FW, o_hq + 3 * FW
    o_ga = o_hq + 4 * FW
    o_gb = o_ga + D
    cols = []
    for c in range(NCORES):
        hs = [2 * c, 2 * c + 1]
        cc = []
        for base in (o_q, o_k, o_v):
            for h in hs:
                cc.append(np.arange(base + h * HD, base + (h + 1) * HD))
        for base in (o_hq, o_hf, o_hi, o_hg):
            for h in hs:
                cc.append(np.arange(base + h * HD, base + (h + 1) * HD))
        cc.append(np.array([o_f + hs[0], o_f + hs[1]]))
        cols.append(np.concatenate(cc))
    NCOL = len(cols[0])
    c_, e_ = epi_simple()
    nc = build_gemm(T, NCOL, [dict(xt="xt", w="w", K=D)], e_, consts=c_, w_resident=True, xt_bufs=2)
    res = _run(nc, [{"xt": XT[None], "w": _c(W[:, cols[c]])[None]} for c in range(NCORES)])
    U_ = [r["y"][0] for r in res]
    del res
    tr = lambda a: np.ascontiguousarray(a.transpose(0, 2, 1))
    nc = build_attn(T, 2)
    ims = []
    for c in range(NCORES):
        u = U_[c]
        q = np.stack([u[:, 0:128], u[:, 128:256]]); k = np.stack([u[:, 256:384], u[:, 384:512]])
        v = np.stack([u[:, 512:640], u[:, 640:768]])
        fa = np.stack([u[:, 1792], u[:, 1793]])
        ims.append({"qT": tr(q), "kT": tr(k), "v": _c(v),
                    "fac": np.ascontiguousarray(fa.reshape(2, T // 128, 128).transpose(0, 2, 1)),
                    "bfb": _c(np.broadcast_to(np.asarray(b_fox_f[0], np.float32)[2 * c:2 * c + 2, None, None], (2, 128, 1)))})
    res = _run(nc, ims)
    oaT = np.concatenate([r["oT"].reshape(256, T) for r in res], 0)
    nc = build_hgrn(T, 2)
    ims = []
    lbv = np.asarray(hgrn_lb, np.float32)
    gnv = np.asarray(hgrn_norm_g[0], np.float32)
    for c in range(NCORES):
        u = U_[c]
        hq = np.stack([u[:, 768:896], u[:, 896:1024]]); hf = np.stack([u[:, 1024:1152], u[:, 1152:1280]])
        hi = np.stack([u[:, 1280:1408], u[:, 1408:1536]]); hg = np.stack([u[:, 1536:1664], u[:, 1664:1792]])
        a0 = lbv[0].reshape(16, 128)[2 * c:2 * c + 2]; a1 = lbv[1].reshape(16, 128)[2 * c:2 * c + 2]
        gn = gnv.reshape(16, 128)[2 * c:2 * c + 2]
        ims.append({"fb_tm": _c(hf), "ib_tm": _c(hi), "fbT": tr(hf), "qT": tr(hq), "gbT": tr(hg),
                    "a0r": _c(np.broadcast_to(a0[:, None, :], (2, 128, 128))), "a1r": _c(np.broadcast_to(a1[:, None, :], (2, 128, 128))),
                    "a0c": _c(a0[:, :, None]), "a1c": _c(a1[:, :, None]), "gnc": _c(gn[:, :, None])})
    res = _run(nc, ims)
    obT = np.concatenate([r["obT"].reshape(256, T) for r in res], 0)
    del U_, ims
    c_, e_ = epi_merge()
    nc = build_gemm(TC, D, [dict(xt="xt", w="wga", K=D), dict(xt="xt", w="wgb", K=D),
                            dict(xt="at", w="wa", K=FW), dict(xt="bt", w="wb", K=FW)], e_, consts=c_, NB=256)
    wga = _c(W[:, o_ga:o_ga + D])[None]; wgb = _c(W[:, o_gb:o_gb + D])[None]
    wa = _c(w_branch_a[0])[None]; wb = _c(w_branch_b[0])[None]
    sl = lambda c: slice(c * TC, (c + 1) * TC)
    res = _run(nc, [{"xt": _c(XT[:, sl(c)])[None], "at": _c(oaT[:, sl(c)])[None], "bt": _c(obT[:, sl(c)])[None],
                     "wga": wga, "wgb": wgb, "wa": wa, "wb": wb} for c in range(NCORES)])
    Y = np.concatenate([r["y"][0] for r in res], 0)
    del wga, wgb, W
    c_, e_, rf_ = epi_res_ln()
    nc = build_gemm(TC, D, [dict(xt="xt", w="w", K=D)], e_, extra=[("xres", [TC, D]), ("ln_g", [128, D]), ("ln_b", [128, D])],
                    consts=c_, row_final=rf_, NB=256)
    YT = np.ascontiguousarray(Y.T)
    res = _run(nc, [{"xt": _c(YT[:, sl(c)])[None], "w": _c(w_out[0])[None], "xres": _c(X[sl(c)]),
                     "ln_g": _bc(ln1_g[0]), "ln_b": _bc(ln1_b[0])} for c in range(NCORES)])
    X1 = np.concatenate([r["y"][0] for r in res], 0)
    X1T = np.ascontiguousarray(X1.T)
    wr = _c(np.concatenate([w_group_router[0], w_expert_router[0]], 1))[None]
    br = _bc(np.concatenate([b_group_router[0], b_expert_router[0]]))
    c_, e_ = epi_simple(bias="bias")
    nc = build_gemm(TC, 72, [dict(xt="xt", w="w", K=D)], e_, extra=[("bias", [128, 72])], consts=c_)
    res = _run(nc, [{"xt": _c(X1T[:, sl(c)])[None], "w": wr, "bias": br} for c in range(NCORES)])
    nc = build_route(TC)
    res = _run(nc, [{"lg": _c(res[c]["y"][0])} for c in range(NCORES)])
    R = np.concatenate([r["R"] for r in res], 0)
    gsel = np.rint(R[:, 0]).astype(np.int64)
    eid = np.stack([gsel * EPG + np.rint(R[:, 1]).astype(np.int64), gsel * EPG + np.rint(R[:, 2]).astype(np.int64)], 1)
    gate = R[:, 3:5]
    flat_e = eid.reshape(-1)
    order = np.argsort(flat_e, kind="stable")
    counts = np.bincount(flat_e, minlength=NE)
    starts = np.cumsum(counts) - counts
    pos = np.empty(2 * T, np.int64)
    pos[order] = np.arange(2 * T) - starts[flat_e[order]]
    ME = int(max(128, -(-counts.max() // 128) * 128))
    tok_tab = np.full((NE, ME), -1, np.int64)
    tok_tab[flat_e, pos] = np.repeat(np.arange(T), 2)
    gw_tab = np.zeros((NE, ME, 1), np.float32)
    gw_tab[flat_e, pos, 0] = gate.reshape(-1)
    X1p = np.concatenate([X1, np.zeros((1, D), np.float32)], 0)
    c_, e_ = epi_glu()
    nc = build_gemm(ME, DE, [dict(xt="xt", w="wg", K=D), dict(xt="xt", w="wu", K=D)], e_, consts=c_, B=EPG)
    ims = []
    for c in range(NCORES):
        es = slice(c * EPG, (c + 1) * EPG)
        xg = X1p[tok_tab[es]]
        ims.append({"xt": np.ascontiguousarray(xg.transpose(0, 2, 1)), "wg": _c(w_exp_gate[0, es]), "wu": _c(w_exp_up[0, es])})
    res = _run(nc, ims)
    H = [r["y"] for r in res]
    del ims
    c_, e_ = epi_simple(rowscale="rs")
    nc = build_gemm(ME, D, [dict(xt="xt", w="w", K=DE)], e_, extra=[("rs", [EPG, ME, 1])], consts=c_, B=EPG)
    res = _run(nc, [{"xt": np.ascontiguousarray(H[c].transpose(0, 2, 1)), "w": _c(w_exp_down[0, c * EPG:(c + 1) * EPG]),
                     "rs": gw_tab[c * EPG:(c + 1) * EPG]} for c in range(NCORES)])
    O = np.concatenate([r["y"] for r in res], 0)
    pos2 = pos.reshape(T, 2)
    O1 = O[eid[:, 0], pos2[:, 0]]
    O2 = O[eid[:, 1], pos2[:, 1]]
    del O, H
    nc = build_ln2(TC)
    res = _run(nc, [{"x1": _c(X1[sl(c)]), "o1": _c(O1[sl(c)]), "o2": _c(O2[sl(c)]),
                     "ln_g": _bc(ln2_g[0]), "ln_b": _bc(ln2_b[0])} for c in range(NCORES)])
    X2 = np.concatenate([r["y"] for r in res], 0)
    X2T = np.ascontiguousarray(X2.T)
    PT = np.ascontiguousarray(np.asarray(p[0, 0], np.float32).T)
    c_, e_ = epi_ple()
    nc = build_gemm(TC, D, [dict(xt="xt", w="wpg", K=D), dict(xt="pt", w="wpe", K=PLE)], e_,
                    extra=[("xres", [TC, D]), ("bias", [128, D])], consts=c_)
    res = _run(nc, [{"xt": _c(X2T[:, sl(c)])[None], "pt": _c(PT[:, sl(c)])[None], "wpg": _c(w_ple_gate[0])[None],
                     "wpe": _c(w_ple_proj[0])[None], "xres": _c(X2[sl(c)]), "bias": _bc(b_ple_gate[0])} for c in range(NCORES)])
    out = np.concatenate([r["y"][0] for r in res], 0)
    return out[None].astype(np.float32)
```

```python
import numpy as np
from contextlib import ExitStack
import concourse.bass as bass
import concourse.mybir as mybir
from concourse.bass_utils import run_bass_kernel_spmd

F32 = mybir.dt.float32
BF16 = mybir.dt.bfloat16
U32 = mybir.dt.uint32
AF = mybir.ActivationFunctionType
ALU = mybir.AluOpType
AX = mybir.AxisListType

NCORES = 8
D = 4096
FOXH = 16
HD = 128
NG = 8
EPG = 8
NE = 64
DE = 512
PLE = 256
ALPHA = 2.0 ** 0.25
LN_EPS = 1e-5
RMS_EPS = 1e-6
import os
SELF_WAIT = os.environ.get('SELF_WAIT', '1') == '1'


class Sched:
    COMPUTE = ("tensor", "vector", "scalar", "gpsimd")
    LIMIT = 30000

    def __init__(self, nc, stack, ndma=8, nrot=3):
        self.nc = nc
        self.eng = {"tensor": nc.tensor, "vector": nc.vector, "scalar": nc.scalar,
                    "gpsimd": nc.gpsimd, "sync": nc.sync}
        self.csem = {e: [stack.enter_context(nc.semaphore(f"c_{e}_{k}")) for k in range(nrot)]
                     for e in self.COMPUTE}
        self.ccount = {e: 0 for e in self.COMPUTE}
        self.dsem = {q: [stack.enter_context(nc.semaphore(f"d_{q}_{k}")) for k in range(ndma)]
                     for q in ("sync", "gpsimd")}
        self.dcount = {q: [0] * ndma for q in ("sync", "gpsimd")}
        self.dnext = {q: 0 for q in ("sync", "gpsimd")}
        self.known = {e: {} for e in self.eng}
        self.kord = {e: {c: -1 for c in self.COMPUTE} for e in self.eng}
        self.last_w = {}
        self.readers = {}
        self.out_events = []

    def _wait(self, e, ev):
        if ev[0] == "c":
            _, src, n = ev
            if src == e and (e == "tensor" or not SELF_WAIT):
                return
            if self.kord[e][src] >= n:
                return
            k, v = divmod(n, self.LIMIT)
            self.eng[e].wait_ge(self.csem[src][k], v + 1)
            self.kord[e][src] = n
        else:
            _, sem, val, key = ev
            if self.known[e].get(key, 0) >= val:
                return
            self.eng[e].wait_ge(sem, val)
            self.known[e][key] = val

    def _deps(self, reads, writes):
        evs = []
        for b in reads:
            if b in self.last_w:
                evs.append(self.last_w[b])
        for b in writes:
            if b in self.last_w:
                evs.append(self.last_w[b])
            r = self.readers.get(b)
            if r:
                evs.extend(r["c"].values())
                evs.extend(r["d"])
        return evs

    def _record(self, ev, reads, writes):
        for b in writes:
            self.last_w[b] = ev
            self.readers[b] = {"c": {}, "d": []}
        for b in reads:
            r = self.readers.setdefault(b, {"c": {}, "d": []})
            if ev[0] == "c":
                r["c"][ev[1]] = ev
            else:
                r["d"].append(ev)

    def op(self, e, fn, reads=(), writes=(), signal=True):
        self.nops = getattr(self, "nops", 0) + 1
        import os
        if self.nops > int(os.environ.get("DBG_LIMIT", "100000000")):
            return None
        for ev in self._deps(reads, writes):
            self._wait(e, ev)
        ins = fn(self.eng[e])
        if not signal:
            return None
        n = self.ccount[e]
        self.ccount[e] += 1
        k, _ = divmod(n, self.LIMIT)
        ins.then_inc(self.csem[e][k], 1)
        ev = ("c", e, n)
        self._record(ev, reads, writes)
        return ev

    def dma(self, q, out, in_, reads=(), writes=(), is_output=False):
        slot = self.dnext[q]
        self.dnext[q] = (slot + 1) % len(self.dsem[q])
        sem = self.dsem[q][slot]
        key = (q, slot)
        if self.dcount[q][slot] > 0:
            self._wait(q, ("d", sem, self.dcount[q][slot], key))
        for ev in self._deps(reads, writes):
            self._wait(q, ev)
        self.eng[q].dma_start(out=out, in_=in_).then_inc(sem, 16)
        self.dcount[q][slot] += 16
        ev = ("d", sem, self.dcount[q][slot], key)
        self._record(ev, reads, writes)
        if is_output:
            self.out_events.append(ev)
        return ev

    def finish(self):
        for c in self.COMPUTE:
            if self.ccount[c] > 0:
                self._wait("sync", ("c", c, self.ccount[c] - 1))
        for ev in self.out_events:
            self._wait("sync", ev)
        for q in self.dsem:
            for slot, sem in enumerate(self.dsem[q]):
                if self.dcount[q][slot] > 0:
                    self._wait("sync", ("d", sem, self.dcount[q][slot], (q, slot)))


def _run(nc, in_maps):
    res = run_bass_kernel_spmd(nc, in_maps, core_ids=list(range(NCORES)))
    return res.results


def _c(a):
    return np.ascontiguousarray(a, dtype=np.float32)


def build_gemm(M, N, groups, epilogue, extra=(), out_cols=None, B=1, NB=512, row_final=None,
               consts=None, outs=None, SBo=None, w_resident=False, xt_bufs=1):
    nc = bass.Bass("TRN2", target_bir_lowering=False)
    out_cols = N if out_cols is None else out_cols
    dr = {}
    for g in groups:
        if g["xt"] not in dr:
            dr[g["xt"]] = nc.dram_tensor(g["xt"], [B, g["K"], M], F32, kind="ExternalInput").ap()
        dr[g["w"]] = nc.dram_tensor(g["w"], [B, g["K"], N], F32, kind="ExternalInput").ap()
    for name, shape in extra:
        dr[name] = nc.dram_tensor(name, list(shape), F32, kind="ExternalInput").ap()
    outs = outs or [("y", [B, M, out_cols])]
    for name, shape in outs:
        dr[name] = nc.dram_tensor(name, list(shape), F32, kind="ExternalOutput").ap()
    if SBo is not None:
        SB = SBo
    elif M <= 1024 and M % 128 == 0:
        SB = M
    else:
        SB = 512 if M % 512 == 0 else 128
    nsub = SB // 128
    with ExitStack() as st:
        s = Sched(nc, st)
        xt_t = {}
        for g in groups:
            if g["xt"] not in xt_t:
                KC = g["K"] // 128
                xt_t[g["xt"]] = [st.enter_context(nc.sbuf_tensor(f"xt_{g['xt']}_{i}", [128, KC, SB], BF16))
                                 for i in range(xt_bufs)]
        w_t = {}
        for g in groups:
            KC = g["K"] // 128
            if w_resident:
                assert B == 1
                w_t[g["w"]] = st.enter_context(nc.sbuf_tensor(f"w_{g['w']}", [128, KC, N], BF16))
            else:
                w_t[g["w"]] = [st.enter_context(nc.sbuf_tensor(f"w_{g['w']}_{i}", [128, KC, NB], BF16))
                               for i in range(2)]
        ps = [[st.enter_context(nc.psum_tensor(f"ps_{gi}_{i}", [128, 512], F32)) for i in range(2)]
              for gi in range(len(groups))]
        ctx = dict(nc=nc, s=s, st=st, dr=dr, SB=SB, nsub=nsub, NB=NB, M=M, N=N)
        if consts is not None:
            consts(ctx)
        it = 0
        nblk = (N + NB - 1) // NB
        if w_resident:
            for g in groups:
                for nb in range(nblk):
                    n0 = nb * NB
                    nbw = min(NB, N - n0)
                    s.dma("gpsimd", w_t[g["w"]][:, :, n0:n0 + nbw],
                          dr[g["w"]][0, :, n0:n0 + nbw].rearrange("(kc p) n -> p kc n", p=128),
                          writes=[("w", g["w"], nb)])
        sbi = 0
        for b in range(B):
            for sb in range(M // SB):
                r0 = sb * SB
                xp = sbi % xt_bufs
                sbi += 1
                for name, t in xt_t.items():
                    s.dma("gpsimd", t[xp][:, :, :],
                          dr[name][b, :, r0:r0 + SB].rearrange("(kc p) m -> p kc m", p=128),
                          writes=[("xt", name, xp)])
                for nb in range(nblk):
                    n0 = nb * NB
                    nbw = min(NB, N - n0)
                    par = it % 2
                    it += 1
                    if not w_resident:
                        for g in groups:
                            s.dma("gpsimd", w_t[g["w"]][par][:, :, 0:nbw],
                                  dr[g["w"]][b, :, n0:n0 + nbw].rearrange("(kc p) n -> p kc n", p=128),
                                  writes=[("w", g["w"], par)])
                    for sub in range(nsub):
                        pp = (it * nsub + sub) % 2
                        for gi, g in enumerate(groups):
                            KC = g["K"] // 128
                            if w_resident:
                                wt, wk, wo = w_t[g["w"]], ("w", g["w"], nb), n0
                            else:
                                wt, wk, wo = w_t[g["w"]][par], ("w", g["w"], par), 0
                            for kc in range(KC):
                                s.op("tensor",
                                     lambda e, gi=gi, g=g, kc=kc, pp=pp, sub=sub, KC=KC, nbw=nbw, wt=wt, wo=wo, xp=xp:
                                     e.matmul(ps[gi][pp][:, 0:nbw],
                                              xt_t[g["xt"]][xp][:, kc, sub * 128:(sub + 1) * 128],
                                              wt[:, kc, wo:wo + nbw],
                                              start=(kc == 0), stop=(kc == KC - 1)),
                                     reads=[("xt", g["xt"], xp), wk],
                                     writes=[("ps", gi, pp)], signal=(kc == KC - 1))
                        epilogue(ctx, [ps[gi][pp] for gi in range(len(groups))],
                                 [("ps", gi, pp) for gi in range(len(groups))],
                                 b, r0 + sub * 128, n0, nbw)
                if row_final is not None:
                    row_final(ctx, b, r0)
        s.finish()
    return nc


class Rot:
    def __init__(self, ctx, name, shape, dtype, n=2):
        self.t = [ctx["st"].enter_context(ctx["nc"].sbuf_tensor(f"{name}_{i}", list(shape), dtype))
                  for i in range(n)]
        self.name = name
        self.i = 0

    def next(self):
        k = self.i % len(self.t)
        self.i += 1
        return self.t[k], (self.name, k)


def epi_simple(func=None, bias=None, rowscale=None, out="y"):
    S = {}

    def consts(ctx):
        nc, s, dr = ctx["nc"], ctx["s"], ctx["dr"]
        S["o"] = Rot(ctx, "ost", [128, 512], F32, 3)
        if bias:
            S["b"] = ctx["st"].enter_context(nc.sbuf_tensor("biasb", [128, ctx["N"]], F32))
            s.dma("sync", S["b"][:, :], dr[bias][:, :], writes=["biasb"])
        if rowscale:
            S["rs"] = Rot(ctx, "rs", [128, 1], F32, 2)

    def epi(ctx, ps, keys, b, row0, n0, nbw):
        s, dr = ctx["s"], ctx["dr"]
        o, ok = S["o"].next()
        if bias:
            s.op("vector", lambda e: e.tensor_tensor(o[:, 0:nbw], ps[0][:, 0:nbw], S["b"][:, n0:n0 + nbw], ALU.add),
                 reads=[keys[0], "biasb"], writes=[ok])
            if func is not None:
                s.op("scalar", lambda e: e.activation(out=o[:, 0:nbw], in_=o[:, 0:nbw], func=func),
                     reads=[ok], writes=[ok])
        else:
            s.op("scalar", lambda e: e.activation(out=o[:, 0:nbw], in_=ps[0][:, 0:nbw],
                                                  func=(func if func is not None else AF.Copy)),
                 reads=[keys[0]], writes=[ok])
        if rowscale:
            rs, rk = S["rs"].next()
            s.dma("sync", rs[:, :], dr[rowscale][b, row0:row0 + 128, :], writes=[rk])
            s.op("vector", lambda e: e.tensor_scalar(o[:, 0:nbw], o[:, 0:nbw], rs[:, 0:1], None, ALU.mult),
                 reads=[ok, rk], writes=[ok])
        s.dma("sync", dr[out][b, row0:row0 + 128, n0:n0 + nbw], o[:, 0:nbw], reads=[ok], is_output=True)

    return consts, epi


def epi_glu():
    S = {}

    def consts(ctx):
        S["o"] = Rot(ctx, "ost", [128, 512], F32, 3)

    def epi(ctx, ps, keys, b, row0, n0, nbw):
        s, dr = ctx["s"], ctx["dr"]
        o, ok = S["o"].next()
        s.op("scalar", lambda e: e.activation(out=o[:, 0:nbw], in_=ps[0][:, 0:nbw], func=AF.Silu),
             reads=[keys[0]], writes=[ok])
        s.op("vector", lambda e: e.tensor_tensor(o[:, 0:nbw], o[:, 0:nbw], ps[1][:, 0:nbw], ALU.mult),
             reads=[ok, keys[1]], writes=[ok])
        s.dma("sync", dr["y"][b, row0:row0 + 128, n0:n0 + nbw], o[:, 0:nbw], reads=[ok], is_output=True)

    return consts, epi


def epi_merge():
    S = {}

    def consts(ctx):
        S["o"] = Rot(ctx, "ost", [128, 512], F32, 3)
        S["t"] = Rot(ctx, "tst", [128, 512], F32, 2)

    def epi(ctx, ps, keys, b, row0, n0, nbw):
        s, dr = ctx["s"], ctx["dr"]
        o, ok = S["o"].next()
        t, tk = S["t"].next()
        s.op("scalar", lambda e: e.activation(out=o[:, 0:nbw], in_=ps[0][:, 0:nbw], func=AF.Sigmoid),
             reads=[keys[0]], writes=[ok])
        s.op("scalar", lambda e: e.activation(out=t[:, 0:nbw], in_=ps[1][:, 0:nbw], func=AF.Sigmoid),
             reads=[keys[1]], writes=[tk])
        s.op("vector", lambda e: e.tensor_tensor(o[:, 0:nbw], o[:, 0:nbw], ps[2][:, 0:nbw], ALU.mult),
             reads=[ok, keys[2]], writes=[ok])
        s.op("vector", lambda e: e.tensor_tensor(t[:, 0:nbw], t[:, 0:nbw], ps[3][:, 0:nbw], ALU.mult),
             reads=[tk, keys[3]], writes=[tk])
        s.op("vector", lambda e: e.tensor_tensor(o[:, 0:nbw], o[:, 0:nbw], t[:, 0:nbw], ALU.add),
             reads=[ok, tk], writes=[ok])
        s.dma("sync", dr["y"][b, row0:row0 + 128, n0:n0 + nbw], o[:, 0:nbw], reads=[ok], is_output=True)

    return consts, epi


def ln_rows(ctx, S, r, rk, gk="lng", bk="lnb"):
    s = ctx["s"]
    st1, k1 = S["st"].next()
    s.op("vector", lambda e: e.reduce_sum(st1[:, 0:1], r[:, :], AX.X), reads=[rk], writes=[k1])
    s.op("vector", lambda e: e.tensor_scalar(st1[:, 0:1], st1[:, 0:1], -1.0 / D, None, ALU.mult),
         reads=[k1], writes=[k1])
    s.op("vector", lambda e: e.tensor_scalar(r[:, :], r[:, :], st1[:, 0:1], None, ALU.add),
         reads=[rk, k1], writes=[rk])
    sq, sk = S["sq"].next()
    s.op("scalar", lambda e: e.activation(out=sq[:, :], in_=r[:, :], func=AF.Square, accum_out=st1[:, 1:2]),
         reads=[rk], writes=[sk, k1])
    s.op("vector", lambda e: e.tensor_scalar(st1[:, 1:2], st1[:, 1:2], 1.0 / D, LN_EPS, ALU.mult, ALU.add),
         reads=[k1], writes=[k1])
    s.op("scalar", lambda e: e.activation(out=st1[:, 1:2], in_=st1[:, 1:2], func=AF.Sqrt),
         reads=[k1], writes=[k1])
    s.op("vector", lambda e: e.reciprocal(st1[:, 1:2], st1[:, 1:2]), reads=[k1], writes=[k1])
    s.op("vector", lambda e: e.scalar_tensor_tensor(r[:, :], r[:, :], st1[:, 1:2], S["g"][:, :], ALU.mult, ALU.mult),
         reads=[rk, k1, gk], writes=[rk])
    s.op("vector", lambda e: e.tensor_tensor(r[:, :], r[:, :], S["b"][:, :], ALU.add),
         reads=[rk, bk], writes=[rk])


def ln_consts(ctx, S, gname, bname):
    nc, s, dr, st = ctx["nc"], ctx["s"], ctx["dr"], ctx["st"]
    S["g"] = st.enter_context(nc.sbuf_tensor("lng", [128, D], F32))
    S["b"] = st.enter_context(nc.sbuf_tensor("lnb", [128, D], F32))
    s.dma("sync", S["g"][:, :], dr[gname][:, :], writes=["lng"])
    s.dma("sync", S["b"][:, :], dr[bname][:, :], writes=["lnb"])
    S["st"] = Rot(ctx, "lnst", [128, 2], F32, 2)
    S["sq"] = Rot(ctx, "lnsq", [128, D], BF16, 1)


def epi_res_ln():
    S = {}

    def consts(ctx):
        ln_consts(ctx, S, "ln_g", "ln_b")
        S["r"] = Rot(ctx, "rrow", [128, D], F32, max(2, ctx["nsub"]))
        S["cur"] = {}

    def epi(ctx, ps, keys, b, row0, n0, nbw):
        s, dr = ctx["s"], ctx["dr"]
        if n0 == 0 and row0 not in S["cur"]:
            pass
        key = row0
        if key not in S["cur"]:
            r, rk = S["r"].next()
            S["cur"][key] = (r, rk)
            s.dma("sync", r[:, :], dr["xres"][row0:row0 + 128, :], writes=[rk])
        r, rk = S["cur"][key]
        s.op("vector", lambda e: e.scalar_tensor_tensor(r[:, n0:n0 + nbw], r[:, n0:n0 + nbw], ALPHA,
                                                        ps[0][:, 0:nbw], ALU.mult, ALU.add),
             reads=[rk, keys[0]], writes=[rk])

    def row_final(ctx, b, r0):
        s, dr = ctx["s"], ctx["dr"]
        for sub in range(ctx["nsub"]):
            row0 = r0 + sub * 128
            r, rk = S["cur"].pop(row0)
            ln_rows(ctx, S, r, rk)
            s.dma("sync", dr["y"][b, row0:row0 + 128, :], r[:, :], reads=[rk], is_output=True)

    return consts, epi, row_final


def _mk_consts(nc, s, st):
    C = {}
    C["J"] = st.enter_context(nc.sbuf_tensor("cJ", [128, 512], F32))
    C["U"] = st.enter_context(nc.sbuf_tensor("cU", [128, 128], F32))
    C["onesf"] = st.enter_context(nc.sbuf_tensor("cOf", [128, 128], F32))
    C["onesb"] = st.enter_context(nc.sbuf_tensor("cOb", [128, 128], BF16))
    C["sel"] = st.enter_context(nc.sbuf_tensor("cSel", [128, 128], F32))
    s.op("gpsimd", lambda e: e.iota(C["J"][:, :], [[1, 512]], base=0, channel_multiplier=-1,
                                    allow_small_or_imprecise_dtypes=True), writes=["cJ"])
    s.op("vector", lambda e: e.tensor_single_scalar(C["U"][:, :], C["J"][:, 0:128], 0.0, ALU.is_ge),
         reads=["cJ"], writes=["cU"])
    s.op("vector", lambda e: e.memset(C["onesf"][:, :], 1.0), writes=["cOf"])
    s.op("vector", lambda e: e.memset(C["onesb"][:, :], 1.0), writes=["cOb"])
    s.op("gpsimd", lambda e: e.iota(C["sel"][:, :], [[0, 128]], base=0, channel_multiplier=1,
                                    allow_small_or_imprecise_dtypes=True), writes=["cSel"])
    s.op("vector", lambda e: e.tensor_single_scalar(C["sel"][:, :], C["sel"][:, :], 127.0, ALU.is_equal),
         reads=["cSel"], writes=["cSel"])
    C["Ub"] = st.enter_context(nc.sbuf_tensor("cUb", [128, 128], BF16))
    C["selb"] = st.enter_context(nc.sbuf_tensor("cSelb", [128, 128], BF16))
    s.op("vector", lambda e: e.tensor_copy(C["Ub"][:, :], C["U"][:, :]), reads=["cU"], writes=["cUb"])
    s.op("vector", lambda e: e.tensor_copy(C["selb"][:, :], C["sel"][:, :]), reads=["cSel"], writes=["cSelb"])
    return C


def split_bf16(s, src, skey, parts, tmp, name, n):
    keys = []
    cur, ck = src, skey
    for i in range(n):
        k = (name, i)
        s.op("vector", lambda e, i=i, cur=cur: e.tensor_copy(parts[i], cur), reads=[ck], writes=[k])
        keys.append(k)
        if i + 1 < n:
            tk = (name, "r")
            s.op("vector", lambda e, i=i, cur=cur: e.tensor_tensor(tmp, cur, parts[i], ALU.subtract),
                 reads=[ck, k], writes=[tk])
            cur, ck = tmp, tk
    return keys


def mm_split(s, out, okey, lhs_parts, lkeys, rhs_parts, rkeys):
    pairs = [(a, b, ka, kb) for a, ka in zip(lhs_parts, lkeys) for b, kb in zip(rhs_parts, rkeys)]
    for idx, (a, b, ka, kb) in enumerate(pairs):
        s.op("tensor", lambda e, a=a, b=b, idx=idx: e.matmul(out, a, b, start=(idx == 0), stop=(idx == len(pairs) - 1)),
             reads=[ka, kb], writes=[okey], signal=(idx == len(pairs) - 1))


def build_attn(T, NH=2):
    nc = bass.Bass("TRN2", target_bir_lowering=False)
    NBk = T // 128
    NQ = T // 512
    qT = nc.dram_tensor("qT", [NH, 128, T], F32, kind="ExternalInput").ap()
    kT = nc.dram_tensor("kT", [NH, 128, T], F32, kind="ExternalInput").ap()
    v = nc.dram_tensor("v", [NH, T, 128], F32, kind="ExternalInput").ap()
    fac = nc.dram_tensor("fac", [NH, 128, NBk], F32, kind="ExternalInput").ap()
    bfb = nc.dram_tensor("bfb", [NH, 128, 1], F32, kind="ExternalInput").ap()
    oT = nc.dram_tensor("oT", [NH, 128, T], F32, kind="ExternalOutput").ap()
    scale = float(HD) ** -0.5
    with ExitStack() as st:
        s = Sched(nc, st)
        C = _mk_consts(nc, s, st)
        sb = lambda n, shp, dt=F32: st.enter_context(nc.sbuf_tensor(n, shp, dt))
        masks = [sb(f"mask{i}", [128, 512], BF16) for i in range(4)]
        for i in range(4):
            s.op("vector", lambda e, i=i: e.tensor_single_scalar(masks[i][:, :], C["J"][:, :], 128.0 * i, ALU.is_ge),
                 reads=["cJ"], writes=[("mask", i)])
        q_sb = sb("q_sb", [128, T], BF16)
        k_sb = sb("k_sb", [128, T], BF16)
        v_sb = sb("v_sb", [128, NBk, 128], BF16)
        z = sb("z", [128, NBk]); a = sb("a", [128, NBk]); ls = sb("ls", [128, NBk])
        tot = sb("tot", [128, NBk]); incl = sb("incl", [128, NBk]); ccol = sb("ccol", [128, NBk])
        lsp = [sb(f"lsp{i}", [128, NBk], BF16) for i in range(3)]; lst = sb("lst", [128, NBk])
        cm = sb("cm", [128, NBk]); bfs = sb("bfs", [128, 1]); biast = [sb(f"biast{i}", [128, NBk]) for i in range(2)]
        Pb = [sb(f"P{i}", [128, 512], BF16) for i in range(3)]
        rec = [sb(f"rec{i}", [128, 512]) for i in range(2)]
        osb = [sb(f"osb{i}", [128, 512]) for i in range(2)]
        psS = [st.enter_context(nc.psum_tensor(f"psS{i}", [128, 512], F32)) for i in range(3)]
        psO = [st.enter_context(nc.psum_tensor(f"psO{i}", [128, 512], F32)) for i in range(2)]
        psD = [st.enter_context(nc.psum_tensor(f"psD{i}", [128, 512], F32)) for i in range(2)]
        psM = st.enter_context(nc.psum_tensor("psM", [128, 512], F32))
        gq = 0
        for h in range(NH):
            s.dma("sync", z[:, :], fac[h], writes=["z"])
            s.dma("sync", bfs[:, :], bfb[h], writes=["bfs"])
            s.op("vector", lambda e: e.tensor_scalar(z[:, :], z[:, :], bfs[:, 0:1], None, ALU.add),
                 reads=["z", "bfs"], writes=["z"])
            s.op("vector", lambda e: e.tensor_scalar(a[:, :], z[:, :], -1.0, None, ALU.mult), reads=["z"], writes=["a"])
            s.op("vector", lambda e: e.tensor_tensor(a[:, :], a[:, :], z[:, :], ALU.max), reads=["z", "a"], writes=["a"])
            s.op("scalar", lambda e: e.activation(out=a[:, :], in_=a[:, :], func=AF.Exp, scale=-1.0), reads=["a"], writes=["a"])
            s.op("scalar", lambda e: e.activation(out=a[:, :], in_=a[:, :], func=AF.Ln, bias=1.0), reads=["a"], writes=["a"])
            s.op("vector", lambda e: e.tensor_scalar_min(ls[:, :], z[:, :], 0.0), reads=["z"], writes=["ls"])
            s.op("vector", lambda e: e.tensor_tensor(ls[:, :], ls[:, :], a[:, :], ALU.subtract), reads=["ls", "a"], writes=["ls"])
            lk = split_bf16(s, ls[:, :], "ls", [x[:, :] for x in lsp], lst[:, :], "lsp", 3)
            mm_split(s, psM[:, 0:NBk], "psM0", [C["Ub"][:, :]], ["cUb"], [x[:, :] for x in lsp], lk)
            mm_split(s, psM[:, NBk:2 * NBk], "psM1", [C["onesb"][:, :]], ["cOb"], [x[:, :] for x in lsp], lk)
            s.op("vector", lambda e: e.tensor_copy(tot[:, :], psM[:, NBk:2 * NBk]), reads=["psM1"], writes=["tot"])
            s.op("vector", lambda e: e.tensor_tensor_scan(incl[:, :], C["onesf"][:, 0:NBk], tot[:, :], 0.0, ALU.mult, ALU.add),
                 reads=["tot", "cOf"], writes=["incl"])
            s.op("vector", lambda e: e.tensor_tensor(incl[:, :], incl[:, :], tot[:, :], ALU.subtract), reads=["incl", "tot"], writes=["incl"])
            s.op("vector", lambda e: e.tensor_tensor(ccol[:, :], incl[:, :], psM[:, 0:NBk], ALU.add), reads=["incl", "psM0"], writes=["ccol"])
            ck_ = split_bf16(s, ccol[:, :], "ccol", [x[:, :] for x in lsp], lst[:, :], "lsp", 3)
            mm_split(s, psM[:, 2 * NBk:3 * NBk], "psM2", [C["selb"][:, :]], ["cSelb"], [x[:, :] for x in lsp], ck_)
            s.op("vector", lambda e: e.tensor_copy(cm[:, :], psM[:, 2 * NBk:3 * NBk]), reads=["psM2"], writes=["cm"])
            for j in range(4):
                sl = slice(j * T // 4, (j + 1) * T // 4)
                s.dma("gpsimd", q_sb[:, sl], qT[h, :, sl], writes=[("q", j)])
                s.dma("gpsimd", k_sb[:, sl], kT[h, :, sl], writes=[("k", j)])
                bs = slice(j * NBk // 4, (j + 1) * NBk // 4)
                s.dma("gpsimd", v_sb[:, bs, :], v[h].rearrange("(b p) d -> p b d", p=128)[:, bs, :], writes=[("v", j)])
            allq = [("q", j) for j in range(4)]; allk = [("k", j) for j in range(4)]; allv = [("v", j) for j in range(4)]
            for qb in range(NQ):
                nkb = 4 * (qb + 1)
                par = gq % 2
                gq += 1
                bt = biast[par]
                s.op("vector", lambda e, bt=bt, qb=qb, nkb=nkb: e.tensor_scalar(
                    bt[:, 0:nkb], ccol[:, 0:nkb], -1.0, cm[:, 4 * qb + 1:4 * qb + 2], ALU.mult, ALU.add),
                    reads=["ccol", "cm"], writes=[("bt", par)])
                qs = slice(qb * 512, (qb + 1) * 512)

                def mmS(kb):
                    s.op("tensor", lambda e: e.matmul(psS[kb % 3][:, :], k_sb[:, kb * 128:(kb + 1) * 128], q_sb[:, qs],
                                                      start=True, stop=True),
                         reads=allq + allk, writes=[("S", kb % 3)])
                mmS(0)
                mmS(1)
                for kb in range(nkb):
                    if kb + 2 < nkb:
                        mmS(kb + 2)
                    P = Pb[kb % 3]
                    pk = ("P", kb % 3)
                    s.op("scalar", lambda e, P=P, kb=kb, bt=bt: e.activation(
                        out=P[:, :], in_=psS[kb % 3][:, :], func=AF.Exp, bias=bt[:, kb:kb + 1], scale=scale),
                        reads=[("S", kb % 3), ("bt", par)], writes=[pk])
                    di = kb - 4 * qb
                    if di >= 0:
                        s.op("vector", lambda e, P=P, di=di: e.tensor_tensor(P[:, :], P[:, :], masks[di][:, :], ALU.mult),
                             reads=[pk, ("mask", di)], writes=[pk])
                    s.op("tensor", lambda e, P=P, kb=kb: e.matmul(psO[par][:, :], v_sb[:, kb, :], P[:, :],
                                                                  start=(kb == 0), stop=(kb == nkb - 1)),
                         reads=allv + [pk], writes=[("O", par)], signal=False)
                    s.op("tensor", lambda e, P=P, kb=kb: e.matmul(psD[par][:, :], C["onesb"][:, :], P[:, :],
                                                                  start=(kb == 0), stop=(kb == nkb - 1)),
                         reads=["cOb", pk], writes=[("O", par), ("Dn", par)])
                s.op("vector", lambda e: e.reciprocal(rec[par][:, :], psD[par][:, :]), reads=[("Dn", par)], writes=[("rec", par)])
                s.op("vector", lambda e: e.tensor_tensor(osb[par][:, :], psO[par][:, :], rec[par][:, :], ALU.mult),
                     reads=[("O", par), ("rec", par)], writes=[("osb", par)])
                s.dma("sync", oT[h, :, qs], osb[par][:, :], reads=[("osb", par)], is_output=True)
        s.finish()
    return nc


def build_hgrn(T, NH=2):
    nc = bass.Bass("TRN2", target_bir_lowering=False)
    NCH = T // 128
    SC = 4 if NCH % 4 == 0 else 1
    din = lambda n, shp: nc.dram_tensor(n, shp, F32, kind="ExternalInput").ap()
    fb_tm = din("fb_tm", [NH, T, 128]); ib_tm = din("ib_tm", [NH, T, 128])
    fbT = din("fbT", [NH, 128, T]); qT = din("qT", [NH, 128, T]); gbT = din("gbT", [NH, 128, T])
    a0r = din("a0r", [NH, 128, 128]); a1r = din("a1r", [NH, 128, 128])
    a0c = din("a0c", [NH, 128, 1]); a1c = din("a1c", [NH, 128, 1]); gnc = din("gnc", [NH, 128, 1])
    obT = nc.dram_tensor("obT", [NH, 128, T], F32, kind="ExternalOutput").ap()
    scale = float(HD) ** -0.5
    assert NH * 4 <= 8
    with ExitStack() as st:
        s = Sched(nc, st)
        C = _mk_consts(nc, s, st)
        U = C["U"]
        cnt = [0]

        def sb(shp, dt=F32):
            cnt[0] += 1
            return st.enter_context(nc.sbuf_tensor(f"h{cnt[0]}", shp, dt))

        class NS:
            pass
        V = lambda f, r, w: s.op("vector", f, reads=r, writes=w)
        A = lambda f, r, w: s.op("scalar", f, reads=r, writes=w)
        PE = lambda f, r, w: s.op("tensor", f, reads=r, writes=w)
        H = []
        for h in range(NH):
            n = NS()
            n.h = h
            n.lbr = sb([128, 128]); n.omr = sb([128, 128]); n.lbc = sb([128, 1]); n.omc = sb([128, 1]); n.gn = sb([128, 1])
            n.t1 = sb([128, 128]); n.t1c = sb([128, 1])
            n.in_fb = [sb([128, SC, 128]) for _ in range(2)]; n.in_fbT = [sb([128, SC * 128]) for _ in range(2)]
            n.in_q = [sb([128, SC * 128]) for _ in range(2)]; n.in_g = [sb([128, SC * 128]) for _ in range(2)]
            n.in_v = [sb([128, SC, 128], BF16) for _ in range(2)]
            n.LFp = [sb([128, 128], BF16) for _ in range(3)]; n.LFt = sb([128, 128])
            n.SQp = [sb([128, 128], BF16) for _ in range(2)]; n.SQt = sb([128, 128])
            n.Ftm = sb([128, 128]); n.LF = sb([128, 128]); n.KKtm = sb([128, 128]); n.Ffm = sb([128, 128]); n.KKfm = sb([128, 128])
            n.bfm = sb([128, 128]); n.btm = sb([128, 128]); n.nbm = sb([128, 1])
            n.eQ = sb([128, 128]); n.eK = sb([128, 128]); n.eB = sb([128, 128])
            n.Qt = sb([128, 128], BF16); n.Kt = sb([128, 128], BF16); n.Qb = sb([128, 128], BF16)
            n.ATm = sb([128, 128], BF16); n.dif = sb([128, 128]); n.Kh = sb([128, 128], BF16)
            n.S = sb([128, 128]); n.Sbf = sb([128, 128], BF16); n.ebl = sb([128, 1])
            n.osb = sb([128, 128]); n.sq = sb([128, 128]); n.rstd = sb([128, 128]); n.sg = sb([128, 128])
            n.res = [sb([128, SC * 128]) for _ in range(2)]
            n.pA = st.enter_context(nc.psum_tensor(f"pA{h}", [128, 512], F32))
            n.pD = st.enter_context(nc.psum_tensor(f"pD{h}", [128, 512], F32))
            n.pB = st.enter_context(nc.psum_tensor(f"pB{h}", [128, 512], F32))
            n.pC = st.enter_context(nc.psum_tensor(f"pC{h}", [128, 512], F32))
            H.append(n)

        def head_setup(n):
            h = n.h
            k = lambda name: (name, h)
            s.dma("sync", n.lbr[:, :], a0r[h], writes=[k("lbr")]); s.dma("sync", n.t1[:, :], a1r[h], writes=[k("t1")])
            s.dma("sync", n.lbc[:, :], a0c[h], writes=[k("lbc")]); s.dma("sync", n.t1c[:, :], a1c[h], writes=[k("t1c")])
            s.dma("sync", n.gn[:, :], gnc[h], writes=[k("gn")])
            V(lambda e: e.tensor_tensor(n.lbr[:, :], n.lbr[:, :], n.t1[:, :], ALU.subtract), [k("lbr"), k("t1")], [k("lbr")])
            A(lambda e: e.activation(out=n.lbr[:, :], in_=n.lbr[:, :], func=AF.Sigmoid), [k("lbr")], [k("lbr")])
            V(lambda e: e.tensor_scalar(n.omr[:, :], n.lbr[:, :], -1.0, 1.0, ALU.mult, ALU.add), [k("lbr")], [k("omr")])
            V(lambda e: e.tensor_tensor(n.lbc[:, :], n.lbc[:, :], n.t1c[:, :], ALU.subtract), [k("lbc"), k("t1c")], [k("lbc")])
            A(lambda e: e.activation(out=n.lbc[:, :], in_=n.lbc[:, :], func=AF.Sigmoid), [k("lbc")], [k("lbc")])
            V(lambda e: e.tensor_scalar(n.omc[:, :], n.lbc[:, :], -1.0, 1.0, ALU.mult, ALU.add), [k("lbc")], [k("omc")])
            V(lambda e: e.memset(n.S[:, :], 0.0), [], [k("S")])
            V(lambda e: e.memset(n.Sbf[:, :], 0.0), [], [k("Sbf")])

        def loads(n, sc):
            h = n.h
            k = lambda name: (name, h)
            p = sc % 2
            ts = slice(sc * SC * 128, (sc + 1) * SC * 128)
            s.dma("sync", n.in_fb[p][:, :, :], fb_tm[h, ts, :].rearrange("(c p) d -> p c d", p=128), writes=[k(("ifb", p))])
            s.dma("sync", n.in_fbT[p][:, :], fbT[h, :, ts], writes=[k(("ifbT", p))])
            s.dma("sync", n.in_q[p][:, :], qT[h, :, ts], writes=[k(("iq", p))])
            s.dma("sync", n.in_g[p][:, :], gbT[h, :, ts], writes=[k(("ig", p))])
            s.dma("gpsimd", n.in_v[p][:, :, :], ib_tm[h, ts, :].rearrange("(c p) d -> p c d", p=128), writes=[k(("iv", p))])

        def chunk(n, c):
            ops = []
            h = n.h
            k = lambda name: (name, h)
            sc, ci = divmod(c, SC)
            p = sc % 2
            cs = slice(ci * 128, (ci + 1) * 128)
            V = lambda f, r, w: ops.append(lambda: s.op("vector", f, reads=r, writes=w))
            A = lambda f, r, w: ops.append(lambda: s.op("scalar", f, reads=r, writes=w))
            PE = lambda f, r, w: ops.append(lambda: s.op("tensor", f, reads=r, writes=w))
            fbt, fbTt, qt, gt, vt = n.in_fb[p][:, ci, :], n.in_fbT[p][:, cs], n.in_q[p][:, cs], n.in_g[p][:, cs], n.in_v[p][:, ci, :]
            kfb, kfbT, kq, kg, kv = k(("ifb", p)), k(("ifbT", p)), k(("iq", p)), k(("ig", p)), k(("iv", p))
            A(lambda e: e.activation(out=n.Ftm[:, :], in_=fbt, func=AF.Sigmoid), [kfb], [k("Ftm")])
            V(lambda e: e.tensor_tensor(n.Ftm[:, :], n.Ftm[:, :], n.omr[:, :], ALU.mult), [k("Ftm"), k("omr")], [k("Ftm")])
            V(lambda e: e.tensor_tensor(n.Ftm[:, :], n.Ftm[:, :], n.lbr[:, :], ALU.add), [k("Ftm"), k("lbr")], [k("Ftm")])
            A(lambda e: e.activation(out=n.LF[:, :], in_=n.Ftm[:, :], func=AF.Ln), [k("Ftm")], [k("LF")])
            V(lambda e: e.tensor_scalar(n.KKtm[:, :], n.Ftm[:, :], -1.0, 1.0, ALU.mult, ALU.add), [k("Ftm")], [k("KKtm")])
            A(lambda e: e.activation(out=n.Ffm[:, :], in_=fbTt, func=AF.Sigmoid), [kfbT], [k("Ffm")])
            V(lambda e: e.tensor_scalar(n.Ffm[:, :], n.Ffm[:, :], n.omc[:, 0:1], n.lbc[:, 0:1], ALU.mult, ALU.add),
              [k("Ffm"), k("omc"), k("lbc")], [k("Ffm")])
            V(lambda e: e.tensor_scalar(n.KKfm[:, :], n.Ffm[:, :], -1.0, 1.0, ALU.mult, ALU.add), [k("Ffm")], [k("KKfm")])
            lp = [x[:, :] for x in n.LFp]
            lk = [(k("LFp"), i) for i in range(3)]
            ops.append(lambda: split_bf16(s, n.LF[:, :], k("LF"), lp, n.LFt[:, :], k("LFp"), 3))
            ops.append(lambda: mm_split(s, n.pA[:, 0:128], k("pA0"), [C["Ub"][:, :]], ["cUb"], lp, lk))
            ops.append(lambda: mm_split(s, n.pA[:, 128:256], k("pA1"), lp, lk, [C["Ub"][:, :]], ["cUb"]))
            ops.append(lambda: mm_split(s, n.pD[:, 256:384], k("pA2"), [C["onesb"][:, :]], ["cOb"], lp, lk))
            A(lambda e: e.activation(out=n.bfm[:, :], in_=n.pA[:, 128:256], func=AF.Copy), [k("pA1")], [k("bfm")])
            A(lambda e: e.activation(out=n.btm[:, :], in_=n.pA[:, 0:128], func=AF.Copy), [k("pA0")], [k("btm")])
            V(lambda e: e.tensor_scalar(n.nbm[:, :], n.bfm[:, 63:64], -1.0, None, ALU.mult), [k("bfm")], [k("nbm")])
            A(lambda e: e.activation(out=n.eQ[:, :], in_=n.bfm[:, :], func=AF.Exp, bias=n.nbm[:, 0:1], scale=1.0), [k("bfm"), k("nbm")], [k("eQ")])
            A(lambda e: e.activation(out=n.eK[:, :], in_=n.bfm[:, :], func=AF.Exp, bias=n.bfm[:, 63:64], scale=-1.0), [k("bfm")], [k("eK")])
            A(lambda e: e.activation(out=n.eB[:, :], in_=n.bfm[:, :], func=AF.Exp), [k("bfm")], [k("eB")])
            A(lambda e: e.activation(out=n.ebl[:, :], in_=n.bfm[:, 127:128], func=AF.Exp), [k("bfm")], [k("ebl")])
            V(lambda e: e.scalar_tensor_tensor(n.Qt[:, :], qt, scale, n.eQ[:, :], ALU.mult, ALU.mult), [kq, k("eQ")], [k("Qt")])
            V(lambda e: e.tensor_tensor(n.Kt[:, :], n.KKfm[:, :], n.eK[:, :], ALU.mult), [k("KKfm"), k("eK")], [k("Kt")])
            V(lambda e: e.scalar_tensor_tensor(n.Qb[:, :], qt, scale, n.eB[:, :], ALU.mult, ALU.mult), [kq, k("eB")], [k("Qb")])
            PE(lambda e: e.matmul(n.pB[:, 0:128], n.Kt[:, :], n.Qt[:, :], start=True, stop=True), [k("Kt"), k("Qt")], [k("pB")])
            V(lambda e: e.tensor_tensor(n.ATm[:, :], n.pB[:, 0:128], U[:, :], ALU.mult), [k("pB"), "cU"], [k("ATm")])
            V(lambda e: e.tensor_tensor(n.dif[:, :], n.pD[:, 256:384], n.btm[:, :], ALU.subtract), [k("pA2"), k("btm")], [k("dif")])
            A(lambda e: e.activation(out=n.dif[:, :], in_=n.dif[:, :], func=AF.Exp), [k("dif")], [k("dif")])
            V(lambda e: e.tensor_tensor(n.Kh[:, :], n.KKtm[:, :], n.dif[:, :], ALU.mult), [k("KKtm"), k("dif")], [k("Kh")])
            ops.append(lambda: s.op("tensor", lambda e: e.matmul(n.pC[:, 0:128], vt, n.ATm[:, :], start=True, stop=False),
                                    reads=[kv, k("ATm")], writes=[k("pC")], signal=False))
            PE(lambda e: e.matmul(n.pC[:, 0:128], n.Sbf[:, :], n.Qb[:, :], start=False, stop=True),
               [k("Sbf"), k("Qb"), kv, k("ATm")], [k("pC")])
            PE(lambda e: e.matmul(n.pD[:, 0:128], n.Kh[:, :], vt, start=True, stop=True), [k("Kh"), kv], [k("pD")])
            V(lambda e: e.scalar_tensor_tensor(n.S[:, :], n.S[:, :], n.ebl[:, 0:1], n.pD[:, 0:128], ALU.mult, ALU.add),
              [k("S"), k("ebl"), k("pD")], [k("S")])
            A(lambda e: e.activation(out=n.Sbf[:, :], in_=n.S[:, :], func=AF.Copy), [k("S")], [k("Sbf")])
            A(lambda e: e.activation(out=n.osb[:, :], in_=n.pC[:, 0:128], func=AF.Copy), [k("pC")], [k("osb")])
            A(lambda e: e.activation(out=n.sq[:, :], in_=n.osb[:, :], func=AF.Square), [k("osb")], [k("sq")])
            qp = [x[:, :] for x in n.SQp]
            qk_ = [(k("SQp"), i) for i in range(2)]
            ops.append(lambda: split_bf16(s, n.sq[:, :], k("sq"), qp, n.SQt[:, :], k("SQp"), 2))
            ops.append(lambda: mm_split(s, n.pB[:, 256:384], k("pE"), [C["onesb"][:, :]], ["cOb"], qp, qk_))
            V(lambda e: e.tensor_scalar(n.rstd[:, :], n.pB[:, 256:384], 1.0 / HD, RMS_EPS, ALU.mult, ALU.add), [k("pE")], [k("rstd")])
            A(lambda e: e.activation(out=n.rstd[:, :], in_=n.rstd[:, :], func=AF.Sqrt), [k("rstd")], [k("rstd")])
            V(lambda e: e.reciprocal(n.rstd[:, :], n.rstd[:, :]), [k("rstd")], [k("rstd")])
            A(lambda e: e.activation(out=n.sg[:, :], in_=gt, func=AF.Silu), [kg], [k("sg")])
            r = n.res[p][:, cs]
            kr = k(("res", p, ci))
            V(lambda e: e.tensor_tensor(r, n.osb[:, :], n.rstd[:, :], ALU.mult), [k("osb"), k("rstd")], [kr])
            V(lambda e: e.scalar_tensor_tensor(r, r, n.gn[:, 0:1], n.sg[:, :], ALU.mult, ALU.mult), [kr, k("gn"), k("sg")], [kr])
            if ci == SC - 1:
                ts = slice(sc * SC * 128, (sc + 1) * SC * 128)
                ops.append(lambda: s.dma("sync", obT[h, :, ts], n.res[p][:, :], reads=[k(("res", p, j)) for j in range(SC)],
                                         is_output=True))
            return ops

        from itertools import zip_longest
        for n in H:
            head_setup(n)
            loads(n, 0)
        for c in range(NCH):
            sc, ci = divmod(c, SC)
            if ci == 0 and sc + 1 < NCH // SC:
                for n in H:
                    loads(n, sc + 1)
            for group in zip_longest(*[chunk(n, c) for n in H]):
                for th in group:
                    if th is not None:
                        th()
        s.finish()
    return nc


def build_route(M):
    nc = bass.Bass("TRN2", target_bir_lowering=False)
    lg = nc.dram_tensor("lg", [M, 72], F32, kind="ExternalInput").ap()
    R = nc.dram_tensor("R", [M, 8], F32, kind="ExternalOutput").ap()
    with ExitStack() as st:
        s = Sched(nc, st)
        cnt = [0]

        def sb(shp, dt=F32):
            cnt[0] += 1
            return st.enter_context(nc.sbuf_tensor(f"r{cnt[0]}", shp, dt))
        io8 = sb([128, 8])
        s.op("gpsimd", lambda e: e.iota(io8[:, :], [[1, 8]], base=0, channel_multiplier=0,
                                        allow_small_or_imprecise_dtypes=True), writes=["io8"])
        V = lambda f, r, w: s.op("vector", f, reads=r, writes=w)
        A = lambda f, r, w: s.op("scalar", f, reads=r, writes=w)
        Ls = [sb([128, 72]) for _ in range(2)]
        Ro = [sb([128, 8]) for _ in range(2)]
        G8 = sb([128, 8]); GI = sb([128, 8], U32); gs = sb([128, 1]); nb = sb([128, 1]); ex = sb([128, 8]); sm = sb([128, 1])
        oh = sb([128, 8]); EL = sb([128, 8]); E8 = sb([128, 8]); EI = sb([128, 8], U32); d = sb([128, 1]); r1 = sb([128, 1])
        for blk in range(M // 128):
            p = blk % 2
            L, Rt = Ls[p], Ro[p]
            rs = slice(blk * 128, (blk + 1) * 128)
            s.dma("sync", L[:, :], lg[rs, :], writes=[("L", p)])
            V(lambda e: e.memset(Rt[:, :], 0.0), [], [("R", p)])
            V(lambda e: e.max(G8[:, :], L[:, 0:8]), [("L", p)], ["G8"])
            V(lambda e: e.max_index(GI[:, :], G8[:, :], L[:, 0:8]), [("L", p), "G8"], ["GI"])
            V(lambda e: e.tensor_copy(gs[:, :], GI[:, 0:1]), ["GI"], ["gs"])
            V(lambda e: e.tensor_scalar(nb[:, :], G8[:, 0:1], -1.0, None, ALU.mult), ["G8"], ["nb"])
            A(lambda e: e.activation(out=ex[:, :], in_=L[:, 0:8], func=AF.Exp, bias=nb[:, 0:1], scale=1.0, accum_out=sm[:, 0:1]),
              [("L", p), "nb"], ["ex", "sm"])
            V(lambda e: e.reciprocal(sm[:, :], sm[:, :]), ["sm"], ["sm"])
            V(lambda e: e.tensor_scalar(oh[:, :], io8[:, :], gs[:, 0:1], None, ALU.is_equal), ["io8", "gs"], ["oh"])
            V(lambda e: e.tensor_scalar(EL[:, :], L[:, 8:16], oh[:, 0:1], None, ALU.mult), [("L", p), "oh"], ["EL"])
            for g in range(1, 8):
                V(lambda e, g=g: e.scalar_tensor_tensor(EL[:, :], L[:, 8 + 8 * g:16 + 8 * g], oh[:, g:g + 1], EL[:, :], ALU.mult, ALU.add),
                  [("L", p), "oh", "EL"], ["EL"])
            V(lambda e: e.max(E8[:, :], EL[:, :]), ["EL"], ["E8"])
            V(lambda e: e.max_index(EI[:, :], E8[:, :], EL[:, :]), ["EL", "E8"], ["EI"])
            V(lambda e: e.tensor_tensor(d[:, :], E8[:, 1:2], E8[:, 0:1], ALU.subtract), ["E8"], ["d"])
            A(lambda e: e.activation(out=d[:, :], in_=d[:, :], func=AF.Exp), ["d"], ["d"])
            V(lambda e: e.tensor_scalar(r1[:, :], d[:, :], 1.0, None, ALU.add), ["d"], ["r1"])
            V(lambda e: e.reciprocal(r1[:, :], r1[:, :]), ["r1"], ["r1"])
            V(lambda e: e.tensor_tensor(r1[:, :], r1[:, :], sm[:, :], ALU.mult), ["r1", "sm"], ["r1"])
            V(lambda e: e.tensor_copy(Rt[:, 0:1], gs[:, :]), ["gs", ("R", p)], [("R", p)])
            V(lambda e: e.tensor_copy(Rt[:, 1:3], EI[:, 0:2]), ["EI", ("R", p)], [("R", p)])
            V(lambda e: e.tensor_copy(Rt[:, 3:4], r1[:, :]), ["r1", ("R", p)], [("R", p)])
            V(lambda e: e.tensor_tensor(Rt[:, 4:5], r1[:, :], d[:, :], ALU.mult), ["r1", "d", ("R", p)], [("R", p)])
            s.dma("sync", R[rs, :], Rt[:, :], reads=[("R", p)], is_output=True)
        s.finish()
    return nc


def build_ln2(M):
    nc = bass.Bass("TRN2", target_bir_lowering=False)
    din = lambda n, shp: nc.dram_tensor(n, shp, F32, kind="ExternalInput").ap()
    dr = dict(x1=din("x1", [M, D]), o1=din("o1", [M, D]), o2=din("o2", [M, D]),
              ln_g=din("ln_g", [128, D]), ln_b=din("ln_b", [128, D]))
    y = nc.dram_tensor("y", [M, D], F32, kind="ExternalOutput").ap()
    with ExitStack() as st:
        s = Sched(nc, st)
        ctx = dict(nc=nc, s=s, st=st, dr=dr)
        S = {}
        ln_consts(ctx, S, "ln_g", "ln_b")
        rr = Rot(ctx, "r", [128, D], F32, 2)
        aa = Rot(ctx, "a", [128, D], F32, 2)
        for blk in range(M // 128):
            rs = slice(blk * 128, (blk + 1) * 128)
            r, rk = rr.next()
            s.dma("sync", r[:, :], dr["x1"][rs, :], writes=[rk])
            a, ak = aa.next()
            s.dma("sync", a[:, :], dr["o1"][rs, :], writes=[ak])
            s.op("vector", lambda e, r=r, a=a: e.scalar_tensor_tensor(r[:, :], r[:, :], ALPHA, a[:, :], ALU.mult, ALU.add),
                 reads=[rk, ak], writes=[rk])
            a, ak = aa.next()
            s.dma("sync", a[:, :], dr["o2"][rs, :], writes=[ak])
            s.op("vector", lambda e, r=r, a=a: e.tensor_tensor(r[:, :], r[:, :], a[:, :], ALU.add), reads=[rk, ak], writes=[rk])
            ln_rows(ctx, S, r, rk)
            s.dma("sync", y[rs, :], r[:, :], reads=[rk], is_output=True)
        s.finish()
    return nc


def epi_ple():
    S = {}

    def consts(ctx):
        nc, s, dr = ctx["nc"], ctx["s"], ctx["dr"]
        S["o"] = Rot(ctx, "ost", [128, 512], F32, 3)
        S["x"] = Rot(ctx, "xst", [128, 512], F32, 3)
        S["b"] = ctx["st"].enter_context(nc.sbuf_tensor("biasb", [128, ctx["N"]], F32))
        s.dma("sync", S["b"][:, :], dr["bias"][:, :], writes=["biasb"])

    def epi(ctx, ps, keys, b, row0, n0, nbw):
        s, dr = ctx["s"], ctx["dr"]
        o, ok = S["o"].next()
        x, xk = S["x"].next()
        s.dma("sync", x[:, 0:nbw], dr["xres"][row0:row0 + 128, n0:n0 + nbw], writes=[xk])
        s.op("vector", lambda e: e.tensor_tensor(o[:, 0:nbw], ps[0][:, 0:nbw], S["b"][:, n0:n0 + nbw], ALU.add),
             reads=[keys[0], "biasb"], writes=[ok])
        s.op("scalar", lambda e: e.activation(out=o[:, 0:nbw], in_=o[:, 0:nbw], func=AF.Sigmoid), reads=[ok], writes=[ok])
        s.op("vector", lambda e: e.tensor_tensor(o[:, 0:nbw], o[:, 0:nbw], ps[1][:, 0:nbw], ALU.mult),
             reads=[ok, keys[1]], writes=[ok])
        s.op("vector", lambda e: e.tensor_tensor(o[:, 0:nbw], o[:, 0:nbw], x[:, 0:nbw], ALU.add), reads=[ok, xk], writes=[ok])
        s.dma("sync", dr["y"][b, row0:row0 + 128, n0:n0 + nbw], o[:, 0:nbw], reads=[ok], is_output=True)

    return consts, epi


def _bc(v, n=128):
    v = np.asarray(v, np.float32).reshape(1, -1)
    return np.ascontiguousarray(np.broadcast_to(v, (n, v.shape[1])))


def kernel(x, p, w_in, b_fox_f, hgrn_lb, hgrn_norm_g, w_branch_a, w_branch_b, w_out, ln1_g, ln1_b,
           w_group_router, b_group_router, w_expert_router, b_expert_router, w_exp_gate, w_exp_up,
           w_exp_down, ln2_g, ln2_b, w_ple_gate, b_ple_gate, w_ple_proj):
    x = np.asarray(x, np.float32)
    T = x.shape[1]
    TC = T // NCORES
    X = x[0]
    XT = np.ascontiguousarray(X.T)
    W = np.asarray(w_in[0], np.float32)
    FW = FOXH * HD
    o_q, o_k, o_v, o_f = 0, FW, 2 * FW, 3 * FW
    o_hq = 3 * FW + FOXH
    o_hf, o_hi, o_hg = o_hq + FW, o_hq + 2 * FW, o_hq + 3 * FW
    o_ga = o_hq + 4 * FW
    o_gb = o_ga + D
    cols = []
    for c in range(NCORES):
        hs = [2 * c, 2 * c + 1]
        cc = []
        for base in (o_q, o_k, o_v):
            for h in hs:
                cc.append(np.arange(base + h * HD, base + (h + 1) * HD))
        for base in (o_hq, o_hf, o_hi, o_hg):
            for h in hs:
                cc.append(np.arange(base + h * HD, base + (h + 1) * HD))
        cc.append(np.array([o_f + hs[0], o_f + hs[1]]))
        cols.append(np.concatenate(cc))
    NCOL = len(cols[0])
    c_, e_ = epi_simple()
    nc = build_gemm(T, NCOL, [dict(xt="xt", w="w", K=D)], e_, consts=c_, w_resident=True, xt_bufs=2)
    res = _run(nc, [{"xt": XT[None], "w": _c(W[:, cols[c]])[None]} for c in range(NCORES)])
    U_ = [r["y"][0] for r in res]
    del res
    tr = lambda a: np.ascontiguousarray(a.transpose(0, 2, 1))
    nc = build_attn(T, 2)
    ims = []
    for c in range(NCORES):
        u = U_[c]
        q = np.stack([u[:, 0:128], u[:, 128:256]]); k = np.stack([u[:, 256:384], u[:, 384:512]])
        v = np.stack([u[:, 512:640], u[:, 640:768]])
        fa = np.stack([u[:, 1792], u[:, 1793]])
        ims.append({"qT": tr(q), "kT": tr(k), "v": _c(v),
                    "fac": np.ascontiguousarray(fa.reshape(2, T // 128, 128).transpose(0, 2, 1)),
                    "bfb": _c(np.broadcast_to(np.asarray(b_fox_f[0], np.float32)[2 * c:2 * c + 2, None, None], (2, 128, 1)))})
    res = _run(nc, ims)
    oaT = np.concatenate([r["oT"].reshape(256, T) for r in res], 0)
    nc = build_hgrn(T, 2)
    ims = []
    lbv = np.asarray(hgrn_lb, np.float32)
    gnv = np.asarray(hgrn_norm_g[0], np.float32)
    for c in range(NCORES):
        u = U_[c]
        hq = np.stack([u[:, 768:896], u[:, 896:1024]]); hf = np.stack([u[:, 1024:1152], u[:, 1152:1280]])
        hi = np.stack([u[:, 1280:1408], u[:, 1408:1536]]); hg = np.stack([u[:, 1536:1664], u[:, 1664:1792]])
        a0 = lbv[0].reshape(16, 128)[2 * c:2 * c + 2]; a1 = lbv[1].reshape(16, 128)[2 * c:2 * c + 2]
        gn = gnv.reshape(16, 128)[2 * c:2 * c + 2]
        ims.append({"fb_tm": _c(hf), "ib_tm": _c(hi), "fbT": tr(hf), "qT": tr(hq), "gbT": tr(hg),
                    "a0r": _c(np.broadcast_to(a0[:, None, :], (2, 128, 128))), "a1r": _c(np.broadcast_to(a1[:, None, :], (2, 128, 128))),
                    "a0c": _c(a0[:, :, None]), "a1c": _c(a1[:, :, None]), "gnc": _c(gn[:, :, None])})
    res = _run(nc, ims)
    obT = np.concatenate([r["obT"].reshape(256, T) for r in res], 0)
    del U_, ims
    c_, e_ = epi_merge()
    nc = build_gemm(TC, D, [dict(xt="xt", w="wga", K=D), dict(xt="xt", w="wgb", K=D),
                            dict(xt="at", w="wa", K=FW), dict(xt="bt", w="wb", K=FW)], e_, consts=c_, NB=256)
    wga = _c(W[:, o_ga:o_ga + D])[None]; wgb = _c(W[:, o_gb:o_gb + D])[None]
    wa = _c(w_branch_a[0])[None]; wb = _c(w_branch_b[0])[None]
    sl = lambda c: slice(c * TC, (c + 1) * TC)
    res = _run(nc, [{"xt": _c(XT[:, sl(c)])[None], "at": _c(oaT[:, sl(c)])[None], "bt": _c(obT[:, sl(c)])[None],
                     "wga": wga, "wgb": wgb, "wa": wa, "wb": wb} for c in range(NCORES)])
    Y = np.concatenate([r["y"][0] for r in res], 0)
    del wga, wgb, W
    c_, e_, rf_ = epi_res_ln()
    nc = build_gemm(TC, D, [dict(xt="xt", w="w", K=D)], e_, extra=[("xres", [TC, D]), ("ln_g", [128, D]), ("ln_b", [128, D])],
                    consts=c_, row_final=rf_, NB=256)
    YT = np.ascontiguousarray(Y.T)
    res = _run(nc, [{"xt": _c(YT[:, sl(c)])[None], "w": _c(w_out[0])[None], "xres": _c(X[sl(c)]),
                     "ln_g": _bc(ln1_g[0]), "ln_b": _bc(ln1_b[0])} for c in range(NCORES)])
    X1 = np.concatenate([r["y"][0] for r in res], 0)
    X1T = np.ascontiguousarray(X1.T)
    wr = _c(np.concatenate([w_group_router[0], w_expert_router[0]], 1))[None]
    br = _bc(np.concatenate([b_group_router[0], b_expert_router[0]]))
    c_, e_ = epi_simple(bias="bias")
    nc = build_gemm(TC, 72, [dict(xt="xt", w="w", K=D)], e_, extra=[("bias", [128, 72])], consts=c_)
    res = _run(nc, [{"xt": _c(X1T[:, sl(c)])[None], "w": wr, "bias": br} for c in range(NCORES)])
    nc = build_route(TC)
    res = _run(nc, [{"lg": _c(res[c]["y"][0])} for c in range(NCORES)])
    R = np.concatenate([r["R"] for r in res], 0)
    gsel = np.rint(R[:, 0]).astype(np.int64)
    eid = np.stack([gsel * EPG + np.rint(R[:, 1]).astype(np.int64), gsel * EPG + np.rint(R[:, 2]).astype(np.int64)], 1)
    gate = R[:, 3:5]
    flat_e = eid.reshape(-1)
    order = np.argsort(flat_e, kind="stable")
    counts = np.bincount(flat_e, minlength=NE)
    starts = np.cumsum(counts) - counts
    pos = np.empty(2 * T, np.int64)
    pos[order] = np.arange(2 * T) - starts[flat_e[order]]
    ME = int(max(128, -(-counts.max() // 128) * 128))
    tok_tab = np.full((NE, ME), -1, np.int64)
    tok_tab[flat_e, pos] = np.repeat(np.arange(T), 2)
    gw_tab = np.zeros((NE, ME, 1), np.float32)
    gw_tab[flat_e, pos, 0] = gate.reshape(-1)
    X1p = np.concatenate([X1, np.zeros((1, D), np.float32)], 0)
    c_, e_ = epi_glu()
    nc = build_gemm(ME, DE, [dict(xt="xt", w="wg", K=D), dict(xt="xt", w="wu", K=D)], e_, consts=c_, B=EPG)
    ims = []
    for c in range(NCORES):
        es = slice(c * EPG, (c + 1) * EPG)
        xg = X1p[tok_tab[es]]
        ims.append({"xt": np.ascontiguousarray(xg.transpose(0, 2, 1)), "wg": _c(w_exp_gate[0, es]), "wu": _c(w_exp_up[0, es])})
    res = _run(nc, ims)
    H = [r["y"] for r in res]
    del ims
    c_, e_ = epi_simple(rowscale="rs")
    nc = build_gemm(ME, D, [dict(xt="xt", w="w", K=DE)], e_, extra=[("rs", [EPG, ME, 1])], consts=c_, B=EPG)
    res = _run(nc, [{"xt": np.ascontiguousarray(H[c].transpose(0, 2, 1)), "w": _c(w_exp_down[0, c * EPG:(c + 1) * EPG]),
                     "rs": gw_tab[c * EPG:(c + 1) * EPG]} for c in range(NCORES)])
    O = np.concatenate([r["y"] for r in res], 0)
    pos2 = pos.reshape(T, 2)
    O1 = O[eid[:, 0], pos2[:, 0]]
    O2 = O[eid[:, 1], pos2[:, 1]]
    del O, H
    nc = build_ln2(TC)
    res = _run(nc, [{"x1": _c(X1[sl(c)]), "o1": _c(O1[sl(c)]), "o2": _c(O2[sl(c)]),
                     "ln_g": _bc(ln2_g[0]), "ln_b": _bc(ln2_b[0])} for c in range(NCORES)])
    X2 = np.concatenate([r["y"] for r in res], 0)
    X2T = np.ascontiguousarray(X2.T)
    PT = np.ascontiguousarray(np.asarray(p[0, 0], np.float32).T)
    c_, e_ = epi_ple()
    nc = build_gemm(TC, D, [dict(xt="xt", w="wpg", K=D), dict(xt="pt", w="wpe", K=PLE)], e_,
                    extra=[("xres", [TC, D]), ("bias", [128, D])], consts=c_, SBo=(1024 if TC % 1024 == 0 else None))
    res = _run(nc, [{"xt": _c(X2T[:, sl(c)])[None], "pt": _c(PT[:, sl(c)])[None], "wpg": _c(w_ple_gate[0])[None],
                     "wpe": _c(w_ple_proj[0])[None], "xres": _c(X2[sl(c)]), "bias": _bc(b_ple_gate[0])} for c in range(NCORES)])
    out = np.concatenate([r["y"][0] for r in res], 0)
    return out[None].astype(np.float32)
```

```python
import numpy as np
from contextlib import ExitStack
import concourse.bass as bass
import concourse.mybir as mybir
from concourse.bass_utils import run_bass_kernel_spmd

F32 = mybir.dt.float32
BF16 = mybir.dt.bfloat16
U32 = mybir.dt.uint32
AF = mybir.ActivationFunctionType
ALU = mybir.AluOpType
AX = mybir.AxisListType

NCORES = 8
D = 4096
FOXH = 16
HD = 128
NG = 8
EPG = 8
NE = 64
DE = 512
PLE = 256
ALPHA = 2.0 ** 0.25
LN_EPS = 1e-5
RMS_EPS = 1e-6
import os
SELF_WAIT = os.environ.get('SELF_WAIT', '1') == '1'


class Sched:
    COMPUTE = ("tensor", "vector", "scalar", "gpsimd")
    LIMIT = 30000

    def __init__(self, nc, stack, ndma=8, nrot=3):
        self.nc = nc
        self.eng = {"tensor": nc.tensor, "vector": nc.vector, "scalar": nc.scalar,
                    "gpsimd": nc.gpsimd, "sync": nc.sync}
        self.csem = {e: [stack.enter_context(nc.semaphore(f"c_{e}_{k}")) for k in range(nrot)]
                     for e in self.COMPUTE}
        self.ccount = {e: 0 for e in self.COMPUTE}
        self.dsem = {q: [stack.enter_context(nc.semaphore(f"d_{q}_{k}")) for k in range(ndma)]
                     for q in ("sync", "gpsimd")}
        self.dcount = {q: [0] * ndma for q in ("sync", "gpsimd")}
        self.dnext = {q: 0 for q in ("sync", "gpsimd")}
        self.known = {e: {} for e in self.eng}
        self.kord = {e: {c: -1 for c in self.COMPUTE} for e in self.eng}
        self.last_w = {}
        self.readers = {}
        self.out_events = []

    def _wait(self, e, ev):
        if ev[0] == "c":
            _, src, n = ev
            if src == e and (e == "tensor" or not SELF_WAIT):
                return
            if self.kord[e][src] >= n:
                return
            k, v = divmod(n, self.LIMIT)
            self.eng[e].wait_ge(self.csem[src][k], v + 1)
            self.kord[e][src] = n
        else:
            _, sem, val, key = ev
            if self.known[e].get(key, 0) >= val:
                return
            self.eng[e].wait_ge(sem, val)
            self.known[e][key] = val

    def _deps(self, reads, writes):
        evs = []
        for b in reads:
            if b in self.last_w:
                evs.append(self.last_w[b])
        for b in writes:
            if b in self.last_w:
                evs.append(self.last_w[b])
            r = self.readers.get(b)
            if r:
                evs.extend(r["c"].values())
                evs.extend(r["d"])
        return evs

    def _record(self, ev, reads, writes):
        for b in writes:
            self.last_w[b] = ev
            self.readers[b] = {"c": {}, "d": []}
        for b in reads:
            r = self.readers.setdefault(b, {"c": {}, "d": []})
            if ev[0] == "c":
                r["c"][ev[1]] = ev
            else:
                r["d"].append(ev)

    def op(self, e, fn, reads=(), writes=(), signal=True):
        self.nops = getattr(self, "nops", 0) + 1
        import os
        if self.nops > int(os.environ.get("DBG_LIMIT", "100000000")):
            return None
        for ev in self._deps(reads, writes):
            self._wait(e, ev)
        ins = fn(self.eng[e])
        if not signal:
            return None
        n = self.ccount[e]
        self.ccount[e] += 1
        k, _ = divmod(n, self.LIMIT)
        ins.then_inc(self.csem[e][k], 1)
        ev = ("c", e, n)
        self._record(ev, reads, writes)
        return ev

    def dma(self, q, out, in_, reads=(), writes=(), is_output=False):
        slot = self.dnext[q]
        self.dnext[q] = (slot + 1) % len(self.dsem[q])
        sem = self.dsem[q][slot]
        key = (q, slot)
        if self.dcount[q][slot] > 0:
            self._wait(q, ("d", sem, self.dcount[q][slot], key))
        for ev in self._deps(reads, writes):
            self._wait(q, ev)
        self.eng[q].dma_start(out=out, in_=in_).then_inc(sem, 16)
        self.dcount[q][slot] += 16
        ev = ("d", sem, self.dcount[q][slot], key)
        self._record(ev, reads, writes)
        if is_output:
            self.out_events.append(ev)
        return ev

    def finish(self):
        for c in self.COMPUTE:
            if self.ccount[c] > 0:
                self._wait("sync", ("c", c, self.ccount[c] - 1))
        for ev in self.out_events:
            self._wait("sync", ev)
        for q in self.dsem:
            for slot, sem in enumerate(self.dsem[q]):
                if self.dcount[q][slot] > 0:
                    self._wait("sync", ("d", sem, self.dcount[q][slot], (q, slot)))


def _run(nc, in_maps):
    res = run_bass_kernel_spmd(nc, in_maps, core_ids=list(range(NCORES)))
    return res.results


def _c(a):
    return np.ascontiguousarray(a, dtype=np.float32)


def build_gemm(M, N, groups, epilogue, extra=(), out_cols=None, B=1, NB=512, row_final=None,
               consts=None, outs=None, SBo=None, w_resident=False, xt_bufs=1):
    nc = bass.Bass("TRN2", target_bir_lowering=False)
    out_cols = N if out_cols is None else out_cols
    dr = {}
    for g in groups:
        if g["xt"] not in dr:
            dr[g["xt"]] = nc.dram_tensor(g["xt"], [B, g["K"], M], F32, kind="ExternalInput").ap()
        dr[g["w"]] = nc.dram_tensor(g["w"], [B, g["K"], N], F32, kind="ExternalInput").ap()
    for name, shape in extra:
        dr[name] = nc.dram_tensor(name, list(shape), F32, kind="ExternalInput").ap()
    outs = outs or [("y", [B, M, out_cols])]
    for name, shape in outs:
        dr[name] = nc.dram_tensor(name, list(shape), F32, kind="ExternalOutput").ap()
    if SBo is not None:
        SB = SBo
    elif M <= 1024 and M % 128 == 0:
        SB = M
    else:
        SB = 512 if M % 512 == 0 else 128
    nsub = SB // 128
    with ExitStack() as st:
        s = Sched(nc, st)
        xt_t = {}
        for g in groups:
            if g["xt"] not in xt_t:
                KC = g["K"] // 128
                xt_t[g["xt"]] = [st.enter_context(nc.sbuf_tensor(f"xt_{g['xt']}_{i}", [128, KC, SB], BF16))
                                 for i in range(xt_bufs)]
        w_t = {}
        for g in groups:
            KC = g["K"] // 128
            if w_resident:
                assert B == 1
                w_t[g["w"]] = st.enter_context(nc.sbuf_tensor(f"w_{g['w']}", [128, KC, N], BF16))
            else:
                w_t[g["w"]] = [st.enter_context(nc.sbuf_tensor(f"w_{g['w']}_{i}", [128, KC, NB], BF16))
                               for i in range(2)]
        ps = [[st.enter_context(nc.psum_tensor(f"ps_{gi}_{i}", [128, 512], F32)) for i in range(2)]
              for gi in range(len(groups))]
        ctx = dict(nc=nc, s=s, st=st, dr=dr, SB=SB, nsub=nsub, NB=NB, M=M, N=N)
        if consts is not None:
            consts(ctx)
        it = 0
        nblk = (N + NB - 1) // NB
        if w_resident:
            for g in groups:
                for nb in range(nblk):
                    n0 = nb * NB
                    nbw = min(NB, N - n0)
                    s.dma("gpsimd", w_t[g["w"]][:, :, n0:n0 + nbw],
                          dr[g["w"]][0, :, n0:n0 + nbw].rearrange("(kc p) n -> p kc n", p=128),
                          writes=[("w", g["w"], nb)])
        sbi = 0
        for b in range(B):
            for sb in range(M // SB):
                r0 = sb * SB
                xp = sbi % xt_bufs
                sbi += 1
                for name, t in xt_t.items():
                    s.dma("gpsimd", t[xp][:, :, :],
                          dr[name][b, :, r0:r0 + SB].rearrange("(kc p) m -> p kc m", p=128),
                          writes=[("xt", name, xp)])
                for nb in range(nblk):
                    n0 = nb * NB
                    nbw = min(NB, N - n0)
                    par = it % 2
                    it += 1
                    if not w_resident:
                        for g in groups:
                            s.dma("gpsimd", w_t[g["w"]][par][:, :, 0:nbw],
                                  dr[g["w"]][b, :, n0:n0 + nbw].rearrange("(kc p) n -> p kc n", p=128),
                                  writes=[("w", g["w"], par)])
                    for sub in range(nsub):
                        pp = (it * nsub + sub) % 2
                        for gi, g in enumerate(groups):
                            KC = g["K"] // 128
                            if w_resident:
                                wt, wk, wo = w_t[g["w"]], ("w", g["w"], nb), n0
                            else:
                                wt, wk, wo = w_t[g["w"]][par], ("w", g["w"], par), 0
                            for kc in range(KC):
                                s.op("tensor",
                                     lambda e, gi=gi, g=g, kc=kc, pp=pp, sub=sub, KC=KC, nbw=nbw, wt=wt, wo=wo, xp=xp:
                                     e.matmul(ps[gi][pp][:, 0:nbw],
                                              xt_t[g["xt"]][xp][:, kc, sub * 128:(sub + 1) * 128],
                                              wt[:, kc, wo:wo + nbw],
                                              start=(kc == 0), stop=(kc == KC - 1)),
                                     reads=[("xt", g["xt"], xp), wk],
                                     writes=[("ps", gi, pp)], signal=(kc == KC - 1))
                        epilogue(ctx, [ps[gi][pp] for gi in range(len(groups))],
                                 [("ps", gi, pp) for gi in range(len(groups))],
                                 b, r0 + sub * 128, n0, nbw)
                if row_final is not None:
                    row_final(ctx, b, r0)
        s.finish()
    return nc


class Rot:
    def __init__(self, ctx, name, shape, dtype, n=2):
        self.t = [ctx["st"].enter_context(ctx["nc"].sbuf_tensor(f"{name}_{i}", list(shape), dtype))
                  for i in range(n)]
        self.name = name
        self.i = 0

    def next(self):
        k = self.i % len(self.t)
        self.i += 1
        return self.t[k], (self.name, k)


def epi_simple(func=None, bias=None, rowscale=None, out="y"):
    S = {}

    def consts(ctx):
        nc, s, dr = ctx["nc"], ctx["s"], ctx["dr"]
        S["o"] = Rot(ctx, "ost", [128, 512], F32, 3)
        if bias:
            S["b"] = ctx["st"].enter_context(nc.sbuf_tensor("biasb", [128, ctx["N"]], F32))
            s.dma("sync", S["b"][:, :], dr[bias][:, :], writes=["biasb"])
        if rowscale:
            S["rs"] = Rot(ctx, "rs", [128, 1], F32, 2)

    def epi(ctx, ps, keys, b, row0, n0, nbw):
        s, dr = ctx["s"], ctx["dr"]
        o, ok = S["o"].next()
        if bias:
            s.op("vector", lambda e: e.tensor_tensor(o[:, 0:nbw], ps[0][:, 0:nbw], S["b"][:, n0:n0 + nbw], ALU.add),
                 reads=[keys[0], "biasb"], writes=[ok])
            if func is not None:
                s.op("scalar", lambda e: e.activation(out=o[:, 0:nbw], in_=o[:, 0:nbw], func=func),
                     reads=[ok], writes=[ok])
        else:
            s.op("scalar", lambda e: e.activation(out=o[:, 0:nbw], in_=ps[0][:, 0:nbw],
                                                  func=(func if func is not None else AF.Copy)),
                 reads=[keys[0]], writes=[ok])
        if rowscale:
            rs, rk = S["rs"].next()
            s.dma("sync", rs[:, :], dr[rowscale][b, row0:row0 + 128, :], writes=[rk])
            s.op("vector", lambda e: e.tensor_scalar(o[:, 0:nbw], o[:, 0:nbw], rs[:, 0:1], None, ALU.mult),
                 reads=[ok, rk], writes=[ok])
        s.dma("sync", dr[out][b, row0:row0 + 128, n0:n0 + nbw], o[:, 0:nbw], reads=[ok], is_output=True)

    return consts, epi


def epi_glu():
    S = {}

    def consts(ctx):
        S["o"] = Rot(ctx, "ost", [128, 512], F32, 3)

    def epi(ctx, ps, keys, b, row0, n0, nbw):
        s, dr = ctx["s"], ctx["dr"]
        o, ok = S["o"].next()
        s.op("scalar", lambda e: e.activation(out=o[:, 0:nbw], in_=ps[0][:, 0:nbw], func=AF.Silu),
             reads=[keys[0]], writes=[ok])
        s.op("vector", lambda e: e.tensor_tensor(o[:, 0:nbw], o[:, 0:nbw], ps[1][:, 0:nbw], ALU.mult),
             reads=[ok, keys[1]], writes=[ok])
        s.dma("sync", dr["y"][b, row0:row0 + 128, n0:n0 + nbw], o[:, 0:nbw], reads=[ok], is_output=True)

    return consts, epi


def epi_merge():
    S = {}

    def consts(ctx):
        S["o"] = Rot(ctx, "ost", [128, 512], F32, 3)
        S["t"] = Rot(ctx, "tst", [128, 512], F32, 2)

    def epi(ctx, ps, keys, b, row0, n0, nbw):
        s, dr = ctx["s"], ctx["dr"]
        o, ok = S["o"].next()
        t, tk = S["t"].next()
        s.op("scalar", lambda e: e.activation(out=o[:, 0:nbw], in_=ps[0][:, 0:nbw], func=AF.Sigmoid),
             reads=[keys[0]], writes=[ok])
        s.op("scalar", lambda e: e.activation(out=t[:, 0:nbw], in_=ps[1][:, 0:nbw], func=AF.Sigmoid),
             reads=[keys[1]], writes=[tk])
        s.op("vector", lambda e: e.tensor_tensor(o[:, 0:nbw], o[:, 0:nbw], ps[2][:, 0:nbw], ALU.mult),
             reads=[ok, keys[2]], writes=[ok])
        s.op("vector", lambda e: e.tensor_tensor(t[:, 0:nbw], t[:, 0:nbw], ps[3][:, 0:nbw], ALU.mult),
             reads=[tk, keys[3]], writes=[tk])
        s.op("vector", lambda e: e.tensor_tensor(o[:, 0:nbw], o[:, 0:nbw], t[:, 0:nbw], ALU.add),
             reads=[ok, tk], writes=[ok])
        s.dma("sync", dr["y"][b, row0:row0 + 128, n0:n0 + nbw], o[:, 0:nbw], reads=[ok], is_output=True)

    return consts, epi


def ln_rows(ctx, S, r, rk, gk="lng", bk="lnb"):
    s = ctx["s"]
    st1, k1 = S["st"].next()
    s.op("vector", lambda e: e.reduce_sum(st1[:, 0:1], r[:, :], AX.X), reads=[rk], writes=[k1])
    s.op("vector", lambda e: e.tensor_scalar(st1[:, 0:1], st1[:, 0:1], -1.0 / D, None, ALU.mult),
         reads=[k1], writes=[k1])
    s.op("vector", lambda e: e.tensor_scalar(r[:, :], r[:, :], st1[:, 0:1], None, ALU.add),
         reads=[rk, k1], writes=[rk])
    sq, sk = S["sq"].next()
    s.op("scalar", lambda e: e.activation(out=sq[:, :], in_=r[:, :], func=AF.Square, accum_out=st1[:, 1:2]),
         reads=[rk], writes=[sk, k1])
    s.op("vector", lambda e: e.tensor_scalar(st1[:, 1:2], st1[:, 1:2], 1.0 / D, LN_EPS, ALU.mult, ALU.add),
         reads=[k1], writes=[k1])
    s.op("scalar", lambda e: e.activation(out=st1[:, 1:2], in_=st1[:, 1:2], func=AF.Sqrt),
         reads=[k1], writes=[k1])
    s.op("vector", lambda e: e.reciprocal(st1[:, 1:2], st1[:, 1:2]), reads=[k1], writes=[k1])
    s.op("vector", lambda e: e.scalar_tensor_tensor(r[:, :], r[:, :], st1[:, 1:2], S["g"][:, :], ALU.mult, ALU.mult),
         reads=[rk, k1, gk], writes=[rk])
    s.op("vector", lambda e: e.tensor_tensor(r[:, :], r[:, :], S["b"][:, :], ALU.add),
         reads=[rk, bk], writes=[rk])


def ln_consts(ctx, S, gname, bname):
    nc, s, dr, st = ctx["nc"], ctx["s"], ctx["dr"], ctx["st"]
    S["g"] = st.enter_context(nc.sbuf_tensor("lng", [128, D], F32))
    S["b"] = st.enter_context(nc.sbuf_tensor("lnb", [128, D], F32))
    s.dma("sync", S["g"][:, :], dr[gname][:, :], writes=["lng"])
    s.dma("sync", S["b"][:, :], dr[bname][:, :], writes=["lnb"])
    S["st"] = Rot(ctx, "lnst", [128, 2], F32, 2)
    S["sq"] = Rot(ctx, "lnsq", [128, D], BF16, 1)


def epi_res_ln():
    S = {}

    def consts(ctx):
        ln_consts(ctx, S, "ln_g", "ln_b")
        S["r"] = Rot(ctx, "rrow", [128, D], F32, max(2, ctx["nsub"]))
        S["cur"] = {}

    def epi(ctx, ps, keys, b, row0, n0, nbw):
        s, dr = ctx["s"], ctx["dr"]
        if n0 == 0 and row0 not in S["cur"]:
            pass
        key = row0
        if key not in S["cur"]:
            r, rk = S["r"].next()
            S["cur"][key] = (r, rk)
            s.dma("sync", r[:, :], dr["xres"][row0:row0 + 128, :], writes=[rk])
        r, rk = S["cur"][key]
        s.op("vector", lambda e: e.scalar_tensor_tensor(r[:, n0:n0 + nbw], r[:, n0:n0 + nbw], ALPHA,
                                                        ps[0][:, 0:nbw], ALU.mult, ALU.add),
             reads=[rk, keys[0]], writes=[rk])

    def row_final(ctx, b, r0):
        s, dr = ctx["s"], ctx["dr"]
        for sub in range(ctx["nsub"]):
            row0 = r0 + sub * 128
            r, rk = S["cur"].pop(row0)
            ln_rows(ctx, S, r, rk)
            s.dma("sync", dr["y"][b, row0:row0 + 128, :], r[:, :], reads=[rk], is_output=True)

    return consts, epi, row_final


def _mk_consts(nc, s, st):
    C = {}
    C["J"] = st.enter_context(nc.sbuf_tensor("cJ", [128, 512], F32))
    C["U"] = st.enter_context(nc.sbuf_tensor("cU", [128, 128], F32))
    C["onesf"] = st.enter_context(nc.sbuf_tensor("cOf", [128, 128], F32))
    C["onesb"] = st.enter_context(nc.sbuf_tensor("cOb", [128, 128], BF16))
    C["sel"] = st.enter_context(nc.sbuf_tensor("cSel", [128, 128], F32))
    s.op("gpsimd", lambda e: e.iota(C["J"][:, :], [[1, 512]], base=0, channel_multiplier=-1,
                                    allow_small_or_imprecise_dtypes=True), writes=["cJ"])
    s.op("vector", lambda e: e.tensor_single_scalar(C["U"][:, :], C["J"][:, 0:128], 0.0, ALU.is_ge),
         reads=["cJ"], writes=["cU"])
    s.op("vector", lambda e: e.memset(C["onesf"][:, :], 1.0), writes=["cOf"])
    s.op("vector", lambda e: e.memset(C["onesb"][:, :], 1.0), writes=["cOb"])
    s.op("gpsimd", lambda e: e.iota(C["sel"][:, :], [[0, 128]], base=0, channel_multiplier=1,
                                    allow_small_or_imprecise_dtypes=True), writes=["cSel"])
    s.op("vector", lambda e: e.tensor_single_scalar(C["sel"][:, :], C["sel"][:, :], 127.0, ALU.is_equal),
         reads=["cSel"], writes=["cSel"])
    C["Ub"] = st.enter_context(nc.sbuf_tensor("cUb", [128, 128], BF16))
    C["selb"] = st.enter_context(nc.sbuf_tensor("cSelb", [128, 128], BF16))
    s.op("vector", lambda e: e.tensor_copy(C["Ub"][:, :], C["U"][:, :]), reads=["cU"], writes=["cUb"])
    s.op("vector", lambda e: e.tensor_copy(C["selb"][:, :], C["sel"][:, :]), reads=["cSel"], writes=["cSelb"])
    return C


def split_bf16(s, src, skey, parts, tmp, name, n):
    keys = []
    cur, ck = src, skey
    for i in range(n):
        k = (name, i)
        s.op("vector", lambda e, i=i, cur=cur: e.tensor_copy(parts[i], cur), reads=[ck], writes=[k])
        keys.append(k)
        if i + 1 < n:
            tk = (name, "r")
            s.op("vector", lambda e, i=i, cur=cur: e.tensor_tensor(tmp, cur, parts[i], ALU.subtract),
                 reads=[ck, k], writes=[tk])
            cur, ck = tmp, tk
    return keys


def mm_split(s, out, okey, lhs_parts, lkeys, rhs_parts, rkeys):
    pairs = [(a, b, ka, kb) for a, ka in zip(lhs_parts, lkeys) for b, kb in zip(rhs_parts, rkeys)]
    for idx, (a, b, ka, kb) in enumerate(pairs):
        s.op("tensor", lambda e, a=a, b=b, idx=idx: e.matmul(out, a, b, start=(idx == 0), stop=(idx == len(pairs) - 1)),
             reads=[ka, kb], writes=[okey], signal=(idx == len(pairs) - 1))


def build_attn(T, NH=2):
    nc = bass.Bass("TRN2", target_bir_lowering=False)
    NBk = T // 128
    NQ = T // 512
    qT = nc.dram_tensor("qT", [NH, 128, T], F32, kind="ExternalInput").ap()
    kT = nc.dram_tensor("kT", [NH, 128, T], F32, kind="ExternalInput").ap()
    v = nc.dram_tensor("v", [NH, T, 128], F32, kind="ExternalInput").ap()
    fac = nc.dram_tensor("fac", [NH, 128, NBk], F32, kind="ExternalInput").ap()
    bfb = nc.dram_tensor("bfb", [NH, 128, 1], F32, kind="ExternalInput").ap()
    oT = nc.dram_tensor("oT", [NH, 128, T], F32, kind="ExternalOutput").ap()
    scale = float(HD) ** -0.5
    with ExitStack() as st:
        s = Sched(nc, st)
        C = _mk_consts(nc, s, st)
        sb = lambda n, shp, dt=F32: st.enter_context(nc.sbuf_tensor(n, shp, dt))
        masks = [sb(f"mask{i}", [128, 512], BF16) for i in range(4)]
        for i in range(4):
            s.op("vector", lambda e, i=i: e.tensor_single_scalar(masks[i][:, :], C["J"][:, :], 128.0 * i, ALU.is_ge),
                 reads=["cJ"], writes=[("mask", i)])
        q_sb = sb("q_sb", [128, T], BF16)
        k_sb = sb("k_sb", [128, T], BF16)
        v_sb = sb("v_sb", [128, NBk, 128], BF16)
        z = sb("z", [128, NBk]); a = sb("a", [128, NBk]); ls = sb("ls", [128, NBk])
        tot = sb("tot", [128, NBk]); incl = sb("incl", [128, NBk]); ccol = sb("ccol", [128, NBk])
        lsp = [sb(f"lsp{i}", [128, NBk], BF16) for i in range(3)]; lst = sb("lst", [128, NBk])
        cm = sb("cm", [128, NBk]); bfs = sb("bfs", [128, 1]); biast = [sb(f"biast{i}", [128, NBk]) for i in range(2)]
        Pb = [sb(f"P{i}", [128, 512], BF16) for i in range(3)]
        rec = [sb(f"rec{i}", [128, 512]) for i in range(2)]
        osb = [sb(f"osb{i}", [128, 512]) for i in range(2)]
        psS = [st.enter_context(nc.psum_tensor(f"psS{i}", [128, 512], F32)) for i in range(3)]
        psO = [st.enter_context(nc.psum_tensor(f"psO{i}", [128, 512], F32)) for i in range(2)]
        psD = [st.enter_context(nc.psum_tensor(f"psD{i}", [128, 512], F32)) for i in range(2)]
        psM = st.enter_context(nc.psum_tensor("psM", [128, 512], F32))
        gq = 0
        for h in range(NH):
            s.dma("sync", z[:, :], fac[h], writes=["z"])
            s.dma("sync", bfs[:, :], bfb[h], writes=["bfs"])
            s.op("vector", lambda e: e.tensor_scalar(z[:, :], z[:, :], bfs[:, 0:1], None, ALU.add),
                 reads=["z", "bfs"], writes=["z"])
            s.op("vector", lambda e: e.tensor_scalar(a[:, :], z[:, :], -1.0, None, ALU.mult), reads=["z"], writes=["a"])
            s.op("vector", lambda e: e.tensor_tensor(a[:, :], a[:, :], z[:, :], ALU.max), reads=["z", "a"], writes=["a"])
            s.op("scalar", lambda e: e.activation(out=a[:, :], in_=a[:, :], func=AF.Exp, scale=-1.0), reads=["a"], writes=["a"])
            s.op("scalar", lambda e: e.activation(out=a[:, :], in_=a[:, :], func=AF.Ln, bias=1.0), reads=["a"], writes=["a"])
            s.op("vector", lambda e: e.tensor_scalar_min(ls[:, :], z[:, :], 0.0), reads=["z"], writes=["ls"])
            s.op("vector", lambda e: e.tensor_tensor(ls[:, :], ls[:, :], a[:, :], ALU.subtract), reads=["ls", "a"], writes=["ls"])
            lk = split_bf16(s, ls[:, :], "ls", [x[:, :] for x in lsp], lst[:, :], "lsp", 3)
            mm_split(s, psM[:, 0:NBk], "psM0", [C["Ub"][:, :]], ["cUb"], [x[:, :] for x in lsp], lk)
            mm_split(s, psM[:, NBk:2 * NBk], "psM1", [C["onesb"][:, :]], ["cOb"], [x[:, :] for x in lsp], lk)
            s.op("vector", lambda e: e.tensor_copy(tot[:, :], psM[:, NBk:2 * NBk]), reads=["psM1"], writes=["tot"])
            s.op("vector", lambda e: e.tensor_tensor_scan(incl[:, :], C["onesf"][:, 0:NBk], tot[:, :], 0.0, ALU.mult, ALU.add),
                 reads=["tot", "cOf"], writes=["incl"])
            s.op("vector", lambda e: e.tensor_tensor(incl[:, :], incl[:, :], tot[:, :], ALU.subtract), reads=["incl", "tot"], writes=["incl"])
            s.op("vector", lambda e: e.tensor_tensor(ccol[:, :], incl[:, :], psM[:, 0:NBk], ALU.add), reads=["incl", "psM0"], writes=["ccol"])
            ck_ = split_bf16(s, ccol[:, :], "ccol", [x[:, :] for x in lsp], lst[:, :], "lsp", 3)
            mm_split(s, psM[:, 2 * NBk:3 * NBk], "psM2", [C["selb"][:, :]], ["cSelb"], [x[:, :] for x in lsp], ck_)
            s.op("vector", lambda e: e.tensor_copy(cm[:, :], psM[:, 2 * NBk:3 * NBk]), reads=["psM2"], writes=["cm"])
            for j in range(4):
                sl = slice(j * T // 4, (j + 1) * T // 4)
                s.dma("gpsimd", q_sb[:, sl], qT[h, :, sl], writes=[("q", j)])
                s.dma("gpsimd", k_sb[:, sl], kT[h, :, sl], writes=[("k", j)])
                bs = slice(j * NBk // 4, (j + 1) * NBk // 4)
                s.dma("gpsimd", v_sb[:, bs, :], v[h].rearrange("(b p) d -> p b d", p=128)[:, bs, :], writes=[("v", j)])
            allq = [("q", j) for j in range(4)]; allk = [("k", j) for j in range(4)]; allv = [("v", j) for j in range(4)]
            for qb in range(NQ):
                nkb = 4 * (qb + 1)
                par = gq % 2
                gq += 1
                bt = biast[par]
                s.op("vector", lambda e, bt=bt, qb=qb, nkb=nkb: e.tensor_scalar(
                    bt[:, 0:nkb], ccol[:, 0:nkb], -1.0, cm[:, 4 * qb + 1:4 * qb + 2], ALU.mult, ALU.add),
                    reads=["ccol", "cm"], writes=[("bt", par)])
                qs = slice(qb * 512, (qb + 1) * 512)

                def mmS(kb):
                    s.op("tensor", lambda e: e.matmul(psS[kb % 3][:, :], k_sb[:, kb * 128:(kb + 1) * 128], q_sb[:, qs],
                                                      start=True, stop=True),
                         reads=allq + allk, writes=[("S", kb % 3)])
                mmS(0)
                mmS(1)
                for kb in range(nkb):
                    if kb + 2 < nkb:
                        mmS(kb + 2)
                    P = Pb[kb % 3]
                    pk = ("P", kb % 3)
                    s.op("scalar", lambda e, P=P, kb=kb, bt=bt: e.activation(
                        out=P[:, :], in_=psS[kb % 3][:, :], func=AF.Exp, bias=bt[:, kb:kb + 1], scale=scale),
                        reads=[("S", kb % 3), ("bt", par)], writes=[pk])
                    di = kb - 4 * qb
                    if di >= 0:
                        s.op("vector", lambda e, P=P, di=di: e.tensor_tensor(P[:, :], P[:, :], masks[di][:, :], ALU.mult),
                             reads=[pk, ("mask", di)], writes=[pk])
                    s.op("tensor", lambda e, P=P, kb=kb: e.matmul(psO[par][:, :], v_sb[:, kb, :], P[:, :],
                                                                  start=(kb == 0), stop=(kb == nkb - 1)),
                         reads=allv + [pk], writes=[("O", par)], signal=False)
                    s.op("tensor", lambda e, P=P, kb=kb: e.matmul(psD[par][:, :], C["onesb"][:, :], P[:, :],
                                                                  start=(kb == 0), stop=(kb == nkb - 1)),
                         reads=["cOb", pk], writes=[("O", par), ("Dn", par)])
                s.op("vector", lambda e: e.reciprocal(rec[par][:, :], psD[par][:, :]), reads=[("Dn", par)], writes=[("rec", par)])
                s.op("vector", lambda e: e.tensor_tensor(osb[par][:, :], psO[par][:, :], rec[par][:, :], ALU.mult),
                     reads=[("O", par), ("rec", par)], writes=[("osb", par)])
                s.dma("sync", oT[h, :, qs], osb[par][:, :], reads=[("osb", par)], is_output=True)
        s.finish()
    return nc


def build_hgrn(T, NH=2):
    nc = bass.Bass("TRN2", target_bir_lowering=False)
    NCH = T // 128
    SC = 4 if NCH % 4 == 0 else 1
    din = lambda n, shp: nc.dram_tensor(n, shp, F32, kind="ExternalInput").ap()
    fb_tm = din("fb_tm", [NH, T, 128]); ib_tm = din("ib_tm", [NH, T, 128])
    fbT = din("fbT", [NH, 128, T]); qT = din("qT", [NH, 128, T]); gbT = din("gbT", [NH, 128, T])
    a0r = din("a0r", [NH, 128, 128]); a1r = din("a1r", [NH, 128, 128])
    a0c = din("a0c", [NH, 128, 1]); a1c = din("a1c", [NH, 128, 1]); gnc = din("gnc", [NH, 128, 1])
    obT = nc.dram_tensor("obT", [NH, 128, T], F32, kind="ExternalOutput").ap()
    scale = float(HD) ** -0.5
    assert NH * 4 <= 8
    with ExitStack() as st:
        s = Sched(nc, st)
        C = _mk_consts(nc, s, st)
        U = C["U"]
        cnt = [0]

        def sb(shp, dt=F32):
            cnt[0] += 1
            return st.enter_context(nc.sbuf_tensor(f"h{cnt[0]}", shp, dt))

        class NS:
            pass
        V = lambda f, r, w: s.op("vector", f, reads=r, writes=w)
        A = lambda f, r, w: s.op("scalar", f, reads=r, writes=w)
        PE = lambda f, r, w: s.op("tensor", f, reads=r, writes=w)
        H = []
        for h in range(NH):
            n = NS()
            n.h = h
            n.lbr = sb([128, 128]); n.omr = sb([128, 128]); n.lbc = sb([128, 1]); n.omc = sb([128, 1]); n.gn = sb([128, 1])
            n.t1 = sb([128, 128]); n.t1c = sb([128, 1])
            n.in_fb = [sb([128, SC, 128]) for _ in range(2)]; n.in_fbT = [sb([128, SC * 128]) for _ in range(2)]
            n.in_q = [sb([128, SC * 128]) for _ in range(2)]; n.in_g = [sb([128, SC * 128]) for _ in range(2)]
            n.in_v = [sb([128, SC, 128], BF16) for _ in range(2)]
            n.LFp = [sb([128, 128], BF16) for _ in range(3)]; n.LFt = sb([128, 128])
            n.SQp = [sb([128, 128], BF16) for _ in range(2)]; n.SQt = sb([128, 128])
            n.Ftm = sb([128, 128]); n.LF = sb([128, 128]); n.KKtm = sb([128, 128]); n.Ffm = sb([128, 128]); n.KKfm = sb([128, 128])
            n.bfm = sb([128, 128]); n.btm = sb([128, 128]); n.nbm = sb([128, 1])
            n.eQ = sb([128, 128]); n.eK = sb([128, 128]); n.eB = sb([128, 128])
            n.Qt = sb([128, 128], BF16); n.Kt = sb([128, 128], BF16); n.Qb = sb([128, 128], BF16)
            n.ATm = sb([128, 128], BF16); n.dif = sb([128, 128]); n.Kh = sb([128, 128], BF16)
            n.S = sb([128, 128]); n.Sbf = sb([128, 128], BF16); n.ebl = sb([128, 1])
            n.osb = sb([128, 128]); n.sq = sb([128, 128]); n.rstd = sb([128, 128]); n.sg = sb([128, 128])
            n.res = [sb([128, SC * 128]) for _ in range(2)]
            n.pA = st.enter_context(nc.psum_tensor(f"pA{h}", [128, 512], F32))
            n.pD = st.enter_context(nc.psum_tensor(f"pD{h}", [128, 512], F32))
            n.pB = st.enter_context(nc.psum_tensor(f"pB{h}", [128, 512], F32))
            n.pC = st.enter_context(nc.psum_tensor(f"pC{h}", [128, 512], F32))
            H.append(n)

        def head_setup(n):
            h = n.h
            k = lambda name: (name, h)
            s.dma("sync", n.lbr[:, :], a0r[h], writes=[k("lbr")]); s.dma("sync", n.t1[:, :], a1r[h], writes=[k("t1")])
            s.dma("sync", n.lbc[:, :], a0c[h], writes=[k("lbc")]); s.dma("sync", n.t1c[:, :], a1c[h], writes=[k("t1c")])
            s.dma("sync", n.gn[:, :], gnc[h], writes=[k("gn")])
            V(lambda e: e.tensor_tensor(n.lbr[:, :], n.lbr[:, :], n.t1[:, :], ALU.subtract), [k("lbr"), k("t1")], [k("lbr")])
            A(lambda e: e.activation(out=n.lbr[:, :], in_=n.lbr[:, :], func=AF.Sigmoid), [k("lbr")], [k("lbr")])
            V(lambda e: e.tensor_scalar(n.omr[:, :], n.lbr[:, :], -1.0, 1.0, ALU.mult, ALU.add), [k("lbr")], [k("omr")])
            V(lambda e: e.tensor_tensor(n.lbc[:, :], n.lbc[:, :], n.t1c[:, :], ALU.subtract), [k("lbc"), k("t1c")], [k("lbc")])
            A(lambda e: e.activation(out=n.lbc[:, :], in_=n.lbc[:, :], func=AF.Sigmoid), [k("lbc")], [k("lbc")])
            V(lambda e: e.tensor_scalar(n.omc[:, :], n.lbc[:, :], -1.0, 1.0, ALU.mult, ALU.add), [k("lbc")], [k("omc")])
            V(lambda e: e.memset(n.S[:, :], 0.0), [], [k("S")])
            V(lambda e: e.memset(n.Sbf[:, :], 0.0), [], [k("Sbf")])

        def loads(n, sc):
            h = n.h
            k = lambda name: (name, h)
            p = sc % 2
            ts = slice(sc * SC * 128, (sc + 1) * SC * 128)
            s.dma("sync", n.in_fb[p][:, :, :], fb_tm[h, ts, :].rearrange("(c p) d -> p c d", p=128), writes=[k(("ifb", p))])
            s.dma("sync", n.in_fbT[p][:, :], fbT[h, :, ts], writes=[k(("ifbT", p))])
            s.dma("sync", n.in_q[p][:, :], qT[h, :, ts], writes=[k(("iq", p))])
            s.dma("sync", n.in_g[p][:, :], gbT[h, :, ts], writes=[k(("ig", p))])
            s.dma("gpsimd", n.in_v[p][:, :, :], ib_tm[h, ts, :].rearrange("(c p) d -> p c d", p=128), writes=[k(("iv", p))])

        def chunk(n, c):
            ops = []
            h = n.h
            k = lambda name: (name, h)
            sc, ci = divmod(c, SC)
            p = sc % 2
            cs = slice(ci * 128, (ci + 1) * 128)
            V = lambda f, r, w: ops.append(lambda: s.op("vector", f, reads=r, writes=w))
            A = lambda f, r, w: ops.append(lambda: s.op("scalar", f, reads=r, writes=w))
            PE = lambda f, r, w: ops.append(lambda: s.op("tensor", f, reads=r, writes=w))
            fbt, fbTt, qt, gt, vt = n.in_fb[p][:, ci, :], n.in_fbT[p][:, cs], n.in_q[p][:, cs], n.in_g[p][:, cs], n.in_v[p][:, ci, :]
            kfb, kfbT, kq, kg, kv = k(("ifb", p)), k(("ifbT", p)), k(("iq", p)), k(("ig", p)), k(("iv", p))
            A(lambda e: e.activation(out=n.Ftm[:, :], in_=fbt, func=AF.Exp, scale=-1.0), [kfb], [k("Ftm")])
            V(lambda e: e.tensor_scalar(n.Ftm[:, :], n.Ftm[:, :], 1.0, None, ALU.add), [k("Ftm")], [k("Ftm")])
            V(lambda e: e.reciprocal(n.Ftm[:, :], n.Ftm[:, :]), [k("Ftm")], [k("Ftm")])
            V(lambda e: e.tensor_tensor(n.Ftm[:, :], n.Ftm[:, :], n.omr[:, :], ALU.mult), [k("Ftm"), k("omr")], [k("Ftm")])
            V(lambda e: e.tensor_tensor(n.Ftm[:, :], n.Ftm[:, :], n.lbr[:, :], ALU.add), [k("Ftm"), k("lbr")], [k("Ftm")])
            A(lambda e: e.activation(out=n.LF[:, :], in_=n.Ftm[:, :], func=AF.Ln), [k("Ftm")], [k("LF")])
            V(lambda e: e.tensor_scalar(n.KKtm[:, :], n.Ftm[:, :], -1.0, 1.0, ALU.mult, ALU.add), [k("Ftm")], [k("KKtm")])
            A(lambda e: e.activation(out=n.Ffm[:, :], in_=fbTt, func=AF.Exp, scale=-1.0), [kfbT], [k("Ffm")])
            V(lambda e: e.tensor_scalar(n.Ffm[:, :], n.Ffm[:, :], 1.0, None, ALU.add), [k("Ffm")], [k("Ffm")])
            V(lambda e: e.reciprocal(n.Ffm[:, :], n.Ffm[:, :]), [k("Ffm")], [k("Ffm")])
            V(lambda e: e.tensor_scalar(n.Ffm[:, :], n.Ffm[:, :], n.omc[:, 0:1], n.lbc[:, 0:1], ALU.mult, ALU.add),
              [k("Ffm"), k("omc"), k("lbc")], [k("Ffm")])
            V(lambda e: e.tensor_scalar(n.KKfm[:, :], n.Ffm[:, :], -1.0, 1.0, ALU.mult, ALU.add), [k("Ffm")], [k("KKfm")])
            lp = [x[:, :] for x in n.LFp]
            lp = lp[0:2]
            lk = [(k("LFp"), i) for i in range(2)]
            ops.append(lambda: split_bf16(s, n.LF[:, :], k("LF"), lp, n.LFt[:, :], k("LFp"), 2))
            ops.append(lambda: mm_split(s, n.pA[:, 0:128], k("pA0"), [C["Ub"][:, :]], ["cUb"], lp, lk))
            ops.append(lambda: mm_split(s, n.pA[:, 128:256], k("pA1"), lp, lk, [C["Ub"][:, :]], ["cUb"]))
            ops.append(lambda: mm_split(s, n.pD[:, 256:384], k("pA2"), [C["onesb"][:, :]], ["cOb"], lp, lk))
            A(lambda e: e.activation(out=n.bfm[:, :], in_=n.pA[:, 128:256], func=AF.Copy), [k("pA1")], [k("bfm")])
            A(lambda e: e.activation(out=n.btm[:, :], in_=n.pA[:, 0:128], func=AF.Copy), [k("pA0")], [k("btm")])
            V(lambda e: e.tensor_scalar(n.nbm[:, :], n.bfm[:, 63:64], -1.0, None, ALU.mult), [k("bfm")], [k("nbm")])
            A(lambda e: e.activation(out=n.eQ[:, :], in_=n.bfm[:, :], func=AF.Exp, bias=n.nbm[:, 0:1], scale=1.0), [k("bfm"), k("nbm")], [k("eQ")])
            A(lambda e: e.activation(out=n.eK[:, :], in_=n.bfm[:, :], func=AF.Exp, bias=n.bfm[:, 63:64], scale=-1.0), [k("bfm")], [k("eK")])
            A(lambda e: e.activation(out=n.eB[:, :], in_=n.bfm[:, :], func=AF.Exp), [k("bfm")], [k("eB")])
            A(lambda e: e.activation(out=n.ebl[:, :], in_=n.bfm[:, 127:128], func=AF.Exp), [k("bfm")], [k("ebl")])
            V(lambda e: e.scalar_tensor_tensor(n.Qt[:, :], qt, scale, n.eQ[:, :], ALU.mult, ALU.mult), [kq, k("eQ")], [k("Qt")])
            V(lambda e: e.tensor_tensor(n.Kt[:, :], n.KKfm[:, :], n.eK[:, :], ALU.mult), [k("KKfm"), k("eK")], [k("Kt")])
            V(lambda e: e.scalar_tensor_tensor(n.Qb[:, :], qt, scale, n.eB[:, :], ALU.mult, ALU.mult), [kq, k("eB")], [k("Qb")])
            PE(lambda e: e.matmul(n.pB[:, 0:128], n.Kt[:, :], n.Qt[:, :], start=True, stop=True), [k("Kt"), k("Qt")], [k("pB")])
            V(lambda e: e.tensor_tensor(n.ATm[:, :], n.pB[:, 0:128], U[:, :], ALU.mult), [k("pB"), "cU"], [k("ATm")])
            V(lambda e: e.tensor_tensor(n.dif[:, :], n.pD[:, 256:384], n.btm[:, :], ALU.subtract), [k("pA2"), k("btm")], [k("dif")])
            A(lambda e: e.activation(out=n.dif[:, :], in_=n.dif[:, :], func=AF.Exp), [k("dif")], [k("dif")])
            V(lambda e: e.tensor_tensor(n.Kh[:, :], n.KKtm[:, :], n.dif[:, :], ALU.mult), [k("KKtm"), k("dif")], [k("Kh")])
            ops.append(lambda: s.op("tensor", lambda e: e.matmul(n.pC[:, 0:128], vt, n.ATm[:, :], start=True, stop=False),
                                    reads=[kv, k("ATm")], writes=[k("pC")], signal=False))
            PE(lambda e: e.matmul(n.pC[:, 0:128], n.Sbf[:, :], n.Qb[:, :], start=False, stop=True),
               [k("Sbf"), k("Qb"), kv, k("ATm")], [k("pC")])
            PE(lambda e: e.matmul(n.pD[:, 0:128], n.Kh[:, :], vt, start=True, stop=True), [k("Kh"), kv], [k("pD")])
            V(lambda e: e.scalar_tensor_tensor(n.S[:, :], n.S[:, :], n.ebl[:, 0:1], n.pD[:, 0:128], ALU.mult, ALU.add),
              [k("S"), k("ebl"), k("pD")], [k("S")])
            A(lambda e: e.activation(out=n.Sbf[:, :], in_=n.S[:, :], func=AF.Copy), [k("S")], [k("Sbf")])
            A(lambda e: e.activation(out=n.osb[:, :], in_=n.pC[:, 0:128], func=AF.Copy), [k("pC")], [k("osb")])
            A(lambda e: e.activation(out=n.sq[:, :], in_=n.pC[:, 0:128], func=AF.Square), [k("pC")], [k("sq")])
            qp = [x[:, :] for x in n.SQp]
            qk_ = [(k("SQp"), i) for i in range(2)]
            ops.append(lambda: split_bf16(s, n.sq[:, :], k("sq"), qp, n.SQt[:, :], k("SQp"), 2))
            ops.append(lambda: mm_split(s, n.pB[:, 256:384], k("pE"), [C["onesb"][:, :]], ["cOb"], qp, qk_))
            A(lambda e: e.activation(out=n.rstd[:, :], in_=n.pB[:, 256:384], func=AF.Ln, bias=RMS_EPS, scale=1.0 / HD), [k("pE")], [k("rstd")])
            A(lambda e: e.activation(out=n.rstd[:, :], in_=n.rstd[:, :], func=AF.Exp, scale=-0.5), [k("rstd")], [k("rstd")])
            A(lambda e: e.activation(out=n.sg[:, :], in_=gt, func=AF.Exp, scale=-1.0), [kg], [k("sg")])
            V(lambda e: e.tensor_scalar(n.sg[:, :], n.sg[:, :], 1.0, None, ALU.add), [k("sg")], [k("sg")])
            V(lambda e: e.reciprocal(n.sg[:, :], n.sg[:, :]), [k("sg")], [k("sg")])
            V(lambda e: e.tensor_tensor(n.sg[:, :], n.sg[:, :], gt, ALU.mult), [k("sg"), kg], [k("sg")])
            r = n.res[p][:, cs]
            kr = k(("res", p, ci))
            V(lambda e: e.tensor_tensor(r, n.osb[:, :], n.rstd[:, :], ALU.mult), [k("osb"), k("rstd")], [kr])
            V(lambda e: e.scalar_tensor_tensor(r, r, n.gn[:, 0:1], n.sg[:, :], ALU.mult, ALU.mult), [kr, k("gn"), k("sg")], [kr])
            if ci == SC - 1:
                ts = slice(sc * SC * 128, (sc + 1) * SC * 128)
                ops.append(lambda: s.dma("sync", obT[h, :, ts], n.res[p][:, :], reads=[k(("res", p, j)) for j in range(SC)],
                                         is_output=True))
            return ops

        from itertools import zip_longest
        for n in H:
            head_setup(n)
            loads(n, 0)
        for c in range(NCH):
            sc, ci = divmod(c, SC)
            if ci == 0 and sc + 1 < NCH // SC:
                for n in H:
                    loads(n, sc + 1)
            for group in zip_longest(*[chunk(n, c) for n in H]):
                for th in group:
                    if th is not None:
                        th()
        s.finish()
    return nc


def build_route(M):
    nc = bass.Bass("TRN2", target_bir_lowering=False)
    lg = nc.dram_tensor("lg", [M, 72], F32, kind="ExternalInput").ap()
    R = nc.dram_tensor("R", [M, 8], F32, kind="ExternalOutput").ap()
    with ExitStack() as st:
        s = Sched(nc, st)
        cnt = [0]

        def sb(shp, dt=F32):
            cnt[0] += 1
            return st.enter_context(nc.sbuf_tensor(f"r{cnt[0]}", shp, dt))
        io8 = sb([128, 8])
        s.op("gpsimd", lambda e: e.iota(io8[:, :], [[1, 8]], base=0, channel_multiplier=0,
                                        allow_small_or_imprecise_dtypes=True), writes=["io8"])
        V = lambda f, r, w: s.op("vector", f, reads=r, writes=w)
        A = lambda f, r, w: s.op("scalar", f, reads=r, writes=w)
        Ls = [sb([128, 72]) for _ in range(2)]
        Ro = [sb([128, 8]) for _ in range(2)]
        G8 = sb([128, 8]); GI = sb([128, 8], U32); gs = sb([128, 1]); nb = sb([128, 1]); ex = sb([128, 8]); sm = sb([128, 1])
        oh = sb([128, 8]); EL = sb([128, 8]); E8 = sb([128, 8]); EI = sb([128, 8], U32); d = sb([128, 1]); r1 = sb([128, 1])
        for blk in range(M // 128):
            p = blk % 2
            L, Rt = Ls[p], Ro[p]
            rs = slice(blk * 128, (blk + 1) * 128)
            s.dma("sync", L[:, :], lg[rs, :], writes=[("L", p)])
            V(lambda e: e.memset(Rt[:, :], 0.0), [], [("R", p)])
            V(lambda e: e.max(G8[:, :], L[:, 0:8]), [("L", p)], ["G8"])
            V(lambda e: e.max_index(GI[:, :], G8[:, :], L[:, 0:8]), [("L", p), "G8"], ["GI"])
            V(lambda e: e.tensor_copy(gs[:, :], GI[:, 0:1]), ["GI"], ["gs"])
            V(lambda e: e.tensor_scalar(nb[:, :], G8[:, 0:1], -1.0, None, ALU.mult), ["G8"], ["nb"])
            A(lambda e: e.activation(out=ex[:, :], in_=L[:, 0:8], func=AF.Exp, bias=nb[:, 0:1], scale=1.0, accum_out=sm[:, 0:1]),
              [("L", p), "nb"], ["ex", "sm"])
            V(lambda e: e.reciprocal(sm[:, :], sm[:, :]), ["sm"], ["sm"])
            V(lambda e: e.tensor_scalar(oh[:, :], io8[:, :], gs[:, 0:1], None, ALU.is_equal), ["io8", "gs"], ["oh"])
            V(lambda e: e.tensor_scalar(EL[:, :], L[:, 8:16], oh[:, 0:1], None, ALU.mult), [("L", p), "oh"], ["EL"])
            for g in range(1, 8):
                V(lambda e, g=g: e.scalar_tensor_tensor(EL[:, :], L[:, 8 + 8 * g:16 + 8 * g], oh[:, g:g + 1], EL[:, :], ALU.mult, ALU.add),
                  [("L", p), "oh", "EL"], ["EL"])
            V(lambda e: e.max(E8[:, :], EL[:, :]), ["EL"], ["E8"])
            V(lambda e: e.max_index(EI[:, :], E8[:, :], EL[:, :]), ["EL", "E8"], ["EI"])
            V(lambda e: e.tensor_tensor(d[:, :], E8[:, 1:2], E8[:, 0:1], ALU.subtract), ["E8"], ["d"])
            A(lambda e: e.activation(out=d[:, :], in_=d[:, :], func=AF.Exp), ["d"], ["d"])
            V(lambda e: e.tensor_scalar(r1[:, :], d[:, :], 1.0, None, ALU.add), ["d"], ["r1"])
            V(lambda e: e.reciprocal(r1[:, :], r1[:, :]), ["r1"], ["r1"])
            V(lambda e: e.tensor_tensor(r1[:, :], r1[:, :], sm[:, :], ALU.mult), ["r1", "sm"], ["r1"])
            V(lambda e: e.tensor_copy(Rt[:, 0:1], gs[:, :]), ["gs", ("R", p)], [("R", p)])
            V(lambda e: e.tensor_copy(Rt[:, 1:3], EI[:, 0:2]), ["EI", ("R", p)], [("R", p)])
            V(lambda e: e.tensor_copy(Rt[:, 3:4], r1[:, :]), ["r1", ("R", p)], [("R", p)])
            V(lambda e: e.tensor_tensor(Rt[:, 4:5], r1[:, :], d[:, :], ALU.mult), ["r1", "d", ("R", p)], [("R", p)])
            s.dma("sync", R[rs, :], Rt[:, :], reads=[("R", p)], is_output=True)
        s.finish()
    return nc


def build_ln2(M):
    nc = bass.Bass("TRN2", target_bir_lowering=False)
    din = lambda n, shp: nc.dram_tensor(n, shp, F32, kind="ExternalInput").ap()
    dr = dict(x1=din("x1", [M, D]), o1=din("o1", [M, D]), o2=din("o2", [M, D]),
              ln_g=din("ln_g", [128, D]), ln_b=din("ln_b", [128, D]))
    y = nc.dram_tensor("y", [M, D], F32, kind="ExternalOutput").ap()
    with ExitStack() as st:
        s = Sched(nc, st)
        ctx = dict(nc=nc, s=s, st=st, dr=dr)
        S = {}
        ln_consts(ctx, S, "ln_g", "ln_b")
        rr = Rot(ctx, "r", [128, D], F32, 2)
        aa = Rot(ctx, "a", [128, D], F32, 2)
        for blk in range(M // 128):
            rs = slice(blk * 128, (blk + 1) * 128)
            r, rk = rr.next()
            s.dma("sync", r[:, :], dr["x1"][rs, :], writes=[rk])
            a, ak = aa.next()
            s.dma("sync", a[:, :], dr["o1"][rs, :], writes=[ak])
            s.op("vector", lambda e, r=r, a=a: e.scalar_tensor_tensor(r[:, :], r[:, :], ALPHA, a[:, :], ALU.mult, ALU.add),
                 reads=[rk, ak], writes=[rk])
            a, ak = aa.next()
            s.dma("sync", a[:, :], dr["o2"][rs, :], writes=[ak])
            s.op("vector", lambda e, r=r, a=a: e.tensor_tensor(r[:, :], r[:, :], a[:, :], ALU.add), reads=[rk, ak], writes=[rk])
            ln_rows(ctx, S, r, rk)
            s.dma("sync", y[rs, :], r[:, :], reads=[rk], is_output=True)
        s.finish()
    return nc


def epi_ple():
    S = {}

    def consts(ctx):
        nc, s, dr = ctx["nc"], ctx["s"], ctx["dr"]
        S["o"] = Rot(ctx, "ost", [128, 512], F32, 3)
        S["x"] = Rot(ctx, "xst", [128, 512], F32, 3)
        S["b"] = ctx["st"].enter_context(nc.sbuf_tensor("biasb", [128, ctx["N"]], F32))
        s.dma("sync", S["b"][:, :], dr["bias"][:, :], writes=["biasb"])

    def epi(ctx, ps, keys, b, row0, n0, nbw):
        s, dr = ctx["s"], ctx["dr"]
        o, ok = S["o"].next()
        x, xk = S["x"].next()
        s.dma("sync", x[:, 0:nbw], dr["xres"][row0:row0 + 128, n0:n0 + nbw], writes=[xk])
        s.op("vector", lambda e: e.tensor_tensor(o[:, 0:nbw], ps[0][:, 0:nbw], S["b"][:, n0:n0 + nbw], ALU.add),
             reads=[keys[0], "biasb"], writes=[ok])
        s.op("scalar", lambda e: e.activation(out=o[:, 0:nbw], in_=o[:, 0:nbw], func=AF.Sigmoid), reads=[ok], writes=[ok])
        s.op("vector", lambda e: e.tensor_tensor(o[:, 0:nbw], o[:, 0:nbw], ps[1][:, 0:nbw], ALU.mult),
             reads=[ok, keys[1]], writes=[ok])
        s.op("vector", lambda e: e.tensor_tensor(o[:, 0:nbw], o[:, 0:nbw], x[:, 0:nbw], ALU.add), reads=[ok, xk], writes=[ok])
        s.dma("sync", dr["y"][b, row0:row0 + 128, n0:n0 + nbw], o[:, 0:nbw], reads=[ok], is_output=True)

    return consts, epi


def _bc(v, n=128):
    v = np.asarray(v, np.float32).reshape(1, -1)
    return np.ascontiguousarray(np.broadcast_to(v, (n, v.shape[1])))


def kernel(x, p, w_in, b_fox_f, hgrn_lb, hgrn_norm_g, w_branch_a, w_branch_b, w_out, ln1_g, ln1_b,
           w_group_router, b_group_router, w_expert_router, b_expert_router, w_exp_gate, w_exp_up,
           w_exp_down, ln2_g, ln2_b, w_ple_gate, b_ple_gate, w_ple_proj):
    x = np.asarray(x, np.float32)
    T = x.shape[1]
    TC = T // NCORES
    X = x[0]
    XT = np.ascontiguousarray(X.T)
    W = np.asarray(w_in[0], np.float32)
    FW = FOXH * HD
    o_q, o_k, o_v, o_f = 0, FW, 2 * FW, 3 * FW
    o_hq = 3 * FW + FOXH
    o_hf, o_hi, o_hg = o_hq + FW, o_hq + 2 * FW, o_hq + 3 * FW
    o_ga = o_hq + 4 * FW
    o_gb = o_ga + D
    cols = []
    for c in range(NCORES):
        hs = [2 * c, 2 * c + 1]
        cc = []
        for base in (o_q, o_k, o_v):
            for h in hs:
                cc.append(np.arange(base + h * HD, base + (h + 1) * HD))
        for base in (o_hq, o_hf, o_hi, o_hg):
            for h in hs:
                cc.append(np.arange(base + h * HD, base + (h + 1) * HD))
        cc.append(np.array([o_f + hs[0], o_f + hs[1]]))
        cols.append(np.concatenate(cc))
    NCOL = len(cols[0])
    c_, e_ = epi_simple()
    nc = build_gemm(T, NCOL, [dict(xt="xt", w="w", K=D)], e_, consts=c_, w_resident=True, xt_bufs=2)
    res = _run(nc, [{"xt": XT[None], "w": _c(W[:, cols[c]])[None]} for c in range(NCORES)])
    U_ = [r["y"][0] for r in res]
    del res
    tr = lambda a: np.ascontiguousarray(a.transpose(0, 2, 1))
    nc = build_attn(T, 2)
    ims = []
    for c in range(NCORES):
        u = U_[c]
        q = np.stack([u[:, 0:128], u[:, 128:256]]); k = np.stack([u[:, 256:384], u[:, 384:512]])
        v = np.stack([u[:, 512:640], u[:, 640:768]])
        fa = np.stack([u[:, 1792], u[:, 1793]])
        ims.append({"qT": tr(q), "kT": tr(k), "v": _c(v),
                    "fac": np.ascontiguousarray(fa.reshape(2, T // 128, 128).transpose(0, 2, 1)),
                    "bfb": _c(np.broadcast_to(np.asarray(b_fox_f[0], np.float32)[2 * c:2 * c + 2, None, None], (2, 128, 1)))})
    res = _run(nc, ims)
    oaT = np.concatenate([r["oT"].reshape(256, T) for r in res], 0)
    nc = build_hgrn(T, 2)
    ims = []
    lbv = np.asarray(hgrn_lb, np.float32)
    gnv = np.asarray(hgrn_norm_g[0], np.float32)
    for c in range(NCORES):
        u = U_[c]
        hq = np.stack([u[:, 768:896], u[:, 896:1024]]); hf = np.stack([u[:, 1024:1152], u[:, 1152:1280]])
        hi = np.stack([u[:, 1280:1408], u[:, 1408:1536]]); hg = np.stack([u[:, 1536:1664], u[:, 1664:1792]])
        a0 = lbv[0].reshape(16, 128)[2 * c:2 * c + 2]; a1 = lbv[1].reshape(16, 128)[2 * c:2 * c + 2]
        gn = gnv.reshape(16, 128)[2 * c:2 * c + 2]
        ims.append({"fb_tm": _c(hf), "ib_tm": _c(hi), "fbT": tr(hf), "qT": tr(hq), "gbT": tr(hg),
                    "a0r": _c(np.broadcast_to(a0[:, None, :], (2, 128, 128))), "a1r": _c(np.broadcast_to(a1[:, None, :], (2, 128, 128))),
                    "a0c": _c(a0[:, :, None]), "a1c": _c(a1[:, :, None]), "gnc": _c(gn[:, :, None])})
    res = _run(nc, ims)
    obT = np.concatenate([r["obT"].reshape(256, T) for r in res], 0)
    del U_, ims
    c_, e_ = epi_merge()
    nc = build_gemm(TC, D, [dict(xt="xt", w="wga", K=D), dict(xt="xt", w="wgb", K=D),
                            dict(xt="at", w="wa", K=FW), dict(xt="bt", w="wb", K=FW)], e_, consts=c_, NB=256)
    wga = _c(W[:, o_ga:o_ga + D])[None]; wgb = _c(W[:, o_gb:o_gb + D])[None]
    wa = _c(w_branch_a[0])[None]; wb = _c(w_branch_b[0])[None]
    sl = lambda c: slice(c * TC, (c + 1) * TC)
    res = _run(nc, [{"xt": _c(XT[:, sl(c)])[None], "at": _c(oaT[:, sl(c)])[None], "bt": _c(obT[:, sl(c)])[None],
                     "wga": wga, "wgb": wgb, "wa": wa, "wb": wb} for c in range(NCORES)])
    Y = np.concatenate([r["y"][0] for r in res], 0)
    del wga, wgb, W
    c_, e_, rf_ = epi_res_ln()
    nc = build_gemm(TC, D, [dict(xt="xt", w="w", K=D)], e_, extra=[("xres", [TC, D]), ("ln_g", [128, D]), ("ln_b", [128, D])],
                    consts=c_, row_final=rf_, NB=256)
    YT = np.ascontiguousarray(Y.T)
    res = _run(nc, [{"xt": _c(YT[:, sl(c)])[None], "w": _c(w_out[0])[None], "xres": _c(X[sl(c)]),
                     "ln_g": _bc(ln1_g[0]), "ln_b": _bc(ln1_b[0])} for c in range(NCORES)])
    X1 = np.concatenate([r["y"][0] for r in res], 0)
    X1T = np.ascontiguousarray(X1.T)
    wr = _c(np.concatenate([w_group_router[0], w_expert_router[0]], 1))[None]
    br = _bc(np.concatenate([b_group_router[0], b_expert_router[0]]))
    c_, e_ = epi_simple(bias="bias")
    nc = build_gemm(TC, 72, [dict(xt="xt", w="w", K=D)], e_, extra=[("bias", [128, 72])], consts=c_)
    res = _run(nc, [{"xt": _c(X1T[:, sl(c)])[None], "w": wr, "bias": br} for c in range(NCORES)])
    nc = build_route(TC)
    res = _run(nc, [{"lg": _c(res[c]["y"][0])} for c in range(NCORES)])
    R = np.concatenate([r["R"] for r in res], 0)
    gsel = np.rint(R[:, 0]).astype(np.int64)
    eid = np.stack([gsel * EPG + np.rint(R[:, 1]).astype(np.int64), gsel * EPG + np.rint(R[:, 2]).astype(np.int64)], 1)
    gate = R[:, 3:5]
    flat_e = eid.reshape(-1)
    order = np.argsort(flat_e, kind="stable")
    counts = np.bincount(flat_e, minlength=NE)
    starts = np.cumsum(counts) - counts
    pos = np.empty(2 * T, np.int64)
    pos[order] = np.arange(2 * T) - starts[flat_e[order]]
    ME = int(max(128, -(-counts.max() // 128) * 128))
    tok_tab = np.full((NE, ME), -1, np.int64)
    tok_tab[flat_e, pos] = np.repeat(np.arange(T), 2)
    gw_tab = np.zeros((NE, ME, 1), np.float32)
    gw_tab[flat_e, pos, 0] = gate.reshape(-1)
    X1p = np.concatenate([X1, np.zeros((1, D), np.float32)], 0)
    c_, e_ = epi_glu()
    nc = build_gemm(ME, DE, [dict(xt="xt", w="wg", K=D), dict(xt="xt", w="wu", K=D)], e_, consts=c_, B=EPG)
    ims = []
    for c in range(NCORES):
        es = slice(c * EPG, (c + 1) * EPG)
        xg = X1p[tok_tab[es]]
        ims.append({"xt": np.ascontiguousarray(xg.transpose(0, 2, 1)), "wg": _c(w_exp_gate[0, es]), "wu": _c(w_exp_up[0, es])})
    res = _run(nc, ims)
    H = [r["y"] for r in res]
    del ims
    c_, e_ = epi_simple(rowscale="rs")
    nc = build_gemm(ME, D, [dict(xt="xt", w="w", K=DE)], e_, extra=[("rs", [EPG, ME, 1])], consts=c_, B=EPG)
    res = _run(nc, [{"xt": np.ascontiguousarray(H[c].transpose(0, 2, 1)), "w": _c(w_exp_down[0, c * EPG:(c + 1) * EPG]),
                     "rs": gw_tab[c * EPG:(c + 1) * EPG]} for c in range(NCORES)])
    O = np.concatenate([r["y"] for r in res], 0)
    pos2 = pos.reshape(T, 2)
    O1 = O[eid[:, 0], pos2[:, 0]]
    O2 = O[eid[:, 1], pos2[:, 1]]
    del O, H
    nc = build_ln2(TC)
    res = _run(nc, [{"x1": _c(X1[sl(c)]), "o1": _c(O1[sl(c)]), "o2": _c(O2[sl(c)]),
                     "ln_g": _bc(ln2_g[0]), "ln_b": _bc(ln2_b[0])} for c in range(NCORES)])
    X2 = np.concatenate([r["y"] for r in res], 0)
    X2T = np.ascontiguousarray(X2.T)
    PT = np.ascontiguousarray(np.asarray(p[0, 0], np.float32).T)
    c_, e_ = epi_ple()
    nc = build_gemm(TC, D, [dict(xt="xt", w="wpg", K=D), dict(xt="pt", w="wpe", K=PLE)], e_,
                    extra=[("xres", [TC, D]), ("bias", [128, D])], consts=c_, SBo=(1024 if TC % 1024 == 0 else None))
    res = _run(nc, [{"xt": _c(X2T[:, sl(c)])[None], "pt": _c(PT[:, sl(c)])[None], "wpg": _c(w_ple_gate[0])[None],
                     "wpe": _c(w_ple_proj[0])[None], "xres": _c(X2[sl(c)]), "bias": _bc(b_ple_gate[0])} for c in range(NCORES)])
    out = np.concatenate([r["y"][0] for r in res], 0)
    return out[None].astype(np.float32)
```
